# Optimizing a Trainium2 kernel written in Bass

```python
import jax, jax.numpy as jnp
from jax import lax
import numpy as np


D_MODEL = 1024
BATCH = 16
SEQ = 2048
DEPTH = 1

CHUNK = 64
D_MIX = D_MODEL
D_POOL = D_MIX // 2
POOL_WINDOWS = (2, 4, 8, 16)
N_POOL_GROUPS = len(POOL_WINDOWS)
POOL_GROUP = D_POOL // N_POOL_GROUPS
D_SGU = D_MIX - D_POOL
SGU_HEADS = 4
SGU_HEAD_DIM = D_SGU // SGU_HEADS
SGU_BLOCK = 128
D_IN = D_POOL + 2 * D_SGU
PEER_HEADS = 8
N_KEYS = 128
N_EXPERTS = N_KEYS * N_KEYS
PEER_TOPK = 16
D_QUERY = 256
D_HALF = D_QUERY // 2
PEER_TOKEN_BLOCK = 128
EPS = 1e-6

kernel_name = "hybrid_pool_sgu_peer_block"


def rmsnorm(x, g):
    xf = x.astype(jnp.float32)
    y = xf * lax.rsqrt(jnp.mean(xf * xf, axis=-1, keepdims=True) + EPS)
    return (y * g.astype(jnp.float32)).astype(x.dtype)


def layernorm(x, g):
    xf = x.astype(jnp.float32)
    mu = jnp.mean(xf, axis=-1, keepdims=True)
    var = jnp.mean(jnp.square(xf - mu), axis=-1, keepdims=True)
    return ((xf - mu) * lax.rsqrt(var + EPS) * g.astype(jnp.float32)).astype(x.dtype)


def pool_mixer(a, pool_w, pool_scale):
    B, S, _ = a.shape
    af = a.astype(jnp.float32).reshape(B, S, N_POOL_GROUPS, POOL_GROUP)
    cs = jnp.cumsum(af, axis=1)
    pos = jnp.arange(S, dtype=jnp.float32)[None, :, None]
    outs = []
    for g, w in enumerate(POOL_WINDOWS):
        csg = cs[:, :, g]
        lagged = jnp.pad(csg, ((0, 0), (w, 0), (0, 0)))[:, :S]
        count = jnp.minimum(pos + 1.0, float(w))
        outs.append((csg - lagged) / count - af[:, :, g])
    d = jnp.stack(outs, axis=2).astype(a.dtype)
    y = jnp.einsum('bsgc,gcd->bsgd', d, pool_w)
    return y.reshape(B, S, D_POOL) * pool_scale


def sgu_mixer(uv, sgu_norm_g, sgu_w, sgu_b):
    B, S, _ = uv.shape
    uv = jax.nn.gelu(uv)
    u, v = uv[..., :D_SGU], uv[..., D_SGU:]
    v = layernorm(v, sgu_norm_g)
    ch = jnp.arange(SGU_BLOCK) // CHUNK
    mask = ch[None, :] <= ch[:, None]
    ws = jnp.where(mask[None], sgu_w, jnp.zeros_like(sgu_w))
    vb = v.reshape(B, S // SGU_BLOCK, SGU_BLOCK, SGU_HEADS, SGU_HEAD_DIM)
    gate = jnp.einsum('hij,bnjhc->bnihc', ws, vb) + jnp.transpose(sgu_b)[None, None, :, :, None]
    return u * gate.reshape(B, S, D_SGU)


def peer_ffn(h, wq, keys, u_tab, v_tab):
    B, S, D = h.shape
    xt = h.reshape((B * S) // PEER_TOKEN_BLOCK, PEER_TOKEN_BLOCK, D)

    def block(xb):
        T = xb.shape[0]
        q = (xb @ wq).reshape(T, PEER_HEADS, 2, D_HALF)
        s = jnp.einsum('thpd,phkd->thpk', q, keys).astype(jnp.float32)
        s_top, i_top = lax.top_k(s, PEER_TOPK)
        cand_s = s_top[:, :, 0, :, None] + s_top[:, :, 1, None, :]
        cand_i = i_top[:, :, 0, :, None] * N_KEYS + i_top[:, :, 1, None, :]
        cand_s = cand_s.reshape(T, PEER_HEADS, PEER_TOPK * PEER_TOPK)
        cand_i = cand_i.reshape(T, PEER_HEADS, PEER_TOPK * PEER_TOPK)
        best_s, best_pos = lax.top_k(cand_s, PEER_TOPK)
        idx = jnp.take_along_axis(cand_i, best_pos, axis=-1)
        gate = jax.nn.softmax(best_s, axis=-1)
        u = u_tab[idx]
        v = v_tab[idx]
        act = jax.nn.gelu(jnp.einsum('td,thkd->thk', xb, u).astype(jnp.float32))
        return jnp.einsum('thk,thkd->td', (gate * act).astype(xb.dtype), v)

    return lax.map(block, xt).reshape(B, S, D)


def setup_inputs(seed: int = 0) -> dict:
    key = jax.random.key(seed)
    ks = jax.random.split(key, 16)
    f32 = jnp.float32
    L, D = DEPTH, D_MODEL
    x = jax.random.normal(ks[0], (BATCH, SEQ, D), f32)
    norm1_g = 1.0 + 0.02 * jax.random.normal(ks[1], (L, D), f32)
    w_in = jax.random.normal(ks[2], (L, D, D_IN), f32) * D ** -0.5
    pool_w = jax.random.normal(ks[3], (L, N_POOL_GROUPS, POOL_GROUP, POOL_GROUP), f32) * POOL_GROUP ** -0.5
    pool_scale = 1.0 + 0.02 * jax.random.normal(ks[4], (L, D_POOL), f32)
    sgu_norm_g = 1.0 + 0.02 * jax.random.normal(ks[5], (L, D_SGU), f32)
    sgu_w = jax.random.normal(ks[6], (L, SGU_HEADS, SGU_BLOCK, SGU_BLOCK), f32) * SGU_BLOCK ** -0.5
    sgu_b = 1.0 + 0.02 * jax.random.normal(ks[7], (L, SGU_HEADS, SGU_BLOCK), f32)
    w_out = jax.random.normal(ks[8], (L, D_MIX, D), f32) * D_MIX ** -0.5
    norm2_g = 1.0 + 0.02 * jax.random.normal(ks[9], (L, D), f32)
    peer_wq = jax.random.normal(ks[10], (L, D, PEER_HEADS * D_QUERY), f32) * D ** -0.5
    peer_keys = jax.random.normal(ks[11], (L, 2, PEER_HEADS, N_KEYS, D_HALF), f32) * D_HALF ** -0.5
    peer_u = jax.random.normal(ks[12], (L, N_EXPERTS, D), f32) * D ** -0.5
    peer_v = jax.random.normal(ks[13], (L, N_EXPERTS, D), f32) * PEER_HEADS ** -0.5
    final_g = 1.0 + 0.02 * jax.random.normal(ks[14], (D,), f32)
    return {"x": x, "norm1_g": norm1_g, "w_in": w_in, "pool_w": pool_w,
            "pool_scale": pool_scale, "sgu_norm_g": sgu_norm_g, "sgu_w": sgu_w,
            "sgu_b": sgu_b, "w_out": w_out, "norm2_g": norm2_g, "peer_wq": peer_wq,
            "peer_keys": peer_keys, "peer_u": peer_u, "peer_v": peer_v,
            "final_g": final_g}


def reference(x, norm1_g, w_in, pool_w, pool_scale, sgu_norm_g, sgu_w, sgu_b,
              w_out, norm2_g, peer_wq, peer_keys, peer_u, peer_v, final_g):
    h = x
    for l in range(DEPTH):
        z = rmsnorm(h, norm1_g[l]) @ w_in[l]
        a = pool_mixer(z[..., :D_POOL], pool_w[l], pool_scale[l])
        b = sgu_mixer(z[..., D_POOL:], sgu_norm_g[l], sgu_w[l], sgu_b[l])
        h = h + jnp.concatenate([a, b], axis=-1) @ w_out[l]
        h = h + peer_ffn(rmsnorm(h, norm2_g[l]), peer_wq[l], peer_keys[l], peer_u[l], peer_v[l])
    return rmsnorm(h, final_g)
```

```python
from contextlib import ExitStack
import numpy as np
import concourse.bass as bass
import concourse.mybir as mybir
from concourse.bass_utils import run_bass_kernel_spmd

F32 = mybir.dt.float32
BF16 = mybir.dt.bfloat16
U32 = mybir.dt.uint32
I32 = mybir.dt.int32
ALU = mybir.AluOpType
AF = mybir.ActivationFunctionType
AX = mybir.AxisListType

D = 1024
SEQ = 2048
NCORES = 8
TOK_PER_CORE = 2 * SEQ
NT = TOK_PER_CORE // 128
TILES_PER_SEQ = SEQ // 128
D_IN = 1536
NEXP = 16384
EPS = 1e-6
WINDOWS = (2, 4, 8, 16)
NS = 4
NS2 = 6
TB = 32
TG = 256
NW = 8
NEG = -1.0e30
ARENA_COLS = 47400
SEM_LIMIT = 12000
JGMAX = 16
EXTRA_SEMS = 0


class Sem:
    def __init__(self, h):
        self.h = h
        self.n = 0


class Res:
    def __init__(self, name):
        self.name = name
        self.writers = {}
        self.readers = {}


class Prog:
    def __init__(self, nc, stack):
        self.nc = nc
        self.stack = stack
        self.all_sems = []
        self.ops = {k: [] for k in ("pe", "dve", "act", "pool", "sp")}
        self.esem = {k: self.new_sem("e_" + k) for k in self.ops}
        self.pe_sems = {self.esem["pe"]}
        self.epoch = {k: 0 for k in self.ops}
        self.pending = {k: False for k in self.ops}
        self.waited = {k: {} for k in self.ops}

    def new_sem(self, name):
        s = Sem(self.stack.enter_context(self.nc.semaphore(name)))
        self.all_sems.append(s)
        return s

    def emit(self, eng, fn, reads=(), writes=(), dma_sem=None, inc_sem=True):
        if dma_sem is None and self.esem[eng].n >= SEM_LIMIT and not self.pending[eng]:
            self.epoch[eng] += 1
            self.esem[eng] = self.new_sem("e_%s_%d" % (eng, self.epoch[eng]))
            if eng == "pe":
                self.pe_sems.add(self.esem[eng])
        mysem = dma_sem if dma_sem is not None else self.esem[eng]
        inc = 16 if dma_sem is not None else 1
        deps = {}
        for r in reads:
            for s, v in r.writers.items():
                deps[s] = max(deps.get(s, 0), v)
        for w in writes:
            for s, v in w.writers.items():
                deps[s] = max(deps.get(s, 0), v)
            for s, v in w.readers.items():
                deps[s] = max(deps.get(s, 0), v)
        waits = []
        for s, v in deps.items():
            if eng == "pe" and s in self.pe_sems and dma_sem is None:
                continue
            if dma_sem is not None and s is dma_sem:
                continue
            if self.waited[eng].get(s, 0) >= v:
                continue
            self.waited[eng][s] = v
            waits.append((s.h, v))
        if inc_sem:
            mysem.n += inc
            val = mysem.n
            if dma_sem is None:
                self.pending[eng] = False
        else:
            assert dma_sem is None
            val = mysem.n + inc
            self.pending[eng] = True
        for w in writes:
            w.writers = {mysem: val}
            w.readers = {}
        for r in reads:
            if r not in writes:
                r.readers[mysem] = val
        h = mysem.h

        def run(e, waits=waits, fn=fn, h=h, inc=inc, inc_sem=inc_sem):
            for (sh, v) in waits:
                e.wait_ge(sh, v)
            if inc_sem:
                fn(e).then_inc(h, inc)
            else:
                fn(e)

        self.ops[eng].append(run)

    def barrier(self):
        snap = [(s, s.n) for s in self.all_sems if s.n > 0]
        for eng in self.ops:
            lst = [(s.h, v) for (s, v) in snap if self.waited[eng].get(s, 0) < v]
            for (s, v) in snap:
                self.waited[eng][s] = v

            def run(e, lst=lst):
                for (sh, v) in lst:
                    e.wait_ge(sh, v)

            self.ops[eng].append(run)

    def final_wait(self, eng, sems):
        lst = [(s.h, s.n) for s in sems if s.n > 0]

        def run(e, lst=lst):
            for (sh, v) in lst:
                e.wait_ge(sh, v)

        self.ops[eng].append(run)


def bc_ap(ap, dims):
    return bass.AP(ap.tensor, ap.offset, [list(ap.ap[0])] + [list(d) for d in dims])


def build_program(NT=NT, NHALF=2, debug=False):
    TOK = NT * 128
    NTH = NT // NHALF
    THALF = NTH * 128
    NG = THALF // TG
    nc = bass.Bass("TRN2", target_bir_lowering=False)
    dt_in = lambda name, shape, dt=F32: nc.dram_tensor(name, list(shape), dt, kind="ExternalInput").ap()
    x_d = dt_in("x", [TOK, D])
    w_in_d = dt_in("w_in", [D, D_IN])
    w_out_d = dt_in("w_out", [D, D])
    pool_w_d = dt_in("pool_w", [4, 128, 128])
    wst_d = dt_in("sgu_wt", [4, 128, 128])
    sgub_d = dt_in("sgu_b", [512])
    sgug_d = dt_in("sgu_g", [512])
    g1col_d = dt_in("g1col", [128, 8])
    pscale_d = dt_in("pscale", [128, 4])
    g2_d = dt_in("g2", [D])
    fg_d = dt_in("fg", [D])
    wqt_d = dt_in("wq_t", [16, 128, D])
    keyst_d = dt_in("keys_t", [16, 128, 128])
    ut_d = dt_in("peer_ut", [128, 128, D])
    vt_d = dt_in("peer_vt", [128, 128, D])
    ident_d = dt_in("ident", [128, 128])
    pm_d = dt_in("pmats", [12, 128, 128])
    iota_d = dt_in("iota16", [128, 16])
    iota128_d = dt_in("iota128", [128, 128])
    y_d = nc.dram_tensor("y", [TOK, D], F32, kind="ExternalOutput").ap()
    skind = "ExternalOutput" if debug else "Internal"
    h1_dram = nc.dram_tensor("h1_scr", [TOK, D], F32, kind=skind).ap()
    h2t_dram = nc.dram_tensor("h2t_scr", [128, 8, TOK], BF16, kind=skind).ap()
    g_dram = nc.dram_tensor("g_scr", [16, 128, TOK * 8], BF16, kind=skind).ap()

    with ExitStack() as stack:
        P = Prog(nc, stack)
        for _i in range(EXTRA_SEMS):
            P.new_sem("dummy%d" % _i)

        def sb(name, shape, dt=F32):
            t = stack.enter_context(nc.sbuf_tensor("sb_" + name, list(shape), dt))
            return t, Res(name)

        def ps(name, shape, dt=F32):
            t = stack.enter_context(nc.psum_tensor("ps_" + name, list(shape), dt))
            return t, Res(name)

        poolw_bf, R_poolw = sb("poolw_bf", [128, 4, 128], BF16)
        wst_bf, R_wst = sb("wst_bf", [128, 4, 128], BF16)
        ident_f, R_identf = sb("ident_f", [128, 128], F32)
        ident_bf, R_ident = sb("ident_bf", [128, 128], BF16)
        pm_bf, R_pm = sb("pm_bf", [128, 12, 128], BF16)
        g1col, R_g1col = sb("g1col", [128, 8])
        pscale, R_pscale = sb("pscale", [128, 4])
        iota16, R_iota = sb("iota16", [128, 16])
        iota128, R_iota128 = sb("iota128", [128, 128])
        g2_bc, R_g2 = sb("g2_bc", [128, D])
        fg_bc, R_fg = sb("fg_bc", [128, D])
        sgug_bc, R_sgug = sb("sgug_bc", [128, 512])
        b_bc, R_bbc = sb("b_bc", [128, 512])
        kt = [sb("kt%d" % i, [128, 128]) for i in range(2)]
        small, R_small = sb("small", [128, 32])
        st6, R_st6 = sb("st6", [128, 8])
        zsum, R_zsum = sb("zsum", [128, 16])

        arena = stack.enter_context(nc.sbuf_tensor("sb_arena", [128, ARENA_COLS], F32))
        bump = [0]

        def alloc(name, cols, dt=F32):
            f32cols = cols if dt in (F32, U32, I32) else (cols + 1) // 2
            o = bump[0]
            bump[0] += f32cols
            assert bump[0] <= ARENA_COLS, (name, bump[0])
            a = arena[:, o:o + f32cols]
            if dt != F32:
                a = a.bitcast(dt)
            return a, Res(name)

        w_in_bf, R_w_in = alloc("w_in_bf", 8 * D_IN, BF16)
        w_out_bf, R_w_out = alloc("w_out_bf", 8 * D, BF16)
        wk_bf, R_wk = alloc("wk_bf", 8 * 2048, BF16)
        w_in_v = w_in_bf.rearrange("p (k c) -> p k c", k=8)
        w_out_v = w_out_bf.rearrange("p (k c) -> p k c", k=8)
        wk_v = wk_bf.rearrange("p (k c) -> p k c", k=8)
        ring, _ = alloc("ring", NS * 1024)
        R_slot = [Res("slot%d" % i) for i in range(NS)]
        S_slot = [P.new_sem("s_slot%d" % i) for i in range(max(NS, NS2))]
        xbuf = [alloc("xbuf%d" % i, D) for i in range(2)]
        S_x = [P.new_sem("s_x%d" % i) for i in range(2)]
        xn_bf, R_xn = alloc("xn_bf", D, BF16)
        xnT, R_xnT = alloc("xnT", 8 * 128, BF16)
        xnT_v = xnT.rearrange("p (k t) -> p k t", k=8)
        zp_sb = [alloc("zp_sb%d" % i, 512, BF16) for i in range(2)]
        d_sb, R_d = alloc("d_sb", 512, BF16)
        ab_sb, R_ab = alloc("ab_sb", 8 * 128, BF16)
        ab_v = ab_sb.rearrange("p (k t) -> p k t", k=8)
        u_sb, R_u = alloc("u_sb", 512)
        v_f, R_v = alloc("v_f", 512)
        vn_bf, R_vn = alloc("vn_bf", 512, BF16)
        h1, R_h1 = alloc("h1", D)
        junk, R_junk = alloc("junk", D)
        scores_b = [alloc("scores%d" % i, 2048) for i in range(2)]
        top_s, R_tops = alloc("top_s", 256)
        top_i, R_topi = alloc("top_i", 256, U32)
        top_if, R_topif = alloc("top_if", 256)
        stmp, R_stmp = alloc("stmp", 256)
        best_s, R_best = alloc("best_s", 128)
        pos_u, R_pos = alloc("pos_u", 128, U32)
        hi_u, R_hiu = alloc("hi_u", 128, U32)
        lo_u, R_lou = alloc("lo_u", 128, U32)
        hif, R_hif = alloc("hif", 128)
        lof, R_lof = alloc("lof", 128)
        i1, R_i1 = alloc("i1", 128)
        i2, R_i2 = alloc("i2", 128)
        ework, R_ework = alloc("ework", 128)
        gate, R_gate = alloc("gate", 128)
        aT_sb, R_aT = alloc("aT_sb", 3 * 128)
        aT_v = aT_sb.rearrange("p (q t) -> p q t", q=3)
        Pbuf = [alloc("Pb%d" % i, TB * 128, BF16) for i in range(2)]
        Qbuf = [alloc("Qb%d" % i, TB * 128, BF16) for i in range(1)]
        Gst = [alloc("Gst%d" % i, 16 * TB * 8, BF16) for i in range(2)]
        S_gst = [P.new_sem("s_gst%d" % i) for i in range(2)]
        S_h1 = P.new_sem("s_h1")
        S_xnT = P.new_sem("s_xnT")
        S_c = {}

        def csem(name):
            S_c[name] = P.new_sem("s_" + name)
            return S_c[name]

        ptr, R_ptr = ps("ptr", [128, 8, 128], BF16)
        pAB, _ = ps("pAB", [128, 1024])
        pCD, _ = ps("pCD", [128, 1024])
        pGs, _ = ps("pGs", [128, 1024])
        pT, R_pT = ps("pT", [128, 512])
        pA, pB, pC, pD = pAB[:, 0:512], pAB[:, 512:1024], pCD[:, 0:512], pCD[:, 512:1024]
        R_pA, R_pB, R_pC, R_pD = [Res("pb%d" % i) for i in range(4)]
        pGsb = [pGs[:, 0:512], pGs[:, 512:1024]]
        R_pGs = [Res("pGs%d" % i) for i in range(2)]

        def slot(i, n=1024):
            return ring[:, i * 1024: i * 1024 + n]

        def load_const(dst, R, src, name):
            s = csem(name)
            P.emit("sp", lambda e: e.dma_start(out=dst, in_=src), writes=[R], dma_sem=s)

        load_const(ident_f[:], R_identf, ident_d, "ident")
        load_const(g1col[:], R_g1col, g1col_d, "g1col")
        load_const(pscale[:], R_pscale, pscale_d, "pscale")
        load_const(iota16[:], R_iota, iota_d, "iota")
        load_const(iota128[:], R_iota128, iota128_d, "iota128")
        load_const(g2_bc[:], R_g2, g2_d.partition_broadcast(128), "g2")
        load_const(fg_bc[:], R_fg, fg_d.partition_broadcast(128), "fg")
        load_const(sgug_bc[:], R_sgug, sgug_d.partition_broadcast(128), "sgug")
        load_const(b_bc[:], R_bbc, sgub_d.partition_broadcast(128), "bbc")
        P.emit("dve", lambda e: e.tensor_copy(out=ident_bf[:], in_=ident_f[:]), reads=[R_identf], writes=[R_ident])

        stage_ctr = [0]

        def stage_load(src_ap, ncols, view=None):
            i = stage_ctr[0] % NS
            stage_ctr[0] += 1
            dst = slot(i, ncols)
            if view is not None:
                dst = view(dst)
            P.emit("sp", lambda e: e.dma_start(out=dst, in_=src_ap), writes=[R_slot[i]], dma_sem=S_slot[i])
            return i

        cast_ctr = [0]

        def cast(dst, src, reads, writes, scale_ap=None):
            k = cast_ctr[0]
            cast_ctr[0] += 1
            if k % 2 == 0:
                if scale_ap is None:
                    P.emit("dve", lambda e: e.tensor_copy(out=dst, in_=src), reads=reads, writes=writes)
                else:
                    P.emit("dve", lambda e: e.tensor_scalar(out=dst, in0=src, scalar1=scale_ap, scalar2=None, op0=ALU.mult),
                           reads=reads, writes=writes)
            else:
                if scale_ap is None:
                    P.emit("act", lambda e: e.activation(out=dst, in_=src, func=AF.Copy), reads=reads, writes=writes)
                else:
                    P.emit("act", lambda e: e.activation(out=dst, in_=src, func=AF.Copy, scale=scale_ap), reads=reads, writes=writes)

        for half in range(2):
            i = stage_load(pm_d[half * 6:(half + 1) * 6].rearrange("m a b -> a m b"), 768,
                           view=lambda a: a.rearrange("p (m b) -> p m b", m=6))
            cast(pm_bf[:, half * 6:(half + 1) * 6, :], slot(i, 768).rearrange("p (m b) -> p m b", m=6),
                 [R_slot[i]], [R_pm])
        for kc in range(8):
            for (c0, c1) in ((0, 1024), (1024, 1536)):
                i = stage_load(w_in_d[kc * 128:(kc + 1) * 128, c0:c1], c1 - c0)
                cast(w_in_v[:, kc, c0:c1], slot(i, c1 - c0), [R_slot[i], R_g1col], [R_w_in], scale_ap=g1col[:, kc:kc + 1])
        for kc in range(8):
            i = stage_load(w_out_d[kc * 128:(kc + 1) * 128, :], 1024)
            cast(w_out_v[:, kc, :], slot(i), [R_slot[i]], [R_w_out])
        i = stage_load(pool_w_d.rearrange("g c d -> c g d"), 512, view=lambda a: a.rearrange("p (g d) -> p g d", g=4))
        cast(poolw_bf[:], slot(i, 512).rearrange("p (g d) -> p g d", g=4), [R_slot[i]], [R_poolw])
        i = stage_load(wst_d.rearrange("h j i -> j h i"), 512, view=lambda a: a.rearrange("p (h i) -> p h i", h=4))
        cast(wst_bf[:], slot(i, 512).rearrange("p (h i) -> p h i", h=4), [R_slot[i]], [R_wst])
        P.emit("dve", lambda e: e.memset(wst_bf[64:128, :, 0:64], 0.0), writes=[R_wst])
        for hp in range(16):
            i = stage_load(wqt_d[hp], 1024)
            ktile, R_kt = kt[hp % 2]
            if hp < 2:
                csem("kt%d" % hp)
            s_kt = S_c["kt%d" % (hp % 2)]
            P.emit("sp", lambda e, ktile=ktile, hp=hp: e.dma_start(out=ktile[:], in_=keyst_d[hp]), writes=[R_kt], dma_sem=s_kt)
            banks = ((pA, R_pA), (pB, R_pB)) if hp % 2 == 0 else ((pC, R_pC), (pD, R_pD))
            for b in range(2):
                pb_t, R_pb = banks[b]
                for q in range(4):
                    kc = b * 4 + q
                    P.emit("pe", lambda e, pb_t=pb_t, q=q, i=i, kc=kc, ktile=ktile: e.matmul(
                        pb_t[:, q * 128:(q + 1) * 128], lhsT=slot(i)[:, kc * 128:(kc + 1) * 128], rhs=ktile[:],
                        start=True, stop=True), reads=[R_slot[i], R_kt], writes=[R_pb], inc_sem=(q == 3))
                cast(wk_v[:, b * 4:(b + 1) * 4, hp * 128:(hp + 1) * 128],
                     pb_t.rearrange("p (q k) -> p q k", q=4), [R_pb], [R_wk])

        def load_x(n):
            xt, R_x = xbuf[n % 2]
            P.emit("sp", lambda e: e.dma_start(out=xt, in_=x_d[n * 128:(n + 1) * 128, :]), writes=[R_x], dma_sem=S_x[n % 2])

        def rms_stats(src, R_src, jk, R_jk, col):
            P.emit("act", lambda e: e.activation(out=jk, in_=src, func=AF.Square, accum_out=small[:, col:col + 1]),
                   reads=[R_src], writes=[R_jk, R_small])
            P.emit("act", lambda e: e.activation(out=small[:, col + 1:col + 2], in_=small[:, col:col + 1], func=AF.Sqrt,
                                                 scale=1.0 / D, bias=EPS), reads=[R_small], writes=[R_small])
            P.emit("dve", lambda e: e.reciprocal(out=small[:, col + 2:col + 3], in_=small[:, col + 1:col + 2]),
                   reads=[R_small], writes=[R_small])
            return small[:, col + 2:col + 3]

        def transposes(src_bf, R_src, dstT, R_dst):
            for kc in range(8):
                P.emit("pe", lambda e, kc=kc: e.transpose(ptr[:, kc, :], src_bf[:, kc * 128:(kc + 1) * 128], ident_bf[:]),
                       reads=[R_src, R_ident], writes=[R_ptr], inc_sem=(kc == 7))
            P.emit("act", lambda e: e.activation(out=dstT, in_=ptr[:], func=AF.Copy), reads=[R_ptr], writes=[R_dst])

        def stage_A(n):
            xt, R_x = xbuf[n % 2]
            first = (n % TILES_PER_SEQ == 0)
            zp_cur, R_zpc = zp_sb[n % 2]
            zp_prev, R_zpp = zp_sb[(n + 1) % 2]

            rstd1 = rms_stats(xt, R_x, xn_bf, R_xn, 0)
            P.emit("act", lambda e: e.activation(out=xn_bf, in_=xt, func=AF.Copy, scale=rstd1),
                   reads=[R_x, R_small], writes=[R_xn])
            transposes(xn_bf, R_xn, xnT_v, R_xnT)
            for kc in range(8):
                P.emit("pe", lambda e, kc=kc: e.matmul(pA, lhsT=xnT_v[:, kc, :], rhs=w_in_v[:, kc, 0:512],
                                                       start=(kc == 0), stop=(kc == 7)),
                       reads=[R_xnT, R_w_in], writes=[R_pA], inc_sem=(kc == 7))
            for kc in range(8):
                P.emit("pe", lambda e, kc=kc: e.matmul(pB, lhsT=xnT_v[:, kc, :], rhs=w_in_v[:, kc, 1024:1536],
                                                       start=(kc == 0), stop=(kc == 7)),
                       reads=[R_xnT, R_w_in], writes=[R_pB], inc_sem=(kc == 7))
            for m in range(4):
                for kc in range(8):
                    P.emit("pe", lambda e, kc=kc, m=m: e.matmul(pC[:, m * 128:(m + 1) * 128],
                                                                lhsT=w_in_v[:, kc, 512 + m * 128:512 + (m + 1) * 128],
                                                                rhs=xnT_v[:, kc, :], start=(kc == 0), stop=(kc == 7)),
                           reads=[R_xnT, R_w_in], writes=[R_pC], inc_sem=(kc == 7 and m == 3))
            P.emit("act", lambda e: e.activation(out=zp_cur, in_=pA, func=AF.Copy), reads=[R_pA], writes=[R_zpc])
            for g in range(4):
                mi = (8 + g) if first else g
                P.emit("pe", lambda e, g=g, mi=mi: e.matmul(
                    pD[:, g * 128:(g + 1) * 128], lhsT=zp_cur[:, g * 128:(g + 1) * 128], rhs=pm_bf[:, mi, :],
                    start=True, stop=first), reads=[R_zpc, R_pm], writes=[R_pD], inc_sem=(first and g == 3))
                if not first:
                    P.emit("pe", lambda e, g=g: e.matmul(
                        pD[:, g * 128:(g + 1) * 128], lhsT=zp_prev[:, g * 128:(g + 1) * 128], rhs=pm_bf[:, 4 + g, :],
                        start=False, stop=True), reads=[R_zpp, R_pm], writes=[R_pD], inc_sem=(g == 3))
            P.emit("dve", lambda e: e.tensor_copy(out=d_sb, in_=pD), reads=[R_pD], writes=[R_d])
            for g in range(4):
                P.emit("pe", lambda e, g=g: e.matmul(pA[:, g * 128:(g + 1) * 128], lhsT=poolw_bf[:, g, :],
                                                     rhs=d_sb[:, g * 128:(g + 1) * 128], start=True, stop=True),
                       reads=[R_poolw, R_d], writes=[R_pA], inc_sem=(g == 3))
            P.emit("dve", lambda e: e.tensor_tensor(out=ab_v[:, 0:4, :], in0=pA.rearrange("p (g t) -> p g t", g=4),
                                                    in1=bc_ap(pscale[:], [[1, 4], [0, 128]]), op=ALU.mult),
                   reads=[R_pA, R_pscale], writes=[R_ab])
            P.emit("act", lambda e: e.activation(out=u_sb, in_=pC, func=AF.Gelu_apprx_tanh), reads=[R_pC], writes=[R_u])
            P.emit("act", lambda e: e.activation(out=v_f, in_=pB, func=AF.Gelu_apprx_tanh), reads=[R_pB], writes=[R_v])
            P.emit("dve", lambda e: e.bn_stats(out=st6[:, 0:6], in_=v_f), reads=[R_v], writes=[R_st6])
            P.emit("dve", lambda e: e.bn_aggr(out=st6[:, 6:8], in_=st6[:, 0:6]), reads=[R_st6], writes=[R_st6])
            P.emit("act", lambda e: e.activation(out=small[:, 8:9], in_=st6[:, 7:8], func=AF.Sqrt, bias=EPS),
                   reads=[R_st6], writes=[R_small])
            P.emit("dve", lambda e: e.reciprocal(out=small[:, 9:10], in_=small[:, 8:9]), reads=[R_small], writes=[R_small])
            P.emit("dve", lambda e: e.tensor_scalar(out=v_f, in0=v_f, scalar1=st6[:, 6:7], scalar2=small[:, 9:10],
                                                    op0=ALU.subtract, op1=ALU.mult),
                   reads=[R_v, R_st6, R_small], writes=[R_v])
            P.emit("dve", lambda e: e.tensor_tensor(out=vn_bf, in0=v_f, in1=sgug_bc[:], op=ALU.mult),
                   reads=[R_v, R_sgug], writes=[R_vn])
            for h in range(4):
                P.emit("pe", lambda e, h=h: e.matmul(pB[:, h * 128:(h + 1) * 128], lhsT=vn_bf[:, h * 128:(h + 1) * 128],
                                                     rhs=wst_bf[:, h, :], start=True, stop=True),
                       reads=[R_vn, R_wst], writes=[R_pB], inc_sem=(h == 3))
            P.emit("dve", lambda e: e.tensor_tensor(out=v_f, in0=pB, in1=b_bc[:], op=ALU.add),
                   reads=[R_pB, R_bbc], writes=[R_v])
            P.emit("dve", lambda e: e.tensor_tensor(out=ab_v[:, 4:8, :], in0=v_f.rearrange("p (h t) -> p h t", h=4),
                                                    in1=u_sb.rearrange("p (h t) -> p h t", h=4), op=ALU.mult),
                   reads=[R_v, R_u], writes=[R_ab])
            for half, (pO_, R_pO_) in enumerate(((pC, R_pC), (pD, R_pD))):
                for fc in range(8):
                    P.emit("pe", lambda e, fc=fc, half=half, pO_=pO_: e.matmul(
                        pO_, lhsT=ab_v[:, fc, :], rhs=w_out_v[:, fc, half * 512:(half + 1) * 512],
                        start=(fc == 0), stop=(fc == 7)), reads=[R_ab, R_w_out], writes=[R_pO_], inc_sem=(fc == 7))
                P.emit("dve", lambda e, half=half, pO_=pO_: e.tensor_tensor(
                    out=h1[:, half * 512:(half + 1) * 512], in0=pO_, in1=xt[:, half * 512:(half + 1) * 512], op=ALU.add),
                    reads=[R_pO_, R_x], writes=[R_h1])
            P.emit("sp", lambda e: e.dma_start(out=h1_dram[n * 128:(n + 1) * 128, :], in_=h1), reads=[R_h1], dma_sem=S_h1)
            if n + 2 < NT:
                load_x(n + 2)
            rstd2 = rms_stats(h1, R_h1, junk, R_junk, 3)
            P.emit("dve", lambda e: e.scalar_tensor_tensor(out=xn_bf, in0=h1, scalar=rstd2, in1=g2_bc[:],
                                                           op0=ALU.mult, op1=ALU.mult),
                   reads=[R_h1, R_small, R_g2], writes=[R_xn])
            transposes(xn_bf, R_xn, xnT_v, R_xnT)
            P.emit("sp", lambda e: e.dma_start(out=h2t_dram[:, :, n * 128:(n + 1) * 128], in_=xnT_v), reads=[R_xnT], dma_sem=S_xnT)
            sbanks = ((pA, R_pA), (pB, R_pB), (pC, R_pC), (pD, R_pD))
            scores, R_sc = scores_b[n % 2]
            for c, (pb_t, R_pb) in enumerate(sbanks):
                for kc in range(8):
                    P.emit("pe", lambda e, kc=kc, c=c, pb_t=pb_t: e.matmul(
                        pb_t, lhsT=xnT_v[:, kc, :], rhs=wk_v[:, kc, c * 512:(c + 1) * 512],
                        start=(kc == 0), stop=(kc == 7)), reads=[R_xnT, R_wk], writes=[R_pb], inc_sem=(kc == 7))
                P.emit("act", lambda e, c=c, pb_t=pb_t: e.activation(out=scores[:, c * 512:(c + 1) * 512], in_=pb_t, func=AF.Copy),
                       reads=[R_pb], writes=[R_sc])

        gs_ctr = [0]

        def stage_B(n):
            scores, R_sc = scores_b[n % 2]
            for g in range(16):
                sg = scores[:, g * 128:(g + 1) * 128]
                o = g * 16
                P.emit("dve", lambda e, sg=sg, o=o: e.max(out=top_s[:, o:o + 8], in_=sg), reads=[R_sc], writes=[R_tops])
                P.emit("dve", lambda e, sg=sg, o=o: e.max_index(out=top_i[:, o:o + 8], in_max=top_s[:, o:o + 8], in_values=sg),
                       reads=[R_sc, R_tops], writes=[R_topi])
                P.emit("dve", lambda e, sg=sg, o=o: e.match_replace(out=stmp[:, 0:128], in_to_replace=top_s[:, o:o + 8],
                                                                    in_values=sg, imm_value=NEG),
                       reads=[R_sc, R_tops], writes=[R_stmp])
                P.emit("dve", lambda e, o=o: e.max(out=top_s[:, o + 8:o + 16], in_=stmp[:, 0:128]),
                       reads=[R_stmp], writes=[R_tops])
                P.emit("dve", lambda e, o=o: e.max_index(out=top_i[:, o + 8:o + 16], in_max=top_s[:, o + 8:o + 16],
                                                         in_values=stmp[:, 0:128]),
                       reads=[R_stmp, R_tops], writes=[R_topi])
            P.emit("dve", lambda e: e.tensor_copy(out=top_if, in_=top_i), reads=[R_topi], writes=[R_topif])
            cand = ring[:, 0:2048]
            R_cand = [R_slot[0], R_slot[1]]
            P.emit("dve", lambda e: e.tensor_tensor(
                out=cand.rearrange("p (h a b) -> p h a b", h=8, a=16),
                in0=bc_ap(top_s[:, 0:16], [[32, 8], [1, 16], [0, 16]]),
                in1=bc_ap(top_s[:, 16:32], [[32, 8], [0, 16], [1, 16]]), op=ALU.add),
                reads=[R_tops], writes=R_cand)
            for h in range(8):
                ch = cand[:, h * 256:(h + 1) * 256]
                o = h * 16
                P.emit("dve", lambda e, ch=ch, o=o: e.max(out=best_s[:, o:o + 8], in_=ch), reads=R_cand, writes=[R_best])
                P.emit("dve", lambda e, ch=ch, o=o: e.max_index(out=pos_u[:, o:o + 8], in_max=best_s[:, o:o + 8], in_values=ch),
                       reads=R_cand + [R_best], writes=[R_pos])
                P.emit("dve", lambda e, ch=ch, o=o: e.match_replace(out=stmp, in_to_replace=best_s[:, o:o + 8],
                                                                    in_values=ch, imm_value=NEG),
                       reads=R_cand + [R_best], writes=[R_stmp])
                P.emit("dve", lambda e, o=o: e.max(out=best_s[:, o + 8:o + 16], in_=stmp), reads=[R_stmp], writes=[R_best])
                P.emit("dve", lambda e, o=o: e.max_index(out=pos_u[:, o + 8:o + 16], in_max=best_s[:, o + 8:o + 16],
                                                         in_values=stmp),
                       reads=[R_stmp, R_best], writes=[R_pos])
            P.emit("dve", lambda e: e.tensor_single_scalar(out=hi_u, in_=pos_u, scalar=4, op=ALU.logical_shift_right),
                   reads=[R_pos], writes=[R_hiu])
            P.emit("dve", lambda e: e.tensor_single_scalar(out=lo_u, in_=pos_u, scalar=15, op=ALU.bitwise_and),
                   reads=[R_pos], writes=[R_lou])
            P.emit("dve", lambda e: e.tensor_copy(out=hif, in_=hi_u), reads=[R_hiu], writes=[R_hif])
            P.emit("dve", lambda e: e.tensor_copy(out=lof, in_=lo_u), reads=[R_lou], writes=[R_lof])
            wk = ring[:, 2048:4096]
            R_wkk = [R_slot[2], R_slot[3]]
            for (sel, off, dst, R_dst, R_sel) in ((hif, 0, i1, R_i1, R_hif), (lof, 16, i2, R_i2, R_lof)):
                P.emit("dve", lambda e, sel=sel: e.tensor_tensor(
                    out=wk.rearrange("p (k a) -> p k a", a=16),
                    in0=bc_ap(iota16[:], [[0, 128], [1, 16]]),
                    in1=bc_ap(sel, [[1, 128], [0, 16]]), op=ALU.is_equal),
                    reads=[R_iota, R_sel], writes=R_wkk)
                P.emit("dve", lambda e, off=off: e.tensor_tensor(
                    out=wk.rearrange("p (h k a) -> p h k a", h=8, k=16),
                    in0=wk.rearrange("p (h k a) -> p h k a", h=8, k=16),
                    in1=bc_ap(top_if[:, off:off + 16], [[32, 8], [0, 16], [1, 16]]), op=ALU.mult),
                    reads=R_wkk + [R_topif], writes=R_wkk)
                P.emit("dve", lambda e, dst=dst: e.tensor_reduce(
                    out=dst, in_=wk.rearrange("p (k a) -> p k a", a=16), axis=AX.X, op=ALU.add),
                    reads=R_wkk, writes=[R_dst])
            P.emit("dve", lambda e: e.tensor_tensor(out=ework.rearrange("p (h k) -> p h k", h=8),
                                                    in0=best_s.rearrange("p (h k) -> p h k", h=8),
                                                    in1=bc_ap(best_s, [[16, 8], [0, 16]]), op=ALU.subtract),
                   reads=[R_best], writes=[R_ework])
            P.emit("act", lambda e: e.activation(out=ework, in_=ework, func=AF.Exp), reads=[R_ework], writes=[R_ework])
            P.emit("dve", lambda e: e.tensor_reduce(out=zsum[:, 0:8], in_=ework.rearrange("p (h k) -> p h k", h=8),
                                                    axis=AX.X, op=ALU.add), reads=[R_ework], writes=[R_zsum])
            P.emit("dve", lambda e: e.reciprocal(out=zsum[:, 8:16], in_=zsum[:, 0:8]), reads=[R_zsum], writes=[R_zsum])
            P.emit("dve", lambda e: e.tensor_tensor(out=gate.rearrange("p (h k) -> p h k", h=8),
                                                    in0=ework.rearrange("p (h k) -> p h k", h=8),
                                                    in1=bc_ap(zsum[:, 8:16], [[1, 8], [0, 16]]), op=ALU.mult),
                   reads=[R_ework, R_zsum], writes=[R_gate])
            for q, (src, R_src) in enumerate(((i1, R_i1), (i2, R_i2), (gate, R_gate))):
                P.emit("pe", lambda e, q=q, src=src: e.transpose(pT[:, q * 128:(q + 1) * 128], src, ident_f[:]),
                       reads=[R_src, R_identf], writes=[R_pT], inc_sem=(q == 2))
            P.emit("act", lambda e: e.activation(out=aT_sb, in_=pT[:, 0:384], func=AF.Copy), reads=[R_pT], writes=[R_aT])
            for blk in range(128 // TB):
                t0 = blk * TB
                Pb, R_Pb = Pbuf[blk % 2]
                Qb, R_Qb = Qbuf[0]
                gst, R_gst = Gst[blk % 2]
                s_gst = S_gst[blk % 2]
                P.emit("dve", lambda e, Pb=Pb, t0=t0: e.tensor_tensor(
                    out=Pb.rearrange("p (t i) -> p t i", t=TB),
                    in0=bc_ap(iota128[:], [[0, TB], [1, 128]]),
                    in1=bc_ap(aT_v[:, 0, t0:t0 + TB], [[1, TB], [0, 128]]), op=ALU.is_equal),
                    reads=[R_iota128, R_aT], writes=[R_Pb])
                P.emit("dve", lambda e, Qb=Qb, t0=t0: e.tensor_tensor(
                    out=Qb.rearrange("p (t i) -> p t i", t=TB),
                    in0=bc_ap(iota128[:], [[0, TB], [1, 128]]),
                    in1=bc_ap(aT_v[:, 1, t0:t0 + TB], [[1, TB], [0, 128]]), op=ALU.is_equal),
                    reads=[R_iota128, R_aT], writes=[R_Qb])
                P.emit("pool", lambda e, Pb=Pb, t0=t0: e.tensor_tensor(
                    out=Pb.rearrange("p (t i) -> p t i", t=TB),
                    in0=Pb.rearrange("p (t i) -> p t i", t=TB),
                    in1=bc_ap(aT_v[:, 2, t0:t0 + TB], [[1, TB], [0, 128]]), op=ALU.mult),
                    reads=[R_Pb, R_aT], writes=[R_Pb])
                for t4 in range(TB // 4):
                    bi = gs_ctr[0] % 2
                    gs_ctr[0] += 1
                    bank, R_bank = pGsb[bi], R_pGs[bi]
                    for tk in range(4):
                        t = t4 * 4 + tk
                        P.emit("pe", lambda e, bank=bank, tk=tk, t=t, Pb=Pb, Qb=Qb: e.matmul(
                            bank[:, tk * 128:(tk + 1) * 128], lhsT=Pb[:, t * 128:(t + 1) * 128], rhs=Qb[:, t * 128:(t + 1) * 128],
                            start=True, stop=True), reads=[R_Pb, R_Qb], writes=[R_bank], inc_sem=(tk == 3))
                    P.emit("act", lambda e, bank=bank, gst=gst, t4=t4: e.activation(
                        out=bc_ap(gst[:, t4 * 32:t4 * 32 + 1], [[8, 4], [TB * 8, 16], [1, 8]]),
                        in_=bank.rearrange("p (tk jg jj) -> p tk jg jj", tk=4, jg=16), func=AF.Copy),
                        reads=[R_bank], writes=[R_gst])
                tok = n * 128 + t0
                P.emit("sp", lambda e, gst=gst, tok=tok: e.dma_start(
                    out=g_dram[:, :, tok * 8:(tok + TB) * 8].rearrange("g p x -> p g x"),
                    in_=gst.rearrange("p (g x) -> p g x", g=16)), reads=[R_gst], dma_sem=s_gst)

        load_x(0)
        if NT > 1:
            load_x(1)
        stage_A(0)
        for n in range(NT):
            if n + 1 < NT:
                stage_A(n + 1)
            stage_B(n)

        P.barrier()
        bump[0] = 0
        accum, _ = alloc("accum", NTH * D)
        accum_v = accum.rearrange("p (n d) -> p n d", n=NTH)
        R_acc = [Res("acc%d" % i) for i in range(NTH)]
        h2T, R_h2T = alloc("h2T", 8 * THALF, BF16)
        h2T_v = h2T.rearrange("p (k t) -> p k t", k=8)
        wU = [alloc("wU%d" % i, 1024, BF16) for i in range(NW)]
        wV = [alloc("wV%d" % i, 1024, BF16) for i in range(NW)]
        ring2, _ = alloc("ring2", NS2 * 1024)
        R_slot2 = [Res("slot2_%d" % i) for i in range(NS2)]
        gsl = [alloc("gsl%d" % i, TG * 8, BF16) for i in range(2)]
        S_gsl = [P.new_sem("s_gsl%d" % i) for i in range(2)]
        gsb = [alloc("gsb%d" % i, TG) for i in range(2)]
        mt = [alloc("mt%d" % i, TG, BF16) for i in range(3)]
        ystage = [alloc("ystage%d" % i, D) for i in range(2)]
        S_ys = [P.new_sem("s_ys%d" % i) for i in range(2)]
        junk2, R_junk2 = alloc("junk2", D)
        S_acc = [P.new_sem("s_acc%d" % i) for i in range(4)]
        S_h2T = [P.new_sem("s_h2T%d" % i) for i in range(4)]
        R_h2Tq = [Res("h2Tq%d" % i) for i in range(4)]
        pat = [pGs[:, 0:TG], pGs[:, 512:512 + TG], pT[:, 0:TG]]
        R_pat = [Res("pat%d" % i) for i in range(3)]
        pO = [(pAB, Res("pO0")), (pCD, Res("pO1"))]

        def slot2(i):
            return ring2[:, i * 1024:(i + 1) * 1024]

        st2_ctr = [0]
        ys_ctr = [0]

        et_slots = {}

        def stage_etile(j):
            sl = []
            for src in (ut_d[j], vt_d[j]):
                i = st2_ctr[0] % NS2
                st2_ctr[0] += 1
                P.emit("sp", lambda e, i=i, src=src: e.dma_start(out=slot2(i), in_=src), writes=[R_slot2[i]], dma_sem=S_slot[i])
                sl.append(i)
            et_slots[j] = sl

        def cast_etile(j):
            ws = j % NW
            for i, (dst, R_dst) in zip(et_slots.pop(j), (wU[ws], wV[ws])):
                P.emit("pool", lambda e, i=i, dst=dst: e.tensor_copy(out=dst, in_=slot2(i)), reads=[R_slot2[i]], writes=[R_dst])

        def load_gsl(h, jg, tg, idx):
            b = idx % 2
            tok = h * THALF + tg * TG
            g_t, R_g = gsl[b]
            P.emit("act", lambda e: e.dma_start(out=g_t, in_=g_dram[jg, :, tok * 8:(tok + TG) * 8]), writes=[R_g], dma_sem=S_gsl[b])

        for h in range(NHALF):
            tok0 = h * THALF
            na = max(1, NTH // 4)
            for q in range(0, NTH, na):
                P.emit("sp", lambda e, tok0=tok0, q=q: e.dma_start(
                    out=accum_v[:, q:q + na, :],
                    in_=h1_dram[tok0 + q * 128:tok0 + (q + na) * 128, :].rearrange("(n p) d -> p n d", p=128)),
                    writes=R_acc[q:q + na], dma_sem=S_acc[q // na])
            for q in range(4):
                P.emit("sp", lambda e, tok0=tok0, q=q: e.dma_start(out=h2T_v[:, 2 * q:2 * q + 2, :],
                                                                 in_=h2t_dram[:, 2 * q:2 * q + 2, tok0:tok0 + THALF]),
                       writes=[R_h2Tq[q]], dma_sem=S_h2T[q])
            for j in range(8):
                stage_etile(j)
                cast_etile(j)
            its = [(jg, tg, jj) for jg in range(JGMAX) for tg in range(NG) for jj in range(8)]
            NI = len(its)
            load_gsl(h, 0, 0, 0)
            for k in range(NI + 2):
                if k < NI:
                    jg, tg, jj = its[k]
                    gidx = jg * NG + tg
                    j = jg * 8 + jj
                    ws = j % NW
                    if jj == 0:
                        if gidx + 1 < JGMAX * NG:
                            jg2, tg2 = divmod(gidx + 1, NG)
                            load_gsl(h, jg2, tg2, gidx + 1)
                    if tg == NG - 1 and jg + 1 < JGMAX:
                        stage_etile((jg + 1) * 8 + jj)
                    pt_, R_pt = pat[k % 3], R_pat[k % 3]
                    wU_t, R_wU = wU[ws]
                    for c in range(8):
                        P.emit("pe", lambda e, c=c, pt_=pt_, wU_t=wU_t, tg=tg: e.matmul(
                            pt_, lhsT=wU_t[:, c * 128:(c + 1) * 128], rhs=h2T_v[:, c, tg * TG:(tg + 1) * TG],
                            start=(c == 0), stop=(c == 7)), reads=[R_wU, R_h2Tq[c // 2]], writes=[R_pt], inc_sem=(c == 7))
                    g_t, R_g = gsb[k % 2]
                    P.emit("act", lambda e, pt_=pt_, g_t=g_t: e.activation(out=g_t, in_=pt_, func=AF.Gelu_apprx_tanh),
                           reads=[R_pt], writes=[R_g])
                    m_t, R_m = mt[k % 3]
                    gs_t, R_gs = gsl[gidx % 2]
                    P.emit("pool", lambda e, m_t=m_t, g_t=g_t, gs_t=gs_t, jj=jj: e.tensor_tensor(
                        out=m_t, in0=g_t, in1=bc_ap(gs_t[:, jj:jj + 1], [[8, TG]]), op=ALU.mult),
                        reads=[R_g, R_gs], writes=[R_m])
                if k >= 2:
                    jg, tg, jj = its[k - 2]
                    j = jg * 8 + jj
                    ws = j % NW
                    m_t, R_m = mt[(k - 2) % 3]
                    wV_t, R_wV = wV[ws]
                    for ti in range(TG // 128):
                        pO_t, R_pO = pO[ti]
                        for hf in range(2):
                            P.emit("pe", lambda e, pO_t=pO_t, hf=hf, ti=ti, m_t=m_t, wV_t=wV_t, jj=jj: e.matmul(
                                pO_t[:, hf * 512:(hf + 1) * 512], lhsT=m_t[:, ti * 128:(ti + 1) * 128],
                                rhs=wV_t[:, hf * 512:(hf + 1) * 512], start=(jj == 0), stop=(jj == 7)),
                                reads=[R_m, R_wV], writes=[R_pO], inc_sem=(ti == TG // 128 - 1 and hf == 1))
                    if tg == NG - 1 and jg + 1 < JGMAX:
                        cast_etile((jg + 1) * 8 + jj)
                    if jj == 7:
                        for ti in range(TG // 128):
                            pO_t, R_pO = pO[ti]
                            a = tg * (TG // 128) + ti
                            P.emit("dve", lambda e, pO_t=pO_t, a=a: e.tensor_tensor(
                                out=accum_v[:, a, :], in0=pO_t[:], in1=accum_v[:, a, :], op=ALU.add),
                                reads=[R_pO, R_acc[a]], writes=[R_acc[a]])
            for a in range(NTH):
                n = h * NTH + a
                yb = ys_ctr[0] % 2
                ys_ctr[0] += 1
                y_t, R_y = ystage[yb]
                rstd3 = rms_stats(accum_v[:, a, :], R_acc[a], junk2, R_junk2, 6)
                P.emit("dve", lambda e, a=a, y_t=y_t, rstd3=rstd3: e.scalar_tensor_tensor(
                    out=y_t, in0=accum_v[:, a, :], scalar=rstd3, in1=fg_bc[:], op0=ALU.mult, op1=ALU.mult),
                    reads=[R_acc[a], R_small, R_fg], writes=[R_y])
                P.emit("sp", lambda e, n=n, y_t=y_t: e.dma_start(out=y_d[n * 128:(n + 1) * 128, :], in_=y_t),
                       reads=[R_y], dma_sem=S_ys[yb])

        P.final_wait("sp", S_ys)

        with nc.Block() as block:
            @block.sync
            def _(e):
                for f in P.ops["sp"]:
                    f(e)

            @block.tensor
            def _(e):
                for f in P.ops["pe"]:
                    f(e)

            @block.vector
            def _(e):
                for f in P.ops["dve"]:
                    f(e)

            @block.scalar
            def _(e):
                for f in P.ops["act"]:
                    f(e)

            @block.gpsimd
            def _(e):
                for f in P.ops["pool"]:
                    f(e)
    return nc


def _pool_mats():
    m = np.zeros((12, 128, 128), np.float32)
    tp = np.arange(128)[:, None]
    t = np.arange(128)[None, :]
    for g, w in enumerate(WINDOWS):
        band = ((tp <= t) & (tp > t - w)).astype(np.float32)
        m[g] = band / w - (tp == t)
        m[4 + g] = ((tp - 128) > (t - w)).astype(np.float32) / w
        cnt = np.minimum(t + 1, w).astype(np.float32)
        m[8 + g] = band / cnt - (tp == t)
    return m


def host_layout(inp):
    f = lambda a: np.ascontiguousarray(np.asarray(a, dtype=np.float32))
    wq = f(inp["peer_wq"])[0]
    keys = f(inp["peer_keys"])[0]
    pu = f(inp["peer_u"])[0]
    pv = f(inp["peer_v"])[0]
    return {
        "w_in": f(inp["w_in"])[0],
        "w_out": f(inp["w_out"])[0],
        "pool_w": f(inp["pool_w"])[0],
        "sgu_wt": f(np.transpose(f(inp["sgu_w"])[0], (0, 2, 1))),
        "sgu_b": f(inp["sgu_b"])[0].reshape(512),
        "sgu_g": f(inp["sgu_norm_g"])[0].reshape(512),
        "g1col": f(f(inp["norm1_g"])[0].reshape(8, 128).T),
        "pscale": f(f(inp["pool_scale"])[0].reshape(4, 128).T),
        "g2": f(inp["norm2_g"])[0].reshape(D),
        "fg": f(inp["final_g"]).reshape(D),
        "wq_t": f(wq.reshape(D, 16, 128).transpose(1, 2, 0)),
        "keys_t": f(keys.transpose(1, 0, 3, 2).reshape(16, 128, 128)),
        "peer_ut": f(pu.reshape(128, 128, 8, 128).transpose(1, 3, 2, 0)).reshape(128, 128, D),
        "peer_vt": f(pv.reshape(128, 128, D).transpose(1, 0, 2)),
        "ident": np.eye(128, dtype=np.float32),
        "pmats": _pool_mats(),
        "iota16": f(np.broadcast_to(np.arange(16, dtype=np.float32), (128, 16))),
        "iota128": f(np.broadcast_to(np.arange(128, dtype=np.float32), (128, 128))),
    }


_NC_CACHE = {}


def kernel(x, norm1_g, w_in, pool_w, pool_scale, sgu_norm_g, sgu_w, sgu_b, w_out, norm2_g,
           peer_wq, peer_keys, peer_u, peer_v, final_g):
    x = np.ascontiguousarray(np.asarray(x, dtype=np.float32))
    B = x.shape[0]
    xs = x.reshape(NCORES, (B // NCORES) * SEQ, D)
    shared = host_layout(dict(norm1_g=norm1_g, w_in=w_in, pool_w=pool_w, pool_scale=pool_scale, sgu_norm_g=sgu_norm_g,
                              sgu_w=sgu_w, sgu_b=sgu_b, w_out=w_out, norm2_g=norm2_g, peer_wq=peer_wq,
                              peer_keys=peer_keys, peer_u=peer_u, peer_v=peer_v, final_g=final_g))
    if "nc" not in _NC_CACHE:
        _NC_CACHE["nc"] = build_program()
    nc = _NC_CACHE["nc"]
    in_maps = [dict(shared, x=np.ascontiguousarray(xs[c])) for c in range(NCORES)]
    res = run_bass_kernel_spmd(nc, in_maps, core_ids=list(range(NCORES)))
    out = np.stack([res.results[c]["y"] for c in range(NCORES)], axis=0)
    return out.reshape(B, SEQ, D).astype(np.float32)
```

```python
from contextlib import ExitStack
import numpy as np
import concourse.bass as bass
import concourse.mybir as mybir
from concourse.bass_utils import run_bass_kernel_spmd

F32 = mybir.dt.float32
BF16 = mybir.dt.bfloat16
U32 = mybir.dt.uint32
I32 = mybir.dt.int32
ALU = mybir.AluOpType
AF = mybir.ActivationFunctionType
AX = mybir.AxisListType

D = 1024
SEQ = 2048
NCORES = 8
TOK_PER_CORE = 2 * SEQ
NT = TOK_PER_CORE // 128
TILES_PER_SEQ = SEQ // 128
D_IN = 1536
NEXP = 16384
EPS = 1e-6
WINDOWS = (2, 4, 8, 16)
NS = 4
NS2 = 6
TB = 32
TG = 256
NW = 8
NEG = -1.0e30
ARENA_COLS = 47400
SEM_LIMIT = 12000
JGMAX = 16
EXTRA_SEMS = 0


class Sem:
    def __init__(self, h):
        self.h = h
        self.n = 0


class Res:
    def __init__(self, name):
        self.name = name
        self.writers = {}
        self.readers = {}


class Prog:
    def __init__(self, nc, stack):
        self.nc = nc
        self.stack = stack
        self.all_sems = []
        self.ops = {k: [] for k in ("pe", "dve", "act", "pool", "sp")}
        self.esem = {k: self.new_sem("e_" + k) for k in self.ops}
        self.pe_sems = {self.esem["pe"]}
        self.epoch = {k: 0 for k in self.ops}
        self.pending = {k: False for k in self.ops}
        self.waited = {k: {} for k in self.ops}

    def new_sem(self, name):
        s = Sem(self.stack.enter_context(self.nc.semaphore(name)))
        self.all_sems.append(s)
        return s

    def emit(self, eng, fn, reads=(), writes=(), dma_sem=None, inc_sem=True):
        if dma_sem is None and self.esem[eng].n >= SEM_LIMIT and not self.pending[eng]:
            self.epoch[eng] += 1
            self.esem[eng] = self.new_sem("e_%s_%d" % (eng, self.epoch[eng]))
            if eng == "pe":
                self.pe_sems.add(self.esem[eng])
        mysem = dma_sem if dma_sem is not None else self.esem[eng]
        inc = 16 if dma_sem is not None else 1
        deps = {}
        for r in reads:
            for s, v in r.writers.items():
                deps[s] = max(deps.get(s, 0), v)
        for w in writes:
            for s, v in w.writers.items():
                deps[s] = max(deps.get(s, 0), v)
            for s, v in w.readers.items():
                deps[s] = max(deps.get(s, 0), v)
        waits = []
        for s, v in deps.items():
            if eng == "pe" and s in self.pe_sems and dma_sem is None:
                continue
            if dma_sem is not None and s is dma_sem:
                continue
            if self.waited[eng].get(s, 0) >= v:
                continue
            self.waited[eng][s] = v
            waits.append((s.h, v))
        if inc_sem:
            mysem.n += inc
            val = mysem.n
            if dma_sem is None:
                self.pending[eng] = False
        else:
            assert dma_sem is None
            val = mysem.n + inc
            self.pending[eng] = True
        for w in writes:
            w.writers = {mysem: val}
            w.readers = {}
        for r in reads:
            if r not in writes:
                r.readers[mysem] = val
        h = mysem.h

        def run(e, waits=waits, fn=fn, h=h, inc=inc, inc_sem=inc_sem):
            for (sh, v) in waits:
                e.wait_ge(sh, v)
            if inc_sem:
                fn(e).then_inc(h, inc)
            else:
                fn(e)

        self.ops[eng].append(run)

    def barrier(self):
        snap = [(s, s.n) for s in self.all_sems if s.n > 0]
        for eng in self.ops:
            lst = [(s.h, v) for (s, v) in snap if self.waited[eng].get(s, 0) < v]
            for (s, v) in snap:
                self.waited[eng][s] = v

            def run(e, lst=lst):
                for (sh, v) in lst:
                    e.wait_ge(sh, v)

            self.ops[eng].append(run)

    def final_wait(self, eng, sems):
        lst = [(s.h, s.n) for s in sems if s.n > 0]

        def run(e, lst=lst):
            for (sh, v) in lst:
                e.wait_ge(sh, v)

        self.ops[eng].append(run)


def bc_ap(ap, dims):
    return bass.AP(ap.tensor, ap.offset, [list(ap.ap[0])] + [list(d) for d in dims])


def build_program(NT=NT, NHALF=2, debug=False):
    TOK = NT * 128
    NTH = NT // NHALF
    THALF = NTH * 128
    NG = THALF // TG
    nc = bass.Bass("TRN2", target_bir_lowering=False)
    dt_in = lambda name, shape, dt=F32: nc.dram_tensor(name, list(shape), dt, kind="ExternalInput").ap()
    x_d = dt_in("x", [TOK, D])
    w_in_d = dt_in("w_in", [D, D_IN])
    w_out_d = dt_in("w_out", [D, D])
    pool_w_d = dt_in("pool_w", [4, 128, 128])
    wst_d = dt_in("sgu_wt", [4, 128, 128])
    sgub_d = dt_in("sgu_b", [512])
    sgug_d = dt_in("sgu_g", [512])
    g1col_d = dt_in("g1col", [128, 8])
    pscale_d = dt_in("pscale", [128, 4])
    g2_d = dt_in("g2", [D])
    fg_d = dt_in("fg", [D])
    wqt_d = dt_in("wq_t", [16, 128, D])
    keyst_d = dt_in("keys_t", [16, 128, 128])
    ut_d = dt_in("peer_ut", [128, 128, D])
    vt_d = dt_in("peer_vt", [128, 128, D])
    ident_d = dt_in("ident", [128, 128])
    pm_d = dt_in("pmats", [12, 128, 128])
    iota_d = dt_in("iota16", [128, 16])
    iota128_d = dt_in("iota128", [128, 128])
    y_d = nc.dram_tensor("y", [TOK, D], F32, kind="ExternalOutput").ap()
    skind = "ExternalOutput" if debug else "Internal"
    h1_dram = nc.dram_tensor("h1_scr", [TOK, D], F32, kind=skind).ap()
    h2t_dram = nc.dram_tensor("h2t_scr", [128, 8, TOK], BF16, kind=skind).ap()
    g_dram = nc.dram_tensor("g_scr", [16, 128, TOK * 8], BF16, kind=skind).ap()

    with ExitStack() as stack:
        P = Prog(nc, stack)
        for _i in range(EXTRA_SEMS):
            P.new_sem("dummy%d" % _i)

        def sb(name, shape, dt=F32):
            t = stack.enter_context(nc.sbuf_tensor("sb_" + name, list(shape), dt))
            return t, Res(name)

        def ps(name, shape, dt=F32):
            t = stack.enter_context(nc.psum_tensor("ps_" + name, list(shape), dt))
            return t, Res(name)

        poolw_bf, R_poolw = sb("poolw_bf", [128, 4, 128], BF16)
        wst_bf, R_wst = sb("wst_bf", [128, 4, 128], BF16)
        ident_f, R_identf = sb("ident_f", [128, 128], F32)
        ident_bf, R_ident = sb("ident_bf", [128, 128], BF16)
        pm_bf, R_pm = sb("pm_bf", [128, 12, 128], BF16)
        g1col, R_g1col = sb("g1col", [128, 8])
        pscale, R_pscale = sb("pscale", [128, 4])
        iota16, R_iota = sb("iota16", [128, 16])
        iota128, R_iota128 = sb("iota128", [128, 128])
        g2_bc, R_g2 = sb("g2_bc", [128, D])
        fg_bc, R_fg = sb("fg_bc", [128, D])
        sgug_bc, R_sgug = sb("sgug_bc", [128, 512])
        b_bc, R_bbc = sb("b_bc", [128, 512])
        kt = [sb("kt%d" % i, [128, 128]) for i in range(2)]
        small, R_small = sb("small", [128, 32])
        st6, R_st6 = sb("st6", [128, 8])
        zsum, R_zsum = sb("zsum", [128, 16])

        arena = stack.enter_context(nc.sbuf_tensor("sb_arena", [128, ARENA_COLS], F32))
        bump = [0]

        def alloc(name, cols, dt=F32):
            f32cols = cols if dt in (F32, U32, I32) else (cols + 1) // 2
            o = bump[0]
            bump[0] += f32cols
            assert bump[0] <= ARENA_COLS, (name, bump[0])
            a = arena[:, o:o + f32cols]
            if dt != F32:
                a = a.bitcast(dt)
            return a, Res(name)

        w_in_bf, R_w_in = alloc("w_in_bf", 8 * D_IN, BF16)
        w_out_bf, R_w_out = alloc("w_out_bf", 8 * D, BF16)
        wk_bf, R_wk = alloc("wk_bf", 8 * 2048, BF16)
        w_in_v = w_in_bf.rearrange("p (k c) -> p k c", k=8)
        w_out_v = w_out_bf.rearrange("p (k c) -> p k c", k=8)
        wk_v = wk_bf.rearrange("p (k c) -> p k c", k=8)
        ring, _ = alloc("ring", NS * 1024)
        R_slot = [Res("slot%d" % i) for i in range(NS)]
        S_slot = [P.new_sem("s_slot%d" % i) for i in range(max(NS, NS2))]
        xbuf = [alloc("xbuf%d" % i, D) for i in range(2)]
        S_x = [P.new_sem("s_x%d" % i) for i in range(2)]
        xn_bf, R_xn = alloc("xn_bf", D, BF16)
        xnT, R_xnT = alloc("xnT", 8 * 128, BF16)
        xnT_v = xnT.rearrange("p (k t) -> p k t", k=8)
        zp_sb = [alloc("zp_sb%d" % i, 512, BF16) for i in range(2)]
        d_sb, R_d = alloc("d_sb", 512, BF16)
        ab_sb, R_ab = alloc("ab_sb", 8 * 128, BF16)
        ab_v = ab_sb.rearrange("p (k t) -> p k t", k=8)
        u_sb, R_u = alloc("u_sb", 512)
        v_f, R_v = alloc("v_f", 512)
        vn_bf, R_vn = alloc("vn_bf", 512, BF16)
        h1, R_h1 = alloc("h1", D)
        junk, R_junk = alloc("junk", D)
        scores_b = [alloc("scores%d" % i, 2048) for i in range(2)]
        top_s, R_tops = alloc("top_s", 256)
        top_i, R_topi = alloc("top_i", 256, U32)
        top_if, R_topif = alloc("top_if", 256)
        stmp, R_stmp = alloc("stmp", 256)
        best_s, R_best = alloc("best_s", 128)
        pos_u, R_pos = alloc("pos_u", 128, U32)
        hi_u, R_hiu = alloc("hi_u", 128, U32)
        lo_u, R_lou = alloc("lo_u", 128, U32)
        hif, R_hif = alloc("hif", 128)
        lof, R_lof = alloc("lof", 128)
        i1, R_i1 = alloc("i1", 128)
        i2, R_i2 = alloc("i2", 128)
        ework, R_ework = alloc("ework", 128)
        gate, R_gate = alloc("gate", 128)
        aT_sb, R_aT = alloc("aT_sb", 3 * 128)
        aT_v = aT_sb.rearrange("p (q t) -> p q t", q=3)
        Pbuf = [alloc("Pb%d" % i, TB * 128, BF16) for i in range(2)]
        Qbuf = [alloc("Qb%d" % i, TB * 128, BF16) for i in range(1)]
        Gst = [alloc("Gst%d" % i, 16 * TB * 8, BF16) for i in range(2)]
        S_gst = [P.new_sem("s_gst%d" % i) for i in range(2)]
        S_h1 = P.new_sem("s_h1")
        S_xnT = P.new_sem("s_xnT")
        S_c = {}

        def csem(name):
            S_c[name] = P.new_sem("s_" + name)
            return S_c[name]

        ptr, R_ptr = ps("ptr", [128, 8, 128], BF16)
        pAB, _ = ps("pAB", [128, 1024])
        pCD, _ = ps("pCD", [128, 1024])
        pGs, _ = ps("pGs", [128, 1024])
        pT, R_pT = ps("pT", [128, 512])
        pA, pB, pC, pD = pAB[:, 0:512], pAB[:, 512:1024], pCD[:, 0:512], pCD[:, 512:1024]
        R_pA, R_pB, R_pC, R_pD = [Res("pb%d" % i) for i in range(4)]
        pGsb = [pGs[:, 0:512], pGs[:, 512:1024]]
        R_pGs = [Res("pGs%d" % i) for i in range(2)]

        def slot(i, n=1024):
            return ring[:, i * 1024: i * 1024 + n]

        def load_const(dst, R, src, name):
            s = csem(name)
            P.emit("sp", lambda e: e.dma_start(out=dst, in_=src), writes=[R], dma_sem=s)

        load_const(ident_f[:], R_identf, ident_d, "ident")
        load_const(g1col[:], R_g1col, g1col_d, "g1col")
        load_const(pscale[:], R_pscale, pscale_d, "pscale")
        load_const(iota16[:], R_iota, iota_d, "iota")
        load_const(iota128[:], R_iota128, iota128_d, "iota128")
        load_const(g2_bc[:], R_g2, g2_d.partition_broadcast(128), "g2")
        load_const(fg_bc[:], R_fg, fg_d.partition_broadcast(128), "fg")
        load_const(sgug_bc[:], R_sgug, sgug_d.partition_broadcast(128), "sgug")
        load_const(b_bc[:], R_bbc, sgub_d.partition_broadcast(128), "bbc")
        P.emit("dve", lambda e: e.tensor_copy(out=ident_bf[:], in_=ident_f[:]), reads=[R_identf], writes=[R_ident])

        stage_ctr = [0]

        def stage_load(src_ap, ncols, view=None):
            i = stage_ctr[0] % NS
            stage_ctr[0] += 1
            dst = slot(i, ncols)
            if view is not None:
                dst = view(dst)
            P.emit("sp", lambda e: e.dma_start(out=dst, in_=src_ap), writes=[R_slot[i]], dma_sem=S_slot[i])
            return i

        cast_ctr = [0]

        def cast(dst, src, reads, writes, scale_ap=None):
            k = cast_ctr[0]
            cast_ctr[0] += 1
            if k % 2 == 0:
                if scale_ap is None:
                    P.emit("dve", lambda e: e.tensor_copy(out=dst, in_=src), reads=reads, writes=writes)
                else:
                    P.emit("dve", lambda e: e.tensor_scalar(out=dst, in0=src, scalar1=scale_ap, scalar2=None, op0=ALU.mult),
                           reads=reads, writes=writes)
            else:
                if scale_ap is None:
                    P.emit("act", lambda e: e.activation(out=dst, in_=src, func=AF.Copy), reads=reads, writes=writes)
                else:
                    P.emit("act", lambda e: e.activation(out=dst, in_=src, func=AF.Copy, scale=scale_ap), reads=reads, writes=writes)

        for half in range(2):
            i = stage_load(pm_d[half * 6:(half + 1) * 6].rearrange("m a b -> a m b"), 768,
                           view=lambda a: a.rearrange("p (m b) -> p m b", m=6))
            cast(pm_bf[:, half * 6:(half + 1) * 6, :], slot(i, 768).rearrange("p (m b) -> p m b", m=6),
                 [R_slot[i]], [R_pm])
        for kc in range(8):
            for (c0, c1) in ((0, 1024), (1024, 1536)):
                i = stage_load(w_in_d[kc * 128:(kc + 1) * 128, c0:c1], c1 - c0)
                cast(w_in_v[:, kc, c0:c1], slot(i, c1 - c0), [R_slot[i], R_g1col], [R_w_in], scale_ap=g1col[:, kc:kc + 1])
        for kc in range(8):
            i = stage_load(w_out_d[kc * 128:(kc + 1) * 128, :], 1024)
            cast(w_out_v[:, kc, :], slot(i), [R_slot[i]], [R_w_out])
        i = stage_load(pool_w_d.rearrange("g c d -> c g d"), 512, view=lambda a: a.rearrange("p (g d) -> p g d", g=4))
        cast(poolw_bf[:], slot(i, 512).rearrange("p (g d) -> p g d", g=4), [R_slot[i]], [R_poolw])
        i = stage_load(wst_d.rearrange("h j i -> j h i"), 512, view=lambda a: a.rearrange("p (h i) -> p h i", h=4))
        cast(wst_bf[:], slot(i, 512).rearrange("p (h i) -> p h i", h=4), [R_slot[i]], [R_wst])
        P.emit("dve", lambda e: e.memset(wst_bf[64:128, :, 0:64], 0.0), writes=[R_wst])
        for hp in range(16):
            i = stage_load(wqt_d[hp], 1024)
            ktile, R_kt = kt[hp % 2]
            if hp < 2:
                csem("kt%d" % hp)
            s_kt = S_c["kt%d" % (hp % 2)]
            P.emit("sp", lambda e, ktile=ktile, hp=hp: e.dma_start(out=ktile[:], in_=keyst_d[hp]), writes=[R_kt], dma_sem=s_kt)
            banks = ((pA, R_pA), (pB, R_pB)) if hp % 2 == 0 else ((pC, R_pC), (pD, R_pD))
            for b in range(2):
                pb_t, R_pb = banks[b]
                for q in range(4):
                    kc = b * 4 + q
                    P.emit("pe", lambda e, pb_t=pb_t, q=q, i=i, kc=kc, ktile=ktile: e.matmul(
                        pb_t[:, q * 128:(q + 1) * 128], lhsT=slot(i)[:, kc * 128:(kc + 1) * 128], rhs=ktile[:],
                        start=True, stop=True), reads=[R_slot[i], R_kt], writes=[R_pb], inc_sem=(q == 3))
                cast(wk_v[:, b * 4:(b + 1) * 4, hp * 128:(hp + 1) * 128],
                     pb_t.rearrange("p (q k) -> p q k", q=4), [R_pb], [R_wk])

        def load_x(n):
            xt, R_x = xbuf[n % 2]
            P.emit("sp", lambda e: e.dma_start(out=xt, in_=x_d[n * 128:(n + 1) * 128, :]), writes=[R_x], dma_sem=S_x[n % 2])

        def rms_stats(src, R_src, jk, R_jk, col):
            P.emit("act", lambda e: e.activation(out=jk, in_=src, func=AF.Square, accum_out=small[:, col:col + 1]),
                   reads=[R_src], writes=[R_jk, R_small])
            P.emit("act", lambda e: e.activation(out=small[:, col + 1:col + 2], in_=small[:, col:col + 1], func=AF.Sqrt,
                                                 scale=1.0 / D, bias=EPS), reads=[R_small], writes=[R_small])
            P.emit("dve", lambda e: e.reciprocal(out=small[:, col + 2:col + 3], in_=small[:, col + 1:col + 2]),
                   reads=[R_small], writes=[R_small])
            return small[:, col + 2:col + 3]

        def transposes(src_bf, R_src, dstT, R_dst):
            for kc in range(8):
                P.emit("pe", lambda e, kc=kc: e.transpose(ptr[:, kc, :], src_bf[:, kc * 128:(kc + 1) * 128], ident_bf[:]),
                       reads=[R_src, R_ident], writes=[R_ptr], inc_sem=(kc == 7))
            P.emit("act", lambda e: e.activation(out=dstT, in_=ptr[:], func=AF.Copy), reads=[R_ptr], writes=[R_dst])

        def stage_A(n):
            xt, R_x = xbuf[n % 2]
            first = (n % TILES_PER_SEQ == 0)
            zp_cur, R_zpc = zp_sb[n % 2]
            zp_prev, R_zpp = zp_sb[(n + 1) % 2]

            rstd1 = rms_stats(xt, R_x, xn_bf, R_xn, 0)
            yield
            P.emit("act", lambda e: e.activation(out=xn_bf, in_=xt, func=AF.Copy, scale=rstd1),
                   reads=[R_x, R_small], writes=[R_xn])
            yield
            transposes(xn_bf, R_xn, xnT_v, R_xnT)
            for kc in range(8):
                P.emit("pe", lambda e, kc=kc: e.matmul(pA, lhsT=xnT_v[:, kc, :], rhs=w_in_v[:, kc, 0:512],
                                                       start=(kc == 0), stop=(kc == 7)),
                       reads=[R_xnT, R_w_in], writes=[R_pA], inc_sem=(kc == 7))
            for kc in range(8):
                P.emit("pe", lambda e, kc=kc: e.matmul(pB, lhsT=xnT_v[:, kc, :], rhs=w_in_v[:, kc, 1024:1536],
                                                       start=(kc == 0), stop=(kc == 7)),
                       reads=[R_xnT, R_w_in], writes=[R_pB], inc_sem=(kc == 7))
            for m in range(4):
                for kc in range(8):
                    P.emit("pe", lambda e, kc=kc, m=m: e.matmul(pC[:, m * 128:(m + 1) * 128],
                                                                lhsT=w_in_v[:, kc, 512 + m * 128:512 + (m + 1) * 128],
                                                                rhs=xnT_v[:, kc, :], start=(kc == 0), stop=(kc == 7)),
                           reads=[R_xnT, R_w_in], writes=[R_pC], inc_sem=(kc == 7 and m == 3))
            yield
            P.emit("act", lambda e: e.activation(out=zp_cur, in_=pA, func=AF.Copy), reads=[R_pA], writes=[R_zpc])
            for g in range(4):
                mi = (8 + g) if first else g
                P.emit("pe", lambda e, g=g, mi=mi: e.matmul(
                    pD[:, g * 128:(g + 1) * 128], lhsT=zp_cur[:, g * 128:(g + 1) * 128], rhs=pm_bf[:, mi, :],
                    start=True, stop=first), reads=[R_zpc, R_pm], writes=[R_pD], inc_sem=(first and g == 3))
                if not first:
                    P.emit("pe", lambda e, g=g: e.matmul(
                        pD[:, g * 128:(g + 1) * 128], lhsT=zp_prev[:, g * 128:(g + 1) * 128], rhs=pm_bf[:, 4 + g, :],
                        start=False, stop=True), reads=[R_zpp, R_pm], writes=[R_pD], inc_sem=(g == 3))
            yield
            P.emit("dve", lambda e: e.tensor_copy(out=d_sb, in_=pD), reads=[R_pD], writes=[R_d])
            for g in range(4):
                P.emit("pe", lambda e, g=g: e.matmul(pA[:, g * 128:(g + 1) * 128], lhsT=poolw_bf[:, g, :],
                                                     rhs=d_sb[:, g * 128:(g + 1) * 128], start=True, stop=True),
                       reads=[R_poolw, R_d], writes=[R_pA], inc_sem=(g == 3))
            P.emit("dve", lambda e: e.tensor_tensor(out=ab_v[:, 0:4, :], in0=pA.rearrange("p (g t) -> p g t", g=4),
                                                    in1=bc_ap(pscale[:], [[1, 4], [0, 128]]), op=ALU.mult),
                   reads=[R_pA, R_pscale], writes=[R_ab])
            yield
            P.emit("act", lambda e: e.activation(out=u_sb, in_=pC, func=AF.Gelu_apprx_tanh), reads=[R_pC], writes=[R_u])
            P.emit("act", lambda e: e.activation(out=v_f, in_=pB, func=AF.Gelu_apprx_tanh), reads=[R_pB], writes=[R_v])
            yield
            P.emit("dve", lambda e: e.bn_stats(out=st6[:, 0:6], in_=v_f), reads=[R_v], writes=[R_st6])
            P.emit("dve", lambda e: e.bn_aggr(out=st6[:, 6:8], in_=st6[:, 0:6]), reads=[R_st6], writes=[R_st6])
            P.emit("act", lambda e: e.activation(out=small[:, 8:9], in_=st6[:, 7:8], func=AF.Sqrt, bias=EPS),
                   reads=[R_st6], writes=[R_small])
            yield
            P.emit("dve", lambda e: e.reciprocal(out=small[:, 9:10], in_=small[:, 8:9]), reads=[R_small], writes=[R_small])
            P.emit("dve", lambda e: e.tensor_scalar(out=v_f, in0=v_f, scalar1=st6[:, 6:7], scalar2=small[:, 9:10],
                                                    op0=ALU.subtract, op1=ALU.mult),
                   reads=[R_v, R_st6, R_small], writes=[R_v])
            P.emit("dve", lambda e: e.tensor_tensor(out=vn_bf, in0=v_f, in1=sgug_bc[:], op=ALU.mult),
                   reads=[R_v, R_sgug], writes=[R_vn])
            yield
            for h in range(4):
                P.emit("pe", lambda e, h=h: e.matmul(pB[:, h * 128:(h + 1) * 128], lhsT=vn_bf[:, h * 128:(h + 1) * 128],
                                                     rhs=wst_bf[:, h, :], start=True, stop=True),
                       reads=[R_vn, R_wst], writes=[R_pB], inc_sem=(h == 3))
            yield
            P.emit("dve", lambda e: e.tensor_tensor(out=v_f, in0=pB, in1=b_bc[:], op=ALU.add),
                   reads=[R_pB, R_bbc], writes=[R_v])
            P.emit("dve", lambda e: e.tensor_tensor(out=ab_v[:, 4:8, :], in0=v_f.rearrange("p (h t) -> p h t", h=4),
                                                    in1=u_sb.rearrange("p (h t) -> p h t", h=4), op=ALU.mult),
                   reads=[R_v, R_u], writes=[R_ab])
            yield
            for half, (pO_, R_pO_) in enumerate(((pC, R_pC), (pD, R_pD))):
                for fc in range(8):
                    P.emit("pe", lambda e, fc=fc, half=half, pO_=pO_: e.matmul(
                        pO_, lhsT=ab_v[:, fc, :], rhs=w_out_v[:, fc, half * 512:(half + 1) * 512],
                        start=(fc == 0), stop=(fc == 7)), reads=[R_ab, R_w_out], writes=[R_pO_], inc_sem=(fc == 7))
                P.emit("dve", lambda e, half=half, pO_=pO_: e.tensor_tensor(
                    out=h1[:, half * 512:(half + 1) * 512], in0=pO_, in1=xt[:, half * 512:(half + 1) * 512], op=ALU.add),
                    reads=[R_pO_, R_x], writes=[R_h1])
            yield
            P.emit("sp", lambda e: e.dma_start(out=h1_dram[n * 128:(n + 1) * 128, :], in_=h1), reads=[R_h1], dma_sem=S_h1)
            if n + 2 < NT:
                load_x(n + 2)
            rstd2 = rms_stats(h1, R_h1, junk, R_junk, 3)
            yield
            P.emit("dve", lambda e: e.scalar_tensor_tensor(out=xn_bf, in0=h1, scalar=rstd2, in1=g2_bc[:],
                                                           op0=ALU.mult, op1=ALU.mult),
                   reads=[R_h1, R_small, R_g2], writes=[R_xn])
            transposes(xn_bf, R_xn, xnT_v, R_xnT)
            P.emit("sp", lambda e: e.dma_start(out=h2t_dram[:, :, n * 128:(n + 1) * 128], in_=xnT_v), reads=[R_xnT], dma_sem=S_xnT)
            yield
            sbanks = ((pA, R_pA), (pB, R_pB), (pC, R_pC), (pD, R_pD))
            scores, R_sc = scores_b[n % 2]
            for c, (pb_t, R_pb) in enumerate(sbanks):
                for kc in range(8):
                    P.emit("pe", lambda e, kc=kc, c=c, pb_t=pb_t: e.matmul(
                        pb_t, lhsT=xnT_v[:, kc, :], rhs=wk_v[:, kc, c * 512:(c + 1) * 512],
                        start=(kc == 0), stop=(kc == 7)), reads=[R_xnT, R_wk], writes=[R_pb], inc_sem=(kc == 7))
                P.emit("act", lambda e, c=c, pb_t=pb_t: e.activation(out=scores[:, c * 512:(c + 1) * 512], in_=pb_t, func=AF.Copy),
                       reads=[R_pb], writes=[R_sc])

        gs_ctr = [0]

        def stage_B(n):
            scores, R_sc = scores_b[n % 2]
            for g in range(16):
                sg = scores[:, g * 128:(g + 1) * 128]
                o = g * 16
                P.emit("dve", lambda e, sg=sg, o=o: e.max(out=top_s[:, o:o + 8], in_=sg), reads=[R_sc], writes=[R_tops])
                P.emit("dve", lambda e, sg=sg, o=o: e.max_index(out=top_i[:, o:o + 8], in_max=top_s[:, o:o + 8], in_values=sg),
                       reads=[R_sc, R_tops], writes=[R_topi])
                P.emit("dve", lambda e, sg=sg, o=o: e.match_replace(out=stmp[:, 0:128], in_to_replace=top_s[:, o:o + 8],
                                                                    in_values=sg, imm_value=NEG),
                       reads=[R_sc, R_tops], writes=[R_stmp])
                P.emit("dve", lambda e, o=o: e.max(out=top_s[:, o + 8:o + 16], in_=stmp[:, 0:128]),
                       reads=[R_stmp], writes=[R_tops])
                P.emit("dve", lambda e, o=o: e.max_index(out=top_i[:, o + 8:o + 16], in_max=top_s[:, o + 8:o + 16],
                                                         in_values=stmp[:, 0:128]),
                       reads=[R_stmp, R_tops], writes=[R_topi])
                if g % 2 == 1:
                    yield
            yield
            P.emit("dve", lambda e: e.tensor_copy(out=top_if, in_=top_i), reads=[R_topi], writes=[R_topif])
            cand = ring[:, 0:2048]
            R_cand = [R_slot[0], R_slot[1]]
            P.emit("dve", lambda e: e.tensor_tensor(
                out=cand.rearrange("p (h a b) -> p h a b", h=8, a=16),
                in0=bc_ap(top_s[:, 0:16], [[32, 8], [1, 16], [0, 16]]),
                in1=bc_ap(top_s[:, 16:32], [[32, 8], [0, 16], [1, 16]]), op=ALU.add),
                reads=[R_tops], writes=R_cand)
            for h in range(8):
                ch = cand[:, h * 256:(h + 1) * 256]
                o = h * 16
                P.emit("dve", lambda e, ch=ch, o=o: e.max(out=best_s[:, o:o + 8], in_=ch), reads=R_cand, writes=[R_best])
                P.emit("dve", lambda e, ch=ch, o=o: e.max_index(out=pos_u[:, o:o + 8], in_max=best_s[:, o:o + 8], in_values=ch),
                       reads=R_cand + [R_best], writes=[R_pos])
                P.emit("dve", lambda e, ch=ch, o=o: e.match_replace(out=stmp, in_to_replace=best_s[:, o:o + 8],
                                                                    in_values=ch, imm_value=NEG),
                       reads=R_cand + [R_best], writes=[R_stmp])
                P.emit("dve", lambda e, o=o: e.max(out=best_s[:, o + 8:o + 16], in_=stmp), reads=[R_stmp], writes=[R_best])
                P.emit("dve", lambda e, o=o: e.max_index(out=pos_u[:, o + 8:o + 16], in_max=best_s[:, o + 8:o + 16],
                                                         in_values=stmp),
                       reads=[R_stmp, R_best], writes=[R_pos])
                if h % 2 == 1:
                    yield
            yield
            P.emit("dve", lambda e: e.tensor_single_scalar(out=hi_u, in_=pos_u, scalar=4, op=ALU.logical_shift_right),
                   reads=[R_pos], writes=[R_hiu])
            P.emit("dve", lambda e: e.tensor_single_scalar(out=lo_u, in_=pos_u, scalar=15, op=ALU.bitwise_and),
                   reads=[R_pos], writes=[R_lou])
            P.emit("dve", lambda e: e.tensor_copy(out=hif, in_=hi_u), reads=[R_hiu], writes=[R_hif])
            P.emit("dve", lambda e: e.tensor_copy(out=lof, in_=lo_u), reads=[R_lou], writes=[R_lof])
            wk = ring[:, 2048:4096]
            R_wkk = [R_slot[2], R_slot[3]]
            for (sel, off, dst, R_dst, R_sel) in ((hif, 0, i1, R_i1, R_hif), (lof, 16, i2, R_i2, R_lof)):
                P.emit("dve", lambda e, sel=sel: e.tensor_tensor(
                    out=wk.rearrange("p (k a) -> p k a", a=16),
                    in0=bc_ap(iota16[:], [[0, 128], [1, 16]]),
                    in1=bc_ap(sel, [[1, 128], [0, 16]]), op=ALU.is_equal),
                    reads=[R_iota, R_sel], writes=R_wkk)
                P.emit("dve", lambda e, off=off: e.tensor_tensor(
                    out=wk.rearrange("p (h k a) -> p h k a", h=8, k=16),
                    in0=wk.rearrange("p (h k a) -> p h k a", h=8, k=16),
                    in1=bc_ap(top_if[:, off:off + 16], [[32, 8], [0, 16], [1, 16]]), op=ALU.mult),
                    reads=R_wkk + [R_topif], writes=R_wkk)
                P.emit("dve", lambda e, dst=dst: e.tensor_reduce(
                    out=dst, in_=wk.rearrange("p (k a) -> p k a", a=16), axis=AX.X, op=ALU.add),
                    reads=R_wkk, writes=[R_dst])
            yield
            P.emit("dve", lambda e: e.tensor_tensor(out=ework.rearrange("p (h k) -> p h k", h=8),
                                                    in0=best_s.rearrange("p (h k) -> p h k", h=8),
                                                    in1=bc_ap(best_s, [[16, 8], [0, 16]]), op=ALU.subtract),
                   reads=[R_best], writes=[R_ework])
            P.emit("act", lambda e: e.activation(out=ework, in_=ework, func=AF.Exp), reads=[R_ework], writes=[R_ework])
            yield
            P.emit("dve", lambda e: e.tensor_reduce(out=zsum[:, 0:8], in_=ework.rearrange("p (h k) -> p h k", h=8),
                                                    axis=AX.X, op=ALU.add), reads=[R_ework], writes=[R_zsum])
            P.emit("dve", lambda e: e.reciprocal(out=zsum[:, 8:16], in_=zsum[:, 0:8]), reads=[R_zsum], writes=[R_zsum])
            P.emit("dve", lambda e: e.tensor_tensor(out=gate.rearrange("p (h k) -> p h k", h=8),
                                                    in0=ework.rearrange("p (h k) -> p h k", h=8),
                                                    in1=bc_ap(zsum[:, 8:16], [[1, 8], [0, 16]]), op=ALU.mult),
                   reads=[R_ework, R_zsum], writes=[R_gate])
            yield
            for q, (src, R_src) in enumerate(((i1, R_i1), (i2, R_i2), (gate, R_gate))):
                P.emit("pe", lambda e, q=q, src=src: e.transpose(pT[:, q * 128:(q + 1) * 128], src, ident_f[:]),
                       reads=[R_src, R_identf], writes=[R_pT], inc_sem=(q == 2))
            P.emit("act", lambda e: e.activation(out=aT_sb, in_=pT[:, 0:384], func=AF.Copy), reads=[R_pT], writes=[R_aT])
            yield
            for blk in range(128 // TB):
                t0 = blk * TB
                Pb, R_Pb = Pbuf[blk % 2]
                Qb, R_Qb = Qbuf[0]
                gst, R_gst = Gst[blk % 2]
                s_gst = S_gst[blk % 2]
                P.emit("dve", lambda e, Pb=Pb, t0=t0: e.tensor_tensor(
                    out=Pb.rearrange("p (t i) -> p t i", t=TB),
                    in0=bc_ap(iota128[:], [[0, TB], [1, 128]]),
                    in1=bc_ap(aT_v[:, 0, t0:t0 + TB], [[1, TB], [0, 128]]), op=ALU.is_equal),
                    reads=[R_iota128, R_aT], writes=[R_Pb])
                P.emit("dve", lambda e, Qb=Qb, t0=t0: e.tensor_tensor(
                    out=Qb.rearrange("p (t i) -> p t i", t=TB),
                    in0=bc_ap(iota128[:], [[0, TB], [1, 128]]),
                    in1=bc_ap(aT_v[:, 1, t0:t0 + TB], [[1, TB], [0, 128]]), op=ALU.is_equal),
                    reads=[R_iota128, R_aT], writes=[R_Qb])
                P.emit("pool", lambda e, Pb=Pb, t0=t0: e.tensor_tensor(
                    out=Pb.rearrange("p (t i) -> p t i", t=TB),
                    in0=Pb.rearrange("p (t i) -> p t i", t=TB),
                    in1=bc_ap(aT_v[:, 2, t0:t0 + TB], [[1, TB], [0, 128]]), op=ALU.mult),
                    reads=[R_Pb, R_aT], writes=[R_Pb])
                for t4 in range(TB // 4):
                    bi = gs_ctr[0] % 2
                    gs_ctr[0] += 1
                    bank, R_bank = pGsb[bi], R_pGs[bi]
                    for tk in range(4):
                        t = t4 * 4 + tk
                        P.emit("pe", lambda e, bank=bank, tk=tk, t=t, Pb=Pb, Qb=Qb: e.matmul(
                            bank[:, tk * 128:(tk + 1) * 128], lhsT=Pb[:, t * 128:(t + 1) * 128], rhs=Qb[:, t * 128:(t + 1) * 128],
                            start=True, stop=True), reads=[R_Pb, R_Qb], writes=[R_bank], inc_sem=(tk == 3))
                    P.emit("act", lambda e, bank=bank, gst=gst, t4=t4: e.activation(
                        out=bc_ap(gst[:, t4 * 32:t4 * 32 + 1], [[8, 4], [TB * 8, 16], [1, 8]]),
                        in_=bank.rearrange("p (tk jg jj) -> p tk jg jj", tk=4, jg=16), func=AF.Copy),
                        reads=[R_bank], writes=[R_gst])
                tok = n * 128 + t0
                P.emit("sp", lambda e, gst=gst, tok=tok: e.dma_start(
                    out=g_dram[:, :, tok * 8:(tok + TB) * 8].rearrange("g p x -> p g x"),
                    in_=gst.rearrange("p (g x) -> p g x", g=16)), reads=[R_gst], dma_sem=s_gst)
                yield

        load_x(0)
        if NT > 1:
            load_x(1)
        for _ in stage_A(0):
            pass
        for n in range(NT):
            gb = stage_B(n)
            ga = stage_A(n + 1) if n + 1 < NT else iter(())
            done_a = done_b = False
            while not (done_a and done_b):
                if not done_b:
                    try:
                        next(gb)
                    except StopIteration:
                        done_b = True
                if not done_a:
                    try:
                        next(ga)
                    except StopIteration:
                        done_a = True

        P.barrier()
        bump[0] = 0
        accum, _ = alloc("accum", NTH * D)
        accum_v = accum.rearrange("p (n d) -> p n d", n=NTH)
        R_acc = [Res("acc%d" % i) for i in range(NTH)]
        h2T, R_h2T = alloc("h2T", 8 * THALF, BF16)
        h2T_v = h2T.rearrange("p (k t) -> p k t", k=8)
        wU = [alloc("wU%d" % i, 1024, BF16) for i in range(NW)]
        wV = [alloc("wV%d" % i, 1024, BF16) for i in range(NW)]
        ring2, _ = alloc("ring2", NS2 * 1024)
        R_slot2 = [Res("slot2_%d" % i) for i in range(NS2)]
        gsl = [alloc("gsl%d" % i, TG * 8, BF16) for i in range(2)]
        S_gsl = [P.new_sem("s_gsl%d" % i) for i in range(2)]
        gsb = [alloc("gsb%d" % i, TG) for i in range(2)]
        mt = [alloc("mt%d" % i, TG, BF16) for i in range(3)]
        ystage = [alloc("ystage%d" % i, D) for i in range(2)]
        S_ys = [P.new_sem("s_ys%d" % i) for i in range(2)]
        junk2, R_junk2 = alloc("junk2", D)
        S_acc = [P.new_sem("s_acc%d" % i) for i in range(4)]
        S_h2T = [P.new_sem("s_h2T%d" % i) for i in range(4)]
        R_h2Tq = [Res("h2Tq%d" % i) for i in range(4)]
        pat = [pGs[:, 0:TG], pGs[:, 512:512 + TG], pT[:, 0:TG]]
        R_pat = [Res("pat%d" % i) for i in range(3)]
        pO = [(pAB, Res("pO0")), (pCD, Res("pO1"))]

        def slot2(i):
            return ring2[:, i * 1024:(i + 1) * 1024]

        st2_ctr = [0]
        ys_ctr = [0]

        et_slots = {}

        def stage_etile(j):
            sl = []
            for src in (ut_d[j], vt_d[j]):
                i = st2_ctr[0] % NS2
                st2_ctr[0] += 1
                P.emit("sp", lambda e, i=i, src=src: e.dma_start(out=slot2(i), in_=src), writes=[R_slot2[i]], dma_sem=S_slot[i])
                sl.append(i)
            et_slots[j] = sl

        def cast_etile(j):
            ws = j % NW
            for i, (dst, R_dst) in zip(et_slots.pop(j), (wU[ws], wV[ws])):
                P.emit("pool", lambda e, i=i, dst=dst: e.tensor_copy(out=dst, in_=slot2(i)), reads=[R_slot2[i]], writes=[R_dst])

        def load_gsl(h, jg, tg, idx):
            b = idx % 2
            tok = h * THALF + tg * TG
            g_t, R_g = gsl[b]
            P.emit("act", lambda e: e.dma_start(out=g_t, in_=g_dram[jg, :, tok * 8:(tok + TG) * 8]), writes=[R_g], dma_sem=S_gsl[b])

        for h in range(NHALF):
            tok0 = h * THALF
            na = max(1, NTH // 4)
            for q in range(0, NTH, na):
                P.emit("sp", lambda e, tok0=tok0, q=q: e.dma_start(
                    out=accum_v[:, q:q + na, :],
                    in_=h1_dram[tok0 + q * 128:tok0 + (q + na) * 128, :].rearrange("(n p) d -> p n d", p=128)),
                    writes=R_acc[q:q + na], dma_sem=S_acc[q // na])
            for q in range(4):
                P.emit("sp", lambda e, tok0=tok0, q=q: e.dma_start(out=h2T_v[:, 2 * q:2 * q + 2, :],
                                                                 in_=h2t_dram[:, 2 * q:2 * q + 2, tok0:tok0 + THALF]),
                       writes=[R_h2Tq[q]], dma_sem=S_h2T[q])
            for j in range(8):
                stage_etile(j)
                cast_etile(j)
            its = [(jg, tg, jj) for jg in range(JGMAX) for tg in range(NG) for jj in range(8)]
            NI = len(its)
            load_gsl(h, 0, 0, 0)
            for k in range(NI + 2):
                if k < NI:
                    jg, tg, jj = its[k]
                    gidx = jg * NG + tg
                    j = jg * 8 + jj
                    ws = j % NW
                    if jj == 0:
                        if gidx + 1 < JGMAX * NG:
                            jg2, tg2 = divmod(gidx + 1, NG)
                            load_gsl(h, jg2, tg2, gidx + 1)
                    if tg == NG - 1 and jg + 1 < JGMAX:
                        stage_etile((jg + 1) * 8 + jj)
                    pt_, R_pt = pat[k % 3], R_pat[k % 3]
                    wU_t, R_wU = wU[ws]
                    for c in range(8):
                        P.emit("pe", lambda e, c=c, pt_=pt_, wU_t=wU_t, tg=tg: e.matmul(
                            pt_, lhsT=wU_t[:, c * 128:(c + 1) * 128], rhs=h2T_v[:, c, tg * TG:(tg + 1) * TG],
                            start=(c == 0), stop=(c == 7)), reads=[R_wU, R_h2Tq[c // 2]], writes=[R_pt], inc_sem=(c == 7))
                    g_t, R_g = gsb[k % 2]
                    P.emit("act", lambda e, pt_=pt_, g_t=g_t: e.activation(out=g_t, in_=pt_, func=AF.Gelu_apprx_tanh),
                           reads=[R_pt], writes=[R_g])
                    m_t, R_m = mt[k % 3]
                    gs_t, R_gs = gsl[gidx % 2]
                    P.emit("dve", lambda e, m_t=m_t, g_t=g_t, gs_t=gs_t, jj=jj: e.tensor_tensor(
                        out=m_t, in0=g_t, in1=bc_ap(gs_t[:, jj:jj + 1], [[8, TG]]), op=ALU.mult),
                        reads=[R_g, R_gs], writes=[R_m])
                if k >= 2:
                    jg, tg, jj = its[k - 2]
                    j = jg * 8 + jj
                    ws = j % NW
                    m_t, R_m = mt[(k - 2) % 3]
                    wV_t, R_wV = wV[ws]
                    for ti in range(TG // 128):
                        pO_t, R_pO = pO[ti]
                        for hf in range(2):
                            P.emit("pe", lambda e, pO_t=pO_t, hf=hf, ti=ti, m_t=m_t, wV_t=wV_t, jj=jj: e.matmul(
                                pO_t[:, hf * 512:(hf + 1) * 512], lhsT=m_t[:, ti * 128:(ti + 1) * 128],
                                rhs=wV_t[:, hf * 512:(hf + 1) * 512], start=(jj == 0), stop=(jj == 7)),
                                reads=[R_m, R_wV], writes=[R_pO], inc_sem=(ti == TG // 128 - 1 and hf == 1))
                    if tg == NG - 1 and jg + 1 < JGMAX:
                        cast_etile((jg + 1) * 8 + jj)
                    if jj == 7:
                        for ti in range(TG // 128):
                            pO_t, R_pO = pO[ti]
                            a = tg * (TG // 128) + ti
                            P.emit("dve", lambda e, pO_t=pO_t, a=a: e.tensor_tensor(
                                out=accum_v[:, a, :], in0=pO_t[:], in1=accum_v[:, a, :], op=ALU.add),
                                reads=[R_pO, R_acc[a]], writes=[R_acc[a]])
            for a in range(NTH):
                n = h * NTH + a
                yb = ys_ctr[0] % 2
                ys_ctr[0] += 1
                y_t, R_y = ystage[yb]
                rstd3 = rms_stats(accum_v[:, a, :], R_acc[a], junk2, R_junk2, 6)
                P.emit("dve", lambda e, a=a, y_t=y_t, rstd3=rstd3: e.scalar_tensor_tensor(
                    out=y_t, in0=accum_v[:, a, :], scalar=rstd3, in1=fg_bc[:], op0=ALU.mult, op1=ALU.mult),
                    reads=[R_acc[a], R_small, R_fg], writes=[R_y])
                P.emit("sp", lambda e, n=n, y_t=y_t: e.dma_start(out=y_d[n * 128:(n + 1) * 128, :], in_=y_t),
                       reads=[R_y], dma_sem=S_ys[yb])

        P.final_wait("sp", S_ys)

        with nc.Block() as block:
            @block.sync
            def _(e):
                for f in P.ops["sp"]:
                    f(e)

            @block.tensor
            def _(e):
                for f in P.ops["pe"]:
                    f(e)

            @block.vector
            def _(e):
                for f in P.ops["dve"]:
                    f(e)

            @block.scalar
            def _(e):
                for f in P.ops["act"]:
                    f(e)

            @block.gpsimd
            def _(e):
                for f in P.ops["pool"]:
                    f(e)
    return nc


def _pool_mats():
    m = np.zeros((12, 128, 128), np.float32)
    tp = np.arange(128)[:, None]
    t = np.arange(128)[None, :]
    for g, w in enumerate(WINDOWS):
        band = ((tp <= t) & (tp > t - w)).astype(np.float32)
        m[g] = band / w - (tp == t)
        m[4 + g] = ((tp - 128) > (t - w)).astype(np.float32) / w
        cnt = np.minimum(t + 1, w).astype(np.float32)
        m[8 + g] = band / cnt - (tp == t)
    return m


def host_layout(inp):
    f = lambda a: np.ascontiguousarray(np.asarray(a, dtype=np.float32))
    wq = f(inp["peer_wq"])[0]
    keys = f(inp["peer_keys"])[0]
    pu = f(inp["peer_u"])[0]
    pv = f(inp["peer_v"])[0]
    return {
        "w_in": f(inp["w_in"])[0],
        "w_out": f(inp["w_out"])[0],
        "pool_w": f(inp["pool_w"])[0],
        "sgu_wt": f(np.transpose(f(inp["sgu_w"])[0], (0, 2, 1))),
        "sgu_b": f(inp["sgu_b"])[0].reshape(512),
        "sgu_g": f(inp["sgu_norm_g"])[0].reshape(512),
        "g1col": f(f(inp["norm1_g"])[0].reshape(8, 128).T),
        "pscale": f(f(inp["pool_scale"])[0].reshape(4, 128).T),
        "g2": f(inp["norm2_g"])[0].reshape(D),
        "fg": f(inp["final_g"]).reshape(D),
        "wq_t": f(wq.reshape(D, 16, 128).transpose(1, 2, 0)),
        "keys_t": f(keys.transpose(1, 0, 3, 2).reshape(16, 128, 128)),
        "peer_ut": f(pu.reshape(128, 128, 8, 128).transpose(1, 3, 2, 0)).reshape(128, 128, D),
        "peer_vt": f(pv.reshape(128, 128, D).transpose(1, 0, 2)),
        "ident": np.eye(128, dtype=np.float32),
        "pmats": _pool_mats(),
        "iota16": f(np.broadcast_to(np.arange(16, dtype=np.float32), (128, 16))),
        "iota128": f(np.broadcast_to(np.arange(128, dtype=np.float32), (128, 128))),
    }


_NC_CACHE = {}


def kernel(x, norm1_g, w_in, pool_w, pool_scale, sgu_norm_g, sgu_w, sgu_b, w_out, norm2_g,
           peer_wq, peer_keys, peer_u, peer_v, final_g):
    x = np.ascontiguousarray(np.asarray(x, dtype=np.float32))
    B = x.shape[0]
    xs = x.reshape(NCORES, (B // NCORES) * SEQ, D)
    shared = host_layout(dict(norm1_g=norm1_g, w_in=w_in, pool_w=pool_w, pool_scale=pool_scale, sgu_norm_g=sgu_norm_g,
                              sgu_w=sgu_w, sgu_b=sgu_b, w_out=w_out, norm2_g=norm2_g, peer_wq=peer_wq,
                              peer_keys=peer_keys, peer_u=peer_u, peer_v=peer_v, final_g=final_g))
    if "nc" not in _NC_CACHE:
        _NC_CACHE["nc"] = build_program()
    nc = _NC_CACHE["nc"]
    in_maps = [dict(shared, x=np.ascontiguousarray(xs[c])) for c in range(NCORES)]
    res = run_bass_kernel_spmd(nc, in_maps, core_ids=list(range(NCORES)))
    out = np.stack([res.results[c]["y"] for c in range(NCORES)], axis=0)
    return out.reshape(B, SEQ, D).astype(np.float32)
```

```python
from contextlib import ExitStack
import numpy as np
import concourse.bass as bass
import concourse.mybir as mybir
from concourse.bass_utils import run_bass_kernel_spmd

F32 = mybir.dt.float32
BF16 = mybir.dt.bfloat16
U32 = mybir.dt.uint32
I32 = mybir.dt.int32
ALU = mybir.AluOpType
AF = mybir.ActivationFunctionType
AX = mybir.AxisListType

D = 1024
SEQ = 2048
NCORES = 8
TOK_PER_CORE = 2 * SEQ
NT = TOK_PER_CORE // 128
TILES_PER_SEQ = SEQ // 128
D_IN = 1536
NEXP = 16384
EPS = 1e-6
WINDOWS = (2, 4, 8, 16)
NS = 4
NS2 = 6
TB = 32
TG = 256
NW = 8
NEG = -1.0e30
ARENA_COLS = 47400
SEM_LIMIT = 12000
JGMAX = 16
EXTRA_SEMS = 0


class Sem:
    def __init__(self, h):
        self.h = h
        self.n = 0


class Res:
    def __init__(self, name):
        self.name = name
        self.writers = {}
        self.readers = {}


class Prog:
    def __init__(self, nc, stack):
        self.nc = nc
        self.stack = stack
        self.all_sems = []
        self.ops = {k: [] for k in ("pe", "dve", "act", "pool", "sp")}
        self.esem = {k: self.new_sem("e_" + k) for k in self.ops}
        self.pe_sems = {self.esem["pe"]}
        self.epoch = {k: 0 for k in self.ops}
        self.pending = {k: False for k in self.ops}
        self.waited = {k: {} for k in self.ops}

    def new_sem(self, name):
        s = Sem(self.stack.enter_context(self.nc.semaphore(name)))
        self.all_sems.append(s)
        return s

    def emit(self, eng, fn, reads=(), writes=(), dma_sem=None, inc_sem=True):
        if dma_sem is None and self.esem[eng].n >= SEM_LIMIT and not self.pending[eng]:
            self.epoch[eng] += 1
            self.esem[eng] = self.new_sem("e_%s_%d" % (eng, self.epoch[eng]))
            if eng == "pe":
                self.pe_sems.add(self.esem[eng])
        mysem = dma_sem if dma_sem is not None else self.esem[eng]
        inc = 16 if dma_sem is not None else 1
        deps = {}
        for r in reads:
            for s, v in r.writers.items():
                deps[s] = max(deps.get(s, 0), v)
        for w in writes:
            for s, v in w.writers.items():
                deps[s] = max(deps.get(s, 0), v)
            for s, v in w.readers.items():
                deps[s] = max(deps.get(s, 0), v)
        waits = []
        for s, v in deps.items():
            if eng == "pe" and s in self.pe_sems and dma_sem is None:
                continue
            if dma_sem is not None and s is dma_sem:
                continue
            if self.waited[eng].get(s, 0) >= v:
                continue
            self.waited[eng][s] = v
            waits.append((s.h, v))
        if inc_sem:
            mysem.n += inc
            val = mysem.n
            if dma_sem is None:
                self.pending[eng] = False
        else:
            assert dma_sem is None
            val = mysem.n + inc
            self.pending[eng] = True
        for w in writes:
            w.writers = {mysem: val}
            w.readers = {}
        for r in reads:
            if r not in writes:
                r.readers[mysem] = val
        h = mysem.h

        def run(e, waits=waits, fn=fn, h=h, inc=inc, inc_sem=inc_sem):
            for (sh, v) in waits:
                e.wait_ge(sh, v)
            if inc_sem:
                fn(e).then_inc(h, inc)
            else:
                fn(e)

        self.ops[eng].append(run)

    def barrier(self):
        snap = [(s, s.n) for s in self.all_sems if s.n > 0]
        for eng in self.ops:
            lst = [(s.h, v) for (s, v) in snap if self.waited[eng].get(s, 0) < v]
            for (s, v) in snap:
                self.waited[eng][s] = v

            def run(e, lst=lst):
                for (sh, v) in lst:
                    e.wait_ge(sh, v)

            self.ops[eng].append(run)

    def final_wait(self, eng, sems):
        lst = [(s.h, s.n) for s in sems if s.n > 0]

        def run(e, lst=lst):
            for (sh, v) in lst:
                e.wait_ge(sh, v)

        self.ops[eng].append(run)


def bc_ap(ap, dims):
    return bass.AP(ap.tensor, ap.offset, [list(ap.ap[0])] + [list(d) for d in dims])


def build_program(NT=NT, NHALF=2, debug=False):
    TOK = NT * 128
    NTH = NT // NHALF
    THALF = NTH * 128
    NG = THALF // TG
    nc = bass.Bass("TRN2", target_bir_lowering=False)
    dt_in = lambda name, shape, dt=F32: nc.dram_tensor(name, list(shape), dt, kind="ExternalInput").ap()
    x_d = dt_in("x", [TOK, D])
    w_in_d = dt_in("w_in", [D, D_IN])
    w_out_d = dt_in("w_out", [D, D])
    pool_w_d = dt_in("pool_w", [4, 128, 128])
    wst_d = dt_in("sgu_wt", [4, 128, 128])
    sgub_d = dt_in("sgu_b", [512])
    sgug_d = dt_in("sgu_g", [512])
    g1col_d = dt_in("g1col", [128, 8])
    pscale_d = dt_in("pscale", [128, 4])
    g2_d = dt_in("g2", [D])
    fg_d = dt_in("fg", [D])
    wqt_d = dt_in("wq_t", [16, 128, D])
    keyst_d = dt_in("keys_t", [16, 128, 128])
    ut_d = dt_in("peer_ut", [128, 128, D])
    vt_d = dt_in("peer_vt", [128, 128, D])
    ident_d = dt_in("ident", [128, 128])
    pm_d = dt_in("pmats", [12, 128, 128])
    iota_d = dt_in("iota16", [128, 16])
    iota128_d = dt_in("iota128", [128, 128])
    y_d = nc.dram_tensor("y", [TOK, D], F32, kind="ExternalOutput").ap()
    skind = "ExternalOutput" if debug else "Internal"
    h1_dram = nc.dram_tensor("h1_scr", [TOK, D], F32, kind=skind).ap()
    h2t_dram = nc.dram_tensor("h2t_scr", [128, 8, TOK], BF16, kind=skind).ap()
    g_dram = nc.dram_tensor("g_scr", [16, 128, TOK * 8], BF16, kind=skind).ap()

    with ExitStack() as stack:
        P = Prog(nc, stack)
        for _i in range(EXTRA_SEMS):
            P.new_sem("dummy%d" % _i)

        def sb(name, shape, dt=F32):
            t = stack.enter_context(nc.sbuf_tensor("sb_" + name, list(shape), dt))
            return t, Res(name)

        def ps(name, shape, dt=F32):
            t = stack.enter_context(nc.psum_tensor("ps_" + name, list(shape), dt))
            return t, Res(name)

        poolw_bf, R_poolw = sb("poolw_bf", [128, 4, 128], BF16)
        wst_bf, R_wst = sb("wst_bf", [128, 4, 128], BF16)
        ident_f, R_identf = sb("ident_f", [128, 128], F32)
        ident_bf, R_ident = sb("ident_bf", [128, 128], BF16)
        pm_bf, R_pm = sb("pm_bf", [128, 12, 128], BF16)
        g1col, R_g1col = sb("g1col", [128, 8])
        pscale, R_pscale = sb("pscale", [128, 4])
        iota16, R_iota = sb("iota16", [128, 16])
        iota128, R_iota128 = sb("iota128", [128, 128])
        g2_bc, R_g2 = sb("g2_bc", [128, D])
        fg_bc, R_fg = sb("fg_bc", [128, D])
        sgug_bc, R_sgug = sb("sgug_bc", [128, 512])
        b_bc, R_bbc = sb("b_bc", [128, 512])
        kt = [sb("kt%d" % i, [128, 128]) for i in range(2)]
        small, R_small = sb("small", [128, 32])
        st6, R_st6 = sb("st6", [128, 8])
        zsum, R_zsum = sb("zsum", [128, 16])

        arena = stack.enter_context(nc.sbuf_tensor("sb_arena", [128, ARENA_COLS], F32))
        bump = [0]

        def alloc(name, cols, dt=F32):
            f32cols = cols if dt in (F32, U32, I32) else (cols + 1) // 2
            o = bump[0]
            bump[0] += f32cols
            assert bump[0] <= ARENA_COLS, (name, bump[0])
            a = arena[:, o:o + f32cols]
            if dt != F32:
                a = a.bitcast(dt)
            return a, Res(name)

        w_in_bf, R_w_in = alloc("w_in_bf", 8 * D_IN, BF16)
        w_out_bf, R_w_out = alloc("w_out_bf", 8 * D, BF16)
        wk_bf, R_wk = alloc("wk_bf", 8 * 2048, BF16)
        w_in_v = w_in_bf.rearrange("p (k c) -> p k c", k=8)
        w_out_v = w_out_bf.rearrange("p (k c) -> p k c", k=8)
        wk_v = wk_bf.rearrange("p (k c) -> p k c", k=8)
        ring, _ = alloc("ring", NS * 1024)
        R_slot = [Res("slot%d" % i) for i in range(NS)]
        S_slot = [P.new_sem("s_slot%d" % i) for i in range(max(NS, NS2))]
        xbuf = [alloc("xbuf%d" % i, D) for i in range(2)]
        S_x = [P.new_sem("s_x%d" % i) for i in range(2)]
        xn_bf, R_xn = alloc("xn_bf", D, BF16)
        xnT, R_xnT = alloc("xnT", 8 * 128, BF16)
        xnT_v = xnT.rearrange("p (k t) -> p k t", k=8)
        zp_sb = [alloc("zp_sb%d" % i, 512, BF16) for i in range(2)]
        d_sb, R_d = alloc("d_sb", 512, BF16)
        ab_sb, R_ab = alloc("ab_sb", 8 * 128, BF16)
        ab_v = ab_sb.rearrange("p (k t) -> p k t", k=8)
        u_sb, R_u = alloc("u_sb", 512)
        v_f, R_v = alloc("v_f", 512)
        vn_bf, R_vn = alloc("vn_bf", 512, BF16)
        h1, R_h1 = alloc("h1", D)
        junk, R_junk = alloc("junk", D)
        scores_b = [alloc("scores%d" % i, 2048) for i in range(2)]
        top_s, R_tops = alloc("top_s", 256)
        top_i, R_topi = alloc("top_i", 256, U32)
        top_if, R_topif = alloc("top_if", 256)
        stmp, R_stmp = alloc("stmp", 256)
        best_s, R_best = alloc("best_s", 128)
        pos_u, R_pos = alloc("pos_u", 128, U32)
        hi_u, R_hiu = alloc("hi_u", 128, U32)
        lo_u, R_lou = alloc("lo_u", 128, U32)
        hif, R_hif = alloc("hif", 128)
        lof, R_lof = alloc("lof", 128)
        i1, R_i1 = alloc("i1", 128)
        i2, R_i2 = alloc("i2", 128)
        ework, R_ework = alloc("ework", 128)
        gate, R_gate = alloc("gate", 128)
        aT_sb, R_aT = alloc("aT_sb", 3 * 128)
        aT_v = aT_sb.rearrange("p (q t) -> p q t", q=3)
        Pbuf = [alloc("Pb%d" % i, TB * 128, BF16) for i in range(2)]
        Qbuf = [alloc("Qb%d" % i, TB * 128, BF16) for i in range(1)]
        Gst = [alloc("Gst%d" % i, 16 * TB * 8, BF16) for i in range(2)]
        S_gst = [P.new_sem("s_gst%d" % i) for i in range(2)]
        S_h1 = P.new_sem("s_h1")
        S_xnT = P.new_sem("s_xnT")
        S_c = {}

        def csem(name):
            S_c[name] = P.new_sem("s_" + name)
            return S_c[name]

        ptr, R_ptr = ps("ptr", [128, 8, 128], BF16)
        pAB, _ = ps("pAB", [128, 1024])
        pCD, _ = ps("pCD", [128, 1024])
        pGs, _ = ps("pGs", [128, 1024])
        pT, R_pT = ps("pT", [128, 512])
        pA, pB, pC, pD = pAB[:, 0:512], pAB[:, 512:1024], pCD[:, 0:512], pCD[:, 512:1024]
        R_pA, R_pB, R_pC, R_pD = [Res("pb%d" % i) for i in range(4)]
        pGsb = [pGs[:, 0:512], pGs[:, 512:1024]]
        R_pGs = [Res("pGs%d" % i) for i in range(2)]

        def slot(i, n=1024):
            return ring[:, i * 1024: i * 1024 + n]

        def load_const(dst, R, src, name):
            s = csem(name)
            P.emit("sp", lambda e: e.dma_start(out=dst, in_=src), writes=[R], dma_sem=s)

        load_const(ident_f[:], R_identf, ident_d, "ident")
        load_const(g1col[:], R_g1col, g1col_d, "g1col")
        load_const(pscale[:], R_pscale, pscale_d, "pscale")
        load_const(iota16[:], R_iota, iota_d, "iota")
        load_const(iota128[:], R_iota128, iota128_d, "iota128")
        load_const(g2_bc[:], R_g2, g2_d.partition_broadcast(128), "g2")
        load_const(fg_bc[:], R_fg, fg_d.partition_broadcast(128), "fg")
        load_const(sgug_bc[:], R_sgug, sgug_d.partition_broadcast(128), "sgug")
        load_const(b_bc[:], R_bbc, sgub_d.partition_broadcast(128), "bbc")
        P.emit("dve", lambda e: e.tensor_copy(out=ident_bf[:], in_=ident_f[:]), reads=[R_identf], writes=[R_ident])

        stage_ctr = [0]

        def stage_load(src_ap, ncols, view=None):
            i = stage_ctr[0] % NS
            stage_ctr[0] += 1
            dst = slot(i, ncols)
            if view is not None:
                dst = view(dst)
            P.emit("sp", lambda e: e.dma_start(out=dst, in_=src_ap), writes=[R_slot[i]], dma_sem=S_slot[i])
            return i

        cast_ctr = [0]

        def cast(dst, src, reads, writes, scale_ap=None):
            k = cast_ctr[0]
            cast_ctr[0] += 1
            if k % 2 == 0:
                if scale_ap is None:
                    P.emit("dve", lambda e: e.tensor_copy(out=dst, in_=src), reads=reads, writes=writes)
                else:
                    P.emit("dve", lambda e: e.tensor_scalar(out=dst, in0=src, scalar1=scale_ap, scalar2=None, op0=ALU.mult),
                           reads=reads, writes=writes)
            else:
                if scale_ap is None:
                    P.emit("act", lambda e: e.activation(out=dst, in_=src, func=AF.Copy), reads=reads, writes=writes)
                else:
                    P.emit("act", lambda e: e.activation(out=dst, in_=src, func=AF.Copy, scale=scale_ap), reads=reads, writes=writes)

        for half in range(2):
            i = stage_load(pm_d[half * 6:(half + 1) * 6].rearrange("m a b -> a m b"), 768,
                           view=lambda a: a.rearrange("p (m b) -> p m b", m=6))
            cast(pm_bf[:, half * 6:(half + 1) * 6, :], slot(i, 768).rearrange("p (m b) -> p m b", m=6),
                 [R_slot[i]], [R_pm])
        for kc in range(8):
            for (c0, c1) in ((0, 1024), (1024, 1536)):
                i = stage_load(w_in_d[kc * 128:(kc + 1) * 128, c0:c1], c1 - c0)
                cast(w_in_v[:, kc, c0:c1], slot(i, c1 - c0), [R_slot[i], R_g1col], [R_w_in], scale_ap=g1col[:, kc:kc + 1])
        for kc in range(8):
            i = stage_load(w_out_d[kc * 128:(kc + 1) * 128, :], 1024)
            cast(w_out_v[:, kc, :], slot(i), [R_slot[i]], [R_w_out])
        i = stage_load(pool_w_d.rearrange("g c d -> c g d"), 512, view=lambda a: a.rearrange("p (g d) -> p g d", g=4))
        cast(poolw_bf[:], slot(i, 512).rearrange("p (g d) -> p g d", g=4), [R_slot[i]], [R_poolw])
        i = stage_load(wst_d.rearrange("h j i -> j h i"), 512, view=lambda a: a.rearrange("p (h i) -> p h i", h=4))
        cast(wst_bf[:], slot(i, 512).rearrange("p (h i) -> p h i", h=4), [R_slot[i]], [R_wst])
        P.emit("dve", lambda e: e.memset(wst_bf[64:128, :, 0:64], 0.0), writes=[R_wst])
        for hp in range(16):
            i = stage_load(wqt_d[hp], 1024)
            ktile, R_kt = kt[hp % 2]
            if hp < 2:
                csem("kt%d" % hp)
            s_kt = S_c["kt%d" % (hp % 2)]
            P.emit("sp", lambda e, ktile=ktile, hp=hp: e.dma_start(out=ktile[:], in_=keyst_d[hp]), writes=[R_kt], dma_sem=s_kt)
            banks = ((pA, R_pA), (pB, R_pB)) if hp % 2 == 0 else ((pC, R_pC), (pD, R_pD))
            for b in range(2):
                pb_t, R_pb = banks[b]
                for q in range(4):
                    kc = b * 4 + q
                    P.emit("pe", lambda e, pb_t=pb_t, q=q, i=i, kc=kc, ktile=ktile: e.matmul(
                        pb_t[:, q * 128:(q + 1) * 128], lhsT=slot(i)[:, kc * 128:(kc + 1) * 128], rhs=ktile[:],
                        start=True, stop=True), reads=[R_slot[i], R_kt], writes=[R_pb], inc_sem=(q == 3))
                cast(wk_v[:, b * 4:(b + 1) * 4, hp * 128:(hp + 1) * 128],
                     pb_t.rearrange("p (q k) -> p q k", q=4), [R_pb], [R_wk])

        def load_x(n):
            xt, R_x = xbuf[n % 2]
            P.emit("sp", lambda e: e.dma_start(out=xt, in_=x_d[n * 128:(n + 1) * 128, :]), writes=[R_x], dma_sem=S_x[n % 2])

        def rms_stats(src, R_src, jk, R_jk, col):
            P.emit("act", lambda e: e.activation(out=jk, in_=src, func=AF.Square, accum_out=small[:, col:col + 1]),
                   reads=[R_src], writes=[R_jk, R_small])
            P.emit("act", lambda e: e.activation(out=small[:, col + 1:col + 2], in_=small[:, col:col + 1], func=AF.Sqrt,
                                                 scale=1.0 / D, bias=EPS), reads=[R_small], writes=[R_small])
            P.emit("dve", lambda e: e.reciprocal(out=small[:, col + 2:col + 3], in_=small[:, col + 1:col + 2]),
                   reads=[R_small], writes=[R_small])
            return small[:, col + 2:col + 3]

        def transposes(src_bf, R_src, dstT, R_dst):
            for kc in range(8):
                P.emit("pe", lambda e, kc=kc: e.transpose(ptr[:, kc, :], src_bf[:, kc * 128:(kc + 1) * 128], ident_bf[:]),
                       reads=[R_src, R_ident], writes=[R_ptr], inc_sem=(kc == 7))
            P.emit("act", lambda e: e.activation(out=dstT, in_=ptr[:], func=AF.Copy), reads=[R_ptr], writes=[R_dst])

        def stage_A(n):
            xt, R_x = xbuf[n % 2]
            first = (n % TILES_PER_SEQ == 0)
            zp_cur, R_zpc = zp_sb[n % 2]
            zp_prev, R_zpp = zp_sb[(n + 1) % 2]

            rstd1 = rms_stats(xt, R_x, xn_bf, R_xn, 0)
            yield
            P.emit("act", lambda e: e.activation(out=xn_bf, in_=xt, func=AF.Copy, scale=rstd1),
                   reads=[R_x, R_small], writes=[R_xn])
            yield
            transposes(xn_bf, R_xn, xnT_v, R_xnT)
            for kc in range(8):
                P.emit("pe", lambda e, kc=kc: e.matmul(pA, lhsT=xnT_v[:, kc, :], rhs=w_in_v[:, kc, 0:512],
                                                       start=(kc == 0), stop=(kc == 7)),
                       reads=[R_xnT, R_w_in], writes=[R_pA], inc_sem=(kc == 7))
            for kc in range(8):
                P.emit("pe", lambda e, kc=kc: e.matmul(pB, lhsT=xnT_v[:, kc, :], rhs=w_in_v[:, kc, 1024:1536],
                                                       start=(kc == 0), stop=(kc == 7)),
                       reads=[R_xnT, R_w_in], writes=[R_pB], inc_sem=(kc == 7))
            for m in range(4):
                for kc in range(8):
                    P.emit("pe", lambda e, kc=kc, m=m: e.matmul(pC[:, m * 128:(m + 1) * 128],
                                                                lhsT=w_in_v[:, kc, 512 + m * 128:512 + (m + 1) * 128],
                                                                rhs=xnT_v[:, kc, :], start=(kc == 0), stop=(kc == 7)),
                           reads=[R_xnT, R_w_in], writes=[R_pC], inc_sem=(kc == 7 and m == 3))
            yield
            P.emit("act", lambda e: e.activation(out=zp_cur, in_=pA, func=AF.Copy), reads=[R_pA], writes=[R_zpc])
            for g in range(4):
                mi = (8 + g) if first else g
                P.emit("pe", lambda e, g=g, mi=mi: e.matmul(
                    pD[:, g * 128:(g + 1) * 128], lhsT=zp_cur[:, g * 128:(g + 1) * 128], rhs=pm_bf[:, mi, :],
                    start=True, stop=first), reads=[R_zpc, R_pm], writes=[R_pD], inc_sem=(first and g == 3))
                if not first:
                    P.emit("pe", lambda e, g=g: e.matmul(
                        pD[:, g * 128:(g + 1) * 128], lhsT=zp_prev[:, g * 128:(g + 1) * 128], rhs=pm_bf[:, 4 + g, :],
                        start=False, stop=True), reads=[R_zpp, R_pm], writes=[R_pD], inc_sem=(g == 3))
            yield
            P.emit("dve", lambda e: e.tensor_copy(out=d_sb, in_=pD), reads=[R_pD], writes=[R_d])
            for g in range(4):
                P.emit("pe", lambda e, g=g: e.matmul(pA[:, g * 128:(g + 1) * 128], lhsT=poolw_bf[:, g, :],
                                                     rhs=d_sb[:, g * 128:(g + 1) * 128], start=True, stop=True),
                       reads=[R_poolw, R_d], writes=[R_pA], inc_sem=(g == 3))
            P.emit("dve", lambda e: e.tensor_tensor(out=ab_v[:, 0:4, :], in0=pA.rearrange("p (g t) -> p g t", g=4),
                                                    in1=bc_ap(pscale[:], [[1, 4], [0, 128]]), op=ALU.mult),
                   reads=[R_pA, R_pscale], writes=[R_ab])
            yield
            P.emit("act", lambda e: e.activation(out=u_sb, in_=pC, func=AF.Gelu_apprx_tanh), reads=[R_pC], writes=[R_u])
            P.emit("act", lambda e: e.activation(out=v_f, in_=pB, func=AF.Gelu_apprx_tanh), reads=[R_pB], writes=[R_v])
            yield
            P.emit("dve", lambda e: e.bn_stats(out=st6[:, 0:6], in_=v_f), reads=[R_v], writes=[R_st6])
            P.emit("dve", lambda e: e.bn_aggr(out=st6[:, 6:8], in_=st6[:, 0:6]), reads=[R_st6], writes=[R_st6])
            P.emit("act", lambda e: e.activation(out=small[:, 8:9], in_=st6[:, 7:8], func=AF.Sqrt, bias=EPS),
                   reads=[R_st6], writes=[R_small])
            yield
            P.emit("dve", lambda e: e.reciprocal(out=small[:, 9:10], in_=small[:, 8:9]), reads=[R_small], writes=[R_small])
            P.emit("dve", lambda e: e.tensor_scalar(out=v_f, in0=v_f, scalar1=st6[:, 6:7], scalar2=small[:, 9:10],
                                                    op0=ALU.subtract, op1=ALU.mult),
                   reads=[R_v, R_st6, R_small], writes=[R_v])
            P.emit("dve", lambda e: e.tensor_tensor(out=vn_bf, in0=v_f, in1=sgug_bc[:], op=ALU.mult),
                   reads=[R_v, R_sgug], writes=[R_vn])
            yield
            for h in range(4):
                P.emit("pe", lambda e, h=h: e.matmul(pB[:, h * 128:(h + 1) * 128], lhsT=vn_bf[:, h * 128:(h + 1) * 128],
                                                     rhs=wst_bf[:, h, :], start=True, stop=True),
                       reads=[R_vn, R_wst], writes=[R_pB], inc_sem=(h == 3))
            yield
            P.emit("dve", lambda e: e.tensor_tensor(out=v_f, in0=pB, in1=b_bc[:], op=ALU.add),
                   reads=[R_pB, R_bbc], writes=[R_v])
            P.emit("dve", lambda e: e.tensor_tensor(out=ab_v[:, 4:8, :], in0=v_f.rearrange("p (h t) -> p h t", h=4),
                                                    in1=u_sb.rearrange("p (h t) -> p h t", h=4), op=ALU.mult),
                   reads=[R_v, R_u], writes=[R_ab])
            yield
            for half, (pO_, R_pO_) in enumerate(((pC, R_pC), (pD, R_pD))):
                for fc in range(8):
                    P.emit("pe", lambda e, fc=fc, half=half, pO_=pO_: e.matmul(
                        pO_, lhsT=ab_v[:, fc, :], rhs=w_out_v[:, fc, half * 512:(half + 1) * 512],
                        start=(fc == 0), stop=(fc == 7)), reads=[R_ab, R_w_out], writes=[R_pO_], inc_sem=(fc == 7))
                P.emit("dve", lambda e, half=half, pO_=pO_: e.tensor_tensor(
                    out=h1[:, half * 512:(half + 1) * 512], in0=pO_, in1=xt[:, half * 512:(half + 1) * 512], op=ALU.add),
                    reads=[R_pO_, R_x], writes=[R_h1])
            yield
            P.emit("sp", lambda e: e.dma_start(out=h1_dram[n * 128:(n + 1) * 128, :], in_=h1), reads=[R_h1], dma_sem=S_h1)
            if n + 2 < NT:
                load_x(n + 2)
            rstd2 = rms_stats(h1, R_h1, junk, R_junk, 3)
            yield
            P.emit("dve", lambda e: e.scalar_tensor_tensor(out=xn_bf, in0=h1, scalar=rstd2, in1=g2_bc[:],
                                                           op0=ALU.mult, op1=ALU.mult),
                   reads=[R_h1, R_small, R_g2], writes=[R_xn])
            transposes(xn_bf, R_xn, xnT_v, R_xnT)
            P.emit("sp", lambda e: e.dma_start(out=h2t_dram[:, :, n * 128:(n + 1) * 128], in_=xnT_v), reads=[R_xnT], dma_sem=S_xnT)
            yield
            sbanks = ((pA, R_pA), (pB, R_pB), (pC, R_pC), (pD, R_pD))
            scores, R_sc = scores_b[n % 2]
            for c, (pb_t, R_pb) in enumerate(sbanks):
                for kc in range(8):
                    P.emit("pe", lambda e, kc=kc, c=c, pb_t=pb_t: e.matmul(
                        pb_t, lhsT=xnT_v[:, kc, :], rhs=wk_v[:, kc, c * 512:(c + 1) * 512],
                        start=(kc == 0), stop=(kc == 7)), reads=[R_xnT, R_wk], writes=[R_pb], inc_sem=(kc == 7))
                P.emit("act", lambda e, c=c, pb_t=pb_t: e.activation(out=scores[:, c * 512:(c + 1) * 512], in_=pb_t, func=AF.Copy),
                       reads=[R_pb], writes=[R_sc])

        gs_ctr = [0]

        def stage_B(n):
            scores, R_sc = scores_b[n % 2]
            for g in range(16):
                sg = scores[:, g * 128:(g + 1) * 128]
                o = g * 16
                P.emit("dve", lambda e, sg=sg, o=o: e.max(out=top_s[:, o:o + 8], in_=sg), reads=[R_sc], writes=[R_tops])
                P.emit("dve", lambda e, sg=sg, o=o: e.max_index(out=top_i[:, o:o + 8], in_max=top_s[:, o:o + 8], in_values=sg),
                       reads=[R_sc, R_tops], writes=[R_topi])
                P.emit("dve", lambda e, sg=sg, o=o: e.match_replace(out=stmp[:, 0:128], in_to_replace=top_s[:, o:o + 8],
                                                                    in_values=sg, imm_value=NEG),
                       reads=[R_sc, R_tops], writes=[R_stmp])
                P.emit("dve", lambda e, o=o: e.max(out=top_s[:, o + 8:o + 16], in_=stmp[:, 0:128]),
                       reads=[R_stmp], writes=[R_tops])
                P.emit("dve", lambda e, o=o: e.max_index(out=top_i[:, o + 8:o + 16], in_max=top_s[:, o + 8:o + 16],
                                                         in_values=stmp[:, 0:128]),
                       reads=[R_stmp, R_tops], writes=[R_topi])
                if g % 2 == 1:
                    yield
            yield
            P.emit("dve", lambda e: e.tensor_copy(out=top_if, in_=top_i), reads=[R_topi], writes=[R_topif])
            cand = ring[:, 0:2048]
            R_cand = [R_slot[0], R_slot[1]]
            P.emit("dve", lambda e: e.tensor_tensor(
                out=cand.rearrange("p (h a b) -> p h a b", h=8, a=16),
                in0=bc_ap(top_s[:, 0:16], [[32, 8], [1, 16], [0, 16]]),
                in1=bc_ap(top_s[:, 16:32], [[32, 8], [0, 16], [1, 16]]), op=ALU.add),
                reads=[R_tops], writes=R_cand)
            for h in range(8):
                ch = cand[:, h * 256:(h + 1) * 256]
                o = h * 16
                P.emit("dve", lambda e, ch=ch, o=o: e.max(out=best_s[:, o:o + 8], in_=ch), reads=R_cand, writes=[R_best])
                P.emit("dve", lambda e, ch=ch, o=o: e.max_index(out=pos_u[:, o:o + 8], in_max=best_s[:, o:o + 8], in_values=ch),
                       reads=R_cand + [R_best], writes=[R_pos])
                P.emit("dve", lambda e, ch=ch, o=o: e.match_replace(out=stmp, in_to_replace=best_s[:, o:o + 8],
                                                                    in_values=ch, imm_value=NEG),
                       reads=R_cand + [R_best], writes=[R_stmp])
                P.emit("dve", lambda e, o=o: e.max(out=best_s[:, o + 8:o + 16], in_=stmp), reads=[R_stmp], writes=[R_best])
                P.emit("dve", lambda e, o=o: e.max_index(out=pos_u[:, o + 8:o + 16], in_max=best_s[:, o + 8:o + 16],
                                                         in_values=stmp),
                       reads=[R_stmp, R_best], writes=[R_pos])
                if h % 2 == 1:
                    yield
            yield
            P.emit("dve", lambda e: e.tensor_single_scalar(out=hi_u, in_=pos_u, scalar=4, op=ALU.logical_shift_right),
                   reads=[R_pos], writes=[R_hiu])
            P.emit("dve", lambda e: e.tensor_single_scalar(out=lo_u, in_=pos_u, scalar=15, op=ALU.bitwise_and),
                   reads=[R_pos], writes=[R_lou])
            P.emit("dve", lambda e: e.tensor_copy(out=hif, in_=hi_u), reads=[R_hiu], writes=[R_hif])
            P.emit("dve", lambda e: e.tensor_copy(out=lof, in_=lo_u), reads=[R_lou], writes=[R_lof])
            wk = ring[:, 2048:4096]
            R_wkk = [R_slot[2], R_slot[3]]
            for (sel, off, dst, R_dst, R_sel) in ((hif, 0, i1, R_i1, R_hif), (lof, 16, i2, R_i2, R_lof)):
                P.emit("dve", lambda e, sel=sel: e.tensor_tensor(
                    out=wk.rearrange("p (k a) -> p k a", a=16),
                    in0=bc_ap(iota16[:], [[0, 128], [1, 16]]),
                    in1=bc_ap(sel, [[1, 128], [0, 16]]), op=ALU.is_equal),
                    reads=[R_iota, R_sel], writes=R_wkk)
                P.emit("dve", lambda e, off=off: e.tensor_tensor(
                    out=wk.rearrange("p (h k a) -> p h k a", h=8, k=16),
                    in0=wk.rearrange("p (h k a) -> p h k a", h=8, k=16),
                    in1=bc_ap(top_if[:, off:off + 16], [[32, 8], [0, 16], [1, 16]]), op=ALU.mult),
                    reads=R_wkk + [R_topif], writes=R_wkk)
                P.emit("dve", lambda e, dst=dst: e.tensor_reduce(
                    out=dst, in_=wk.rearrange("p (k a) -> p k a", a=16), axis=AX.X, op=ALU.add),
                    reads=R_wkk, writes=[R_dst])
            yield
            P.emit("dve", lambda e: e.tensor_tensor(out=ework.rearrange("p (h k) -> p h k", h=8),
                                                    in0=best_s.rearrange("p (h k) -> p h k", h=8),
                                                    in1=bc_ap(best_s, [[16, 8], [0, 16]]), op=ALU.subtract),
                   reads=[R_best], writes=[R_ework])
            P.emit("act", lambda e: e.activation(out=ework, in_=ework, func=AF.Exp), reads=[R_ework], writes=[R_ework])
            yield
            P.emit("dve", lambda e: e.tensor_reduce(out=zsum[:, 0:8], in_=ework.rearrange("p (h k) -> p h k", h=8),
                                                    axis=AX.X, op=ALU.add), reads=[R_ework], writes=[R_zsum])
            P.emit("dve", lambda e: e.reciprocal(out=zsum[:, 8:16], in_=zsum[:, 0:8]), reads=[R_zsum], writes=[R_zsum])
            P.emit("dve", lambda e: e.tensor_tensor(out=gate.rearrange("p (h k) -> p h k", h=8),
                                                    in0=ework.rearrange("p (h k) -> p h k", h=8),
                                                    in1=bc_ap(zsum[:, 8:16], [[1, 8], [0, 16]]), op=ALU.mult),
                   reads=[R_ework, R_zsum], writes=[R_gate])
            yield
            for q, (src, R_src) in enumerate(((i1, R_i1), (i2, R_i2), (gate, R_gate))):
                P.emit("pe", lambda e, q=q, src=src: e.transpose(pT[:, q * 128:(q + 1) * 128], src, ident_f[:]),
                       reads=[R_src, R_identf], writes=[R_pT], inc_sem=(q == 2))
            P.emit("act", lambda e: e.activation(out=aT_sb, in_=pT[:, 0:384], func=AF.Copy), reads=[R_pT], writes=[R_aT])
            yield
            for blk in range(128 // TB):
                t0 = blk * TB
                Pb, R_Pb = Pbuf[blk % 2]
                Qb, R_Qb = Qbuf[0]
                gst, R_gst = Gst[blk % 2]
                s_gst = S_gst[blk % 2]
                P.emit("dve", lambda e, Pb=Pb, t0=t0: e.tensor_tensor(
                    out=Pb.rearrange("p (t i) -> p t i", t=TB),
                    in0=bc_ap(iota128[:], [[0, TB], [1, 128]]),
                    in1=bc_ap(aT_v[:, 0, t0:t0 + TB], [[1, TB], [0, 128]]), op=ALU.is_equal),
                    reads=[R_iota128, R_aT], writes=[R_Pb])
                P.emit("dve", lambda e, Qb=Qb, t0=t0: e.tensor_tensor(
                    out=Qb.rearrange("p (t i) -> p t i", t=TB),
                    in0=bc_ap(iota128[:], [[0, TB], [1, 128]]),
                    in1=bc_ap(aT_v[:, 1, t0:t0 + TB], [[1, TB], [0, 128]]), op=ALU.is_equal),
                    reads=[R_iota128, R_aT], writes=[R_Qb])
                P.emit("pool", lambda e, Pb=Pb, t0=t0: e.tensor_tensor(
                    out=Pb.rearrange("p (t i) -> p t i", t=TB),
                    in0=Pb.rearrange("p (t i) -> p t i", t=TB),
                    in1=bc_ap(aT_v[:, 2, t0:t0 + TB], [[1, TB], [0, 128]]), op=ALU.mult),
                    reads=[R_Pb, R_aT], writes=[R_Pb])
                for t4 in range(TB // 4):
                    bi = gs_ctr[0] % 2
                    gs_ctr[0] += 1
                    bank, R_bank = pGsb[bi], R_pGs[bi]
                    for tk in range(4):
                        t = t4 * 4 + tk
                        P.emit("pe", lambda e, bank=bank, tk=tk, t=t, Pb=Pb, Qb=Qb: e.matmul(
                            bank[:, tk * 128:(tk + 1) * 128], lhsT=Pb[:, t * 128:(t + 1) * 128], rhs=Qb[:, t * 128:(t + 1) * 128],
                            start=True, stop=True), reads=[R_Pb, R_Qb], writes=[R_bank], inc_sem=(tk == 3))
                    P.emit("act", lambda e, bank=bank, gst=gst, t4=t4: e.activation(
                        out=bc_ap(gst[:, t4 * 32:t4 * 32 + 1], [[8, 4], [TB * 8, 16], [1, 8]]),
                        in_=bank.rearrange("p (tk jg jj) -> p tk jg jj", tk=4, jg=16), func=AF.Copy),
                        reads=[R_bank], writes=[R_gst])
                tok = n * 128 + t0
                P.emit("sp", lambda e, gst=gst, tok=tok: e.dma_start(
                    out=g_dram[:, :, tok * 8:(tok + TB) * 8].rearrange("g p x -> p g x"),
                    in_=gst.rearrange("p (g x) -> p g x", g=16)), reads=[R_gst], dma_sem=s_gst)
                yield

        load_x(0)
        if NT > 1:
            load_x(1)
        for _ in stage_A(0):
            pass
        for n in range(NT):
            gb = stage_B(n)
            ga = stage_A(n + 1) if n + 1 < NT else iter(())
            done_a = done_b = False
            while not (done_a and done_b):
                if not done_b:
                    try:
                        next(gb)
                    except StopIteration:
                        done_b = True
                if not done_a:
                    try:
                        next(ga)
                    except StopIteration:
                        done_a = True

        P.barrier()
        bump[0] = 0
        accum, _ = alloc("accum", NTH * D)
        accum_v = accum.rearrange("p (n d) -> p n d", n=NTH)
        R_acc = [Res("acc%d" % i) for i in range(NTH)]
        h2T, R_h2T = alloc("h2T", 8 * THALF, BF16)
        h2T_v = h2T.rearrange("p (k t) -> p k t", k=8)
        wU = [alloc("wU%d" % i, 1024, BF16) for i in range(NW)]
        wV = [alloc("wV%d" % i, 1024, BF16) for i in range(NW)]
        ring2, _ = alloc("ring2", NS2 * 1024)
        R_slot2 = [Res("slot2_%d" % i) for i in range(NS2)]
        gsl = [alloc("gsl%d" % i, TG * 8, BF16) for i in range(2)]
        S_gsl = [P.new_sem("s_gsl%d" % i) for i in range(2)]
        gsb = [alloc("gsb%d" % i, TG) for i in range(2)]
        mt = [alloc("mt%d" % i, TG, BF16) for i in range(3)]
        ystage = [alloc("ystage%d" % i, D) for i in range(2)]
        S_ys = [P.new_sem("s_ys%d" % i) for i in range(2)]
        junk2, R_junk2 = alloc("junk2", D)
        S_acc = [P.new_sem("s_acc%d" % i) for i in range(4)]
        S_h2T = [P.new_sem("s_h2T%d" % i) for i in range(4)]
        R_h2Tq = [Res("h2Tq%d" % i) for i in range(4)]
        pat = [pGs[:, 0:TG], pGs[:, 512:512 + TG], pT[:, 0:TG]]
        R_pat = [Res("pat%d" % i) for i in range(3)]
        pO = [(pAB, Res("pO0")), (pCD, Res("pO1"))]

        def slot2(i):
            return ring2[:, i * 1024:(i + 1) * 1024]

        st2_ctr = [0]
        ys_ctr = [0]

        et_slots = {}

        def stage_etile(j):
            sl = []
            for src in (ut_d[j], vt_d[j]):
                i = st2_ctr[0] % NS2
                st2_ctr[0] += 1
                P.emit("sp", lambda e, i=i, src=src: e.dma_start(out=slot2(i), in_=src), writes=[R_slot2[i]], dma_sem=S_slot[i])
                sl.append(i)
            et_slots[j] = sl

        def cast_etile(j):
            ws = j % NW
            for i, (dst, R_dst) in zip(et_slots.pop(j), (wU[ws], wV[ws])):
                P.emit("act", lambda e, i=i, dst=dst: e.activation(out=dst, in_=slot2(i), func=AF.Copy),
                       reads=[R_slot2[i]], writes=[R_dst])

        def load_gsl(h, jg, tg, idx):
            b = idx % 2
            tok = h * THALF + tg * TG
            g_t, R_g = gsl[b]
            P.emit("act", lambda e: e.dma_start(out=g_t, in_=g_dram[jg, :, tok * 8:(tok + TG) * 8]), writes=[R_g], dma_sem=S_gsl[b])

        for h in range(NHALF):
            tok0 = h * THALF
            na = max(1, NTH // 4)
            for q in range(0, NTH, na):
                P.emit("sp", lambda e, tok0=tok0, q=q: e.dma_start(
                    out=accum_v[:, q:q + na, :],
                    in_=h1_dram[tok0 + q * 128:tok0 + (q + na) * 128, :].rearrange("(n p) d -> p n d", p=128)),
                    writes=R_acc[q:q + na], dma_sem=S_acc[q // na])
            for q in range(4):
                P.emit("sp", lambda e, tok0=tok0, q=q: e.dma_start(out=h2T_v[:, 2 * q:2 * q + 2, :],
                                                                 in_=h2t_dram[:, 2 * q:2 * q + 2, tok0:tok0 + THALF]),
                       writes=[R_h2Tq[q]], dma_sem=S_h2T[q])
            for j in range(8):
                stage_etile(j)
                cast_etile(j)
            its = [(jg, tg, jj) for jg in range(JGMAX) for tg in range(NG) for jj in range(8)]
            NI = len(its)
            load_gsl(h, 0, 0, 0)
            for k in range(NI + 2):
                if k < NI:
                    jg, tg, jj = its[k]
                    gidx = jg * NG + tg
                    j = jg * 8 + jj
                    ws = j % NW
                    if jj == 0:
                        if gidx + 1 < JGMAX * NG:
                            jg2, tg2 = divmod(gidx + 1, NG)
                            load_gsl(h, jg2, tg2, gidx + 1)
                    if tg == NG - 1 and jg + 1 < JGMAX:
                        stage_etile((jg + 1) * 8 + jj)
                    pt_, R_pt = pat[k % 3], R_pat[k % 3]
                    wU_t, R_wU = wU[ws]
                    for c in range(8):
                        P.emit("pe", lambda e, c=c, pt_=pt_, wU_t=wU_t, tg=tg: e.matmul(
                            pt_, lhsT=wU_t[:, c * 128:(c + 1) * 128], rhs=h2T_v[:, c, tg * TG:(tg + 1) * TG],
                            start=(c == 0), stop=(c == 7)), reads=[R_wU, R_h2Tq[c // 2]], writes=[R_pt], inc_sem=(c == 7))
                    g_t, R_g = gsb[k % 2]
                    P.emit("act", lambda e, pt_=pt_, g_t=g_t: e.activation(out=g_t, in_=pt_, func=AF.Gelu_apprx_tanh),
                           reads=[R_pt], writes=[R_g])
                    m_t, R_m = mt[k % 3]
                    gs_t, R_gs = gsl[gidx % 2]
                    P.emit("dve", lambda e, m_t=m_t, g_t=g_t, gs_t=gs_t, jj=jj: e.tensor_tensor(
                        out=m_t, in0=g_t, in1=bc_ap(gs_t[:, jj:jj + 1], [[8, TG]]), op=ALU.mult),
                        reads=[R_g, R_gs], writes=[R_m])
                if k >= 2:
                    jg, tg, jj = its[k - 2]
                    j = jg * 8 + jj
                    ws = j % NW
                    m_t, R_m = mt[(k - 2) % 3]
                    wV_t, R_wV = wV[ws]
                    for ti in range(TG // 128):
                        pO_t, R_pO = pO[ti]
                        for hf in range(2):
                            P.emit("pe", lambda e, pO_t=pO_t, hf=hf, ti=ti, m_t=m_t, wV_t=wV_t, jj=jj: e.matmul(
                                pO_t[:, hf * 512:(hf + 1) * 512], lhsT=m_t[:, ti * 128:(ti + 1) * 128],
                                rhs=wV_t[:, hf * 512:(hf + 1) * 512], start=(jj == 0), stop=(jj == 7)),
                                reads=[R_m, R_wV], writes=[R_pO], inc_sem=(ti == TG // 128 - 1 and hf == 1))
                    if tg == NG - 1 and jg + 1 < JGMAX:
                        cast_etile((jg + 1) * 8 + jj)
                    if jj == 7:
                        for ti in range(TG // 128):
                            pO_t, R_pO = pO[ti]
                            a = tg * (TG // 128) + ti
                            P.emit("dve", lambda e, pO_t=pO_t, a=a: e.tensor_tensor(
                                out=accum_v[:, a, :], in0=pO_t[:], in1=accum_v[:, a, :], op=ALU.add),
                                reads=[R_pO, R_acc[a]], writes=[R_acc[a]])
            for a in range(NTH):
                n = h * NTH + a
                yb = ys_ctr[0] % 2
                ys_ctr[0] += 1
                y_t, R_y = ystage[yb]
                rstd3 = rms_stats(accum_v[:, a, :], R_acc[a], junk2, R_junk2, 6)
                P.emit("dve", lambda e, a=a, y_t=y_t, rstd3=rstd3: e.scalar_tensor_tensor(
                    out=y_t, in0=accum_v[:, a, :], scalar=rstd3, in1=fg_bc[:], op0=ALU.mult, op1=ALU.mult),
                    reads=[R_acc[a], R_small, R_fg], writes=[R_y])
                P.emit("sp", lambda e, n=n, y_t=y_t: e.dma_start(out=y_d[n * 128:(n + 1) * 128, :], in_=y_t),
                       reads=[R_y], dma_sem=S_ys[yb])

        P.final_wait("sp", S_ys)

        with nc.Block() as block:
            @block.sync
            def _(e):
                for f in P.ops["sp"]:
                    f(e)

            @block.tensor
            def _(e):
                for f in P.ops["pe"]:
                    f(e)

            @block.vector
            def _(e):
                for f in P.ops["dve"]:
                    f(e)

            @block.scalar
            def _(e):
                for f in P.ops["act"]:
                    f(e)

            @block.gpsimd
            def _(e):
                for f in P.ops["pool"]:
                    f(e)
    return nc


def _pool_mats():
    m = np.zeros((12, 128, 128), np.float32)
    tp = np.arange(128)[:, None]
    t = np.arange(128)[None, :]
    for g, w in enumerate(WINDOWS):
        band = ((tp <= t) & (tp > t - w)).astype(np.float32)
        m[g] = band / w - (tp == t)
        m[4 + g] = ((tp - 128) > (t - w)).astype(np.float32) / w
        cnt = np.minimum(t + 1, w).astype(np.float32)
        m[8 + g] = band / cnt - (tp == t)
    return m


def host_layout(inp):
    f = lambda a: np.ascontiguousarray(np.asarray(a, dtype=np.float32))
    wq = f(inp["peer_wq"])[0]
    keys = f(inp["peer_keys"])[0]
    pu = f(inp["peer_u"])[0]
    pv = f(inp["peer_v"])[0]
    return {
        "w_in": f(inp["w_in"])[0],
        "w_out": f(inp["w_out"])[0],
        "pool_w": f(inp["pool_w"])[0],
        "sgu_wt": f(np.transpose(f(inp["sgu_w"])[0], (0, 2, 1))),
        "sgu_b": f(inp["sgu_b"])[0].reshape(512),
        "sgu_g": f(inp["sgu_norm_g"])[0].reshape(512),
        "g1col": f(f(inp["norm1_g"])[0].reshape(8, 128).T),
        "pscale": f(f(inp["pool_scale"])[0].reshape(4, 128).T),
        "g2": f(inp["norm2_g"])[0].reshape(D),
        "fg": f(inp["final_g"]).reshape(D),
        "wq_t": f(wq.reshape(D, 16, 128).transpose(1, 2, 0)),
        "keys_t": f(keys.transpose(1, 0, 3, 2).reshape(16, 128, 128)),
        "peer_ut": f(pu.reshape(128, 128, 8, 128).transpose(1, 3, 2, 0)).reshape(128, 128, D),
        "peer_vt": f(pv.reshape(128, 128, D).transpose(1, 0, 2)),
        "ident": np.eye(128, dtype=np.float32),
        "pmats": _pool_mats(),
        "iota16": f(np.broadcast_to(np.arange(16, dtype=np.float32), (128, 16))),
        "iota128": f(np.broadcast_to(np.arange(128, dtype=np.float32), (128, 128))),
    }


_NC_CACHE = {}


def kernel(x, norm1_g, w_in, pool_w, pool_scale, sgu_norm_g, sgu_w, sgu_b, w_out, norm2_g,
           peer_wq, peer_keys, peer_u, peer_v, final_g):
    x = np.ascontiguousarray(np.asarray(x, dtype=np.float32))
    B = x.shape[0]
    xs = x.reshape(NCORES, (B // NCORES) * SEQ, D)
    shared = host_layout(dict(norm1_g=norm1_g, w_in=w_in, pool_w=pool_w, pool_scale=pool_scale, sgu_norm_g=sgu_norm_g,
                              sgu_w=sgu_w, sgu_b=sgu_b, w_out=w_out, norm2_g=norm2_g, peer_wq=peer_wq,
                              peer_keys=peer_keys, peer_u=peer_u, peer_v=peer_v, final_g=final_g))
    if "nc" not in _NC_CACHE:
        _NC_CACHE["nc"] = build_program()
    nc = _NC_CACHE["nc"]
    in_maps = [dict(shared, x=np.ascontiguousarray(xs[c])) for c in range(NCORES)]
    res = run_bass_kernel_spmd(nc, in_maps, core_ids=list(range(NCORES)))
    out = np.stack([res.results[c]["y"] for c in range(NCORES)], axis=0)
    return out.reshape(B, SEQ, D).astype(np.float32)
```

```python
from contextlib import ExitStack
import numpy as np
import concourse.bass as bass
import concourse.mybir as mybir
from concourse.bass_utils import run_bass_kernel_spmd

F32 = mybir.dt.float32
BF16 = mybir.dt.bfloat16
U32 = mybir.dt.uint32
I32 = mybir.dt.int32
ALU = mybir.AluOpType
AF = mybir.ActivationFunctionType
AX = mybir.AxisListType

D = 1024
SEQ = 2048
NCORES = 8
TOK_PER_CORE = 2 * SEQ
NT = TOK_PER_CORE // 128
TILES_PER_SEQ = SEQ // 128
D_IN = 1536
NEXP = 16384
EPS = 1e-6
WINDOWS = (2, 4, 8, 16)
NS = 4
NS2 = 6
TB = 32
TG = 256
NW = 8
NEG = -1.0e30
ARENA_COLS = 47400
SEM_LIMIT = 12000
JGMAX = 16
EXTRA_SEMS = 0


class Sem:
    def __init__(self, h):
        self.h = h
        self.n = 0


class Res:
    def __init__(self, name):
        self.name = name
        self.writers = {}
        self.readers = {}


class Prog:
    def __init__(self, nc, stack):
        self.nc = nc
        self.stack = stack
        self.all_sems = []
        self.ops = {k: [] for k in ("pe", "dve", "act", "pool", "sp")}
        self.esem = {k: self.new_sem("e_" + k) for k in self.ops}
        self.pe_sems = {self.esem["pe"]}
        self.epoch = {k: 0 for k in self.ops}
        self.pending = {k: False for k in self.ops}
        self.waited = {k: {} for k in self.ops}

    def new_sem(self, name):
        s = Sem(self.stack.enter_context(self.nc.semaphore(name)))
        self.all_sems.append(s)
        return s

    def emit(self, eng, fn, reads=(), writes=(), dma_sem=None, inc_sem=True):
        if dma_sem is None and self.esem[eng].n >= SEM_LIMIT and not self.pending[eng]:
            self.epoch[eng] += 1
            self.esem[eng] = self.new_sem("e_%s_%d" % (eng, self.epoch[eng]))
            if eng == "pe":
                self.pe_sems.add(self.esem[eng])
        mysem = dma_sem if dma_sem is not None else self.esem[eng]
        inc = 16 if dma_sem is not None else 1
        deps = {}
        for r in reads:
            for s, v in r.writers.items():
                deps[s] = max(deps.get(s, 0), v)
        for w in writes:
            for s, v in w.writers.items():
                deps[s] = max(deps.get(s, 0), v)
            for s, v in w.readers.items():
                deps[s] = max(deps.get(s, 0), v)
        waits = []
        for s, v in deps.items():
            if eng == "pe" and s in self.pe_sems and dma_sem is None:
                continue
            if dma_sem is not None and s is dma_sem:
                continue
            if self.waited[eng].get(s, 0) >= v:
                continue
            self.waited[eng][s] = v
            waits.append((s.h, v))
        if inc_sem:
            mysem.n += inc
            val = mysem.n
            if dma_sem is None:
                self.pending[eng] = False
        else:
            assert dma_sem is None
            val = mysem.n + inc
            self.pending[eng] = True
        for w in writes:
            w.writers = {mysem: val}
            w.readers = {}
        for r in reads:
            if r not in writes:
                r.readers[mysem] = val
        h = mysem.h

        def run(e, waits=waits, fn=fn, h=h, inc=inc, inc_sem=inc_sem):
            for (sh, v) in waits:
                e.wait_ge(sh, v)
            if inc_sem:
                fn(e).then_inc(h, inc)
            else:
                fn(e)

        self.ops[eng].append(run)

    def barrier(self):
        snap = [(s, s.n) for s in self.all_sems if s.n > 0]
        for eng in self.ops:
            lst = [(s.h, v) for (s, v) in snap if self.waited[eng].get(s, 0) < v]
            for (s, v) in snap:
                self.waited[eng][s] = v

            def run(e, lst=lst):
                for (sh, v) in lst:
                    e.wait_ge(sh, v)

            self.ops[eng].append(run)

    def final_wait(self, eng, sems):
        lst = [(s.h, s.n) for s in sems if s.n > 0]

        def run(e, lst=lst):
            for (sh, v) in lst:
                e.wait_ge(sh, v)

        self.ops[eng].append(run)


def bc_ap(ap, dims):
    return bass.AP(ap.tensor, ap.offset, [list(ap.ap[0])] + [list(d) for d in dims])


def build_program(NT=NT, NHALF=2, debug=False):
    TOK = NT * 128
    NTH = NT // NHALF
    THALF = NTH * 128
    NG = THALF // TG
    nc = bass.Bass("TRN2", target_bir_lowering=False)
    dt_in = lambda name, shape, dt=F32: nc.dram_tensor(name, list(shape), dt, kind="ExternalInput").ap()
    x_d = dt_in("x", [TOK, D])
    w_in_d = dt_in("w_in", [D, D_IN])
    w_out_d = dt_in("w_out", [D, D])
    pool_w_d = dt_in("pool_w", [4, 128, 128])
    wst_d = dt_in("sgu_wt", [4, 128, 128])
    sgub_d = dt_in("sgu_b", [512])
    sgug_d = dt_in("sgu_g", [512])
    g1col_d = dt_in("g1col", [128, 8])
    pscale_d = dt_in("pscale", [128, 4])
    g2_d = dt_in("g2", [D])
    fg_d = dt_in("fg", [D])
    wqt_d = dt_in("wq_t", [16, 128, D])
    keyst_d = dt_in("keys_t", [16, 128, 128])
    ut_d = dt_in("peer_ut", [128, 128, D])
    vt_d = dt_in("peer_vt", [128, 128, D])
    ident_d = dt_in("ident", [128, 128])
    pm_d = dt_in("pmats", [12, 128, 128])
    iota_d = dt_in("iota16", [128, 16])
    iota128_d = dt_in("iota128", [128, 128])
    y_d = nc.dram_tensor("y", [TOK, D], F32, kind="ExternalOutput").ap()
    skind = "ExternalOutput" if debug else "Internal"
    h1_dram = nc.dram_tensor("h1_scr", [TOK, D], F32, kind=skind).ap()
    h2t_dram = nc.dram_tensor("h2t_scr", [128, 8, TOK], BF16, kind=skind).ap()
    g_dram = nc.dram_tensor("g_scr", [16, 128, TOK * 8], BF16, kind=skind).ap()

    with ExitStack() as stack:
        P = Prog(nc, stack)
        for _i in range(EXTRA_SEMS):
            P.new_sem("dummy%d" % _i)

        def sb(name, shape, dt=F32):
            t = stack.enter_context(nc.sbuf_tensor("sb_" + name, list(shape), dt))
            return t, Res(name)

        def ps(name, shape, dt=F32):
            t = stack.enter_context(nc.psum_tensor("ps_" + name, list(shape), dt))
            return t, Res(name)

        poolw_bf, R_poolw = sb("poolw_bf", [128, 4, 128], BF16)
        wst_bf, R_wst = sb("wst_bf", [128, 4, 128], BF16)
        ident_f, R_identf = sb("ident_f", [128, 128], F32)
        ident_bf, R_ident = sb("ident_bf", [128, 128], BF16)
        pm_bf, R_pm = sb("pm_bf", [128, 12, 128], BF16)
        g1col, R_g1col = sb("g1col", [128, 8])
        pscale, R_pscale = sb("pscale", [128, 4])
        iota16, R_iota = sb("iota16", [128, 16])
        iota128, R_iota128 = sb("iota128", [128, 128])
        g2_bc, R_g2 = sb("g2_bc", [128, D])
        fg_bc, R_fg = sb("fg_bc", [128, D])
        sgug_bc, R_sgug = sb("sgug_bc", [128, 512])
        b_bc, R_bbc = sb("b_bc", [128, 512])
        kt = [sb("kt%d" % i, [128, 128]) for i in range(2)]
        small, R_small = sb("small", [128, 32])
        st6, R_st6 = sb("st6", [128, 8])
        zsum, R_zsum = sb("zsum", [128, 16])

        arena = stack.enter_context(nc.sbuf_tensor("sb_arena", [128, ARENA_COLS], F32))
        bump = [0]

        def alloc(name, cols, dt=F32):
            f32cols = cols if dt in (F32, U32, I32) else (cols + 1) // 2
            o = bump[0]
            bump[0] += f32cols
            assert bump[0] <= ARENA_COLS, (name, bump[0])
            a = arena[:, o:o + f32cols]
            if dt != F32:
                a = a.bitcast(dt)
            return a, Res(name)

        w_in_bf, R_w_in = alloc("w_in_bf", 8 * D_IN, BF16)
        w_out_bf, R_w_out = alloc("w_out_bf", 8 * D, BF16)
        wk_bf, R_wk = alloc("wk_bf", 8 * 2048, BF16)
        w_in_v = w_in_bf.rearrange("p (k c) -> p k c", k=8)
        w_out_v = w_out_bf.rearrange("p (k c) -> p k c", k=8)
        wk_v = wk_bf.rearrange("p (k c) -> p k c", k=8)
        ring, _ = alloc("ring", NS * 1024)
        R_slot = [Res("slot%d" % i) for i in range(NS)]
        S_slot = [P.new_sem("s_slot%d" % i) for i in range(max(NS, NS2))]
        xbuf = [alloc("xbuf%d" % i, D) for i in range(2)]
        S_x = [P.new_sem("s_x%d" % i) for i in range(2)]
        xn_bf, R_xn = alloc("xn_bf", D, BF16)
        xnT, R_xnT = alloc("xnT", 8 * 128, BF16)
        xnT_v = xnT.rearrange("p (k t) -> p k t", k=8)
        zp_sb = [alloc("zp_sb%d" % i, 512, BF16) for i in range(2)]
        d_sb, R_d = alloc("d_sb", 512, BF16)
        ab_sb, R_ab = alloc("ab_sb", 8 * 128, BF16)
        ab_v = ab_sb.rearrange("p (k t) -> p k t", k=8)
        u_sb, R_u = alloc("u_sb", 512)
        v_f, R_v = alloc("v_f", 512)
        vn_bf, R_vn = alloc("vn_bf", 512, BF16)
        h1, R_h1 = alloc("h1", D)
        junk, R_junk = alloc("junk", D)
        scores_b = [alloc("scores%d" % i, 2048) for i in range(2)]
        top_s, R_tops = alloc("top_s", 256)
        top_i, R_topi = alloc("top_i", 256, U32)
        top_if, R_topif = alloc("top_if", 256)
        stmp, R_stmp = alloc("stmp", 256)
        best_s, R_best = alloc("best_s", 128)
        pos_u, R_pos = alloc("pos_u", 128, U32)
        hi_u, R_hiu = alloc("hi_u", 128, U32)
        lo_u, R_lou = alloc("lo_u", 128, U32)
        hif, R_hif = alloc("hif", 128)
        lof, R_lof = alloc("lof", 128)
        i1, R_i1 = alloc("i1", 128)
        i2, R_i2 = alloc("i2", 128)
        ework, R_ework = alloc("ework", 128)
        gate, R_gate = alloc("gate", 128)
        aT_sb, R_aT = alloc("aT_sb", 3 * 128)
        aT_v = aT_sb.rearrange("p (q t) -> p q t", q=3)
        Pbuf = [alloc("Pb%d" % i, TB * 128, BF16) for i in range(2)]
        Qbuf = [alloc("Qb%d" % i, TB * 128, BF16) for i in range(1)]
        Gst = [alloc("Gst%d" % i, 16 * TB * 8, BF16) for i in range(2)]
        S_gst = [P.new_sem("s_gst%d" % i) for i in range(2)]
        S_h1 = P.new_sem("s_h1")
        S_xnT = P.new_sem("s_xnT")
        S_c = {}

        def csem(name):
            S_c[name] = P.new_sem("s_" + name)
            return S_c[name]

        ptr, R_ptr = ps("ptr", [128, 8, 128], BF16)
        pAB, _ = ps("pAB", [128, 1024])
        pCD, _ = ps("pCD", [128, 1024])
        pGs, _ = ps("pGs", [128, 1024])
        pT, R_pT = ps("pT", [128, 512])
        pA, pB, pC, pD = pAB[:, 0:512], pAB[:, 512:1024], pCD[:, 0:512], pCD[:, 512:1024]
        R_pA, R_pB, R_pC, R_pD = [Res("pb%d" % i) for i in range(4)]
        pGsb = [pGs[:, 0:512], pGs[:, 512:1024]]
        R_pGs = [Res("pGs%d" % i) for i in range(2)]

        def slot(i, n=1024):
            return ring[:, i * 1024: i * 1024 + n]

        def load_const(dst, R, src, name):
            s = csem(name)
            P.emit("sp", lambda e: e.dma_start(out=dst, in_=src), writes=[R], dma_sem=s)

        load_const(ident_f[:], R_identf, ident_d, "ident")
        load_const(g1col[:], R_g1col, g1col_d, "g1col")
        load_const(pscale[:], R_pscale, pscale_d, "pscale")
        load_const(iota16[:], R_iota, iota_d, "iota")
        load_const(iota128[:], R_iota128, iota128_d, "iota128")
        load_const(g2_bc[:], R_g2, g2_d.partition_broadcast(128), "g2")
        load_const(fg_bc[:], R_fg, fg_d.partition_broadcast(128), "fg")
        load_const(sgug_bc[:], R_sgug, sgug_d.partition_broadcast(128), "sgug")
        load_const(b_bc[:], R_bbc, sgub_d.partition_broadcast(128), "bbc")
        P.emit("dve", lambda e: e.tensor_copy(out=ident_bf[:], in_=ident_f[:]), reads=[R_identf], writes=[R_ident])

        stage_ctr = [0]

        def stage_load(src_ap, ncols, view=None):
            i = stage_ctr[0] % NS
            stage_ctr[0] += 1
            dst = slot(i, ncols)
            if view is not None:
                dst = view(dst)
            P.emit("sp", lambda e: e.dma_start(out=dst, in_=src_ap), writes=[R_slot[i]], dma_sem=S_slot[i])
            return i

        cast_ctr = [0]

        def cast(dst, src, reads, writes, scale_ap=None):
            k = cast_ctr[0]
            cast_ctr[0] += 1
            if k % 2 == 0:
                if scale_ap is None:
                    P.emit("dve", lambda e: e.tensor_copy(out=dst, in_=src), reads=reads, writes=writes)
                else:
                    P.emit("dve", lambda e: e.tensor_scalar(out=dst, in0=src, scalar1=scale_ap, scalar2=None, op0=ALU.mult),
                           reads=reads, writes=writes)
            else:
                if scale_ap is None:
                    P.emit("act", lambda e: e.activation(out=dst, in_=src, func=AF.Copy), reads=reads, writes=writes)
                else:
                    P.emit("act", lambda e: e.activation(out=dst, in_=src, func=AF.Copy, scale=scale_ap), reads=reads, writes=writes)

        for half in range(2):
            i = stage_load(pm_d[half * 6:(half + 1) * 6].rearrange("m a b -> a m b"), 768,
                           view=lambda a: a.rearrange("p (m b) -> p m b", m=6))
            cast(pm_bf[:, half * 6:(half + 1) * 6, :], slot(i, 768).rearrange("p (m b) -> p m b", m=6),
                 [R_slot[i]], [R_pm])
        for kc in range(8):
            for (c0, c1) in ((0, 1024), (1024, 1536)):
                i = stage_load(w_in_d[kc * 128:(kc + 1) * 128, c0:c1], c1 - c0)
                cast(w_in_v[:, kc, c0:c1], slot(i, c1 - c0), [R_slot[i], R_g1col], [R_w_in], scale_ap=g1col[:, kc:kc + 1])
        for kc in range(8):
            i = stage_load(w_out_d[kc * 128:(kc + 1) * 128, :], 1024)
            cast(w_out_v[:, kc, :], slot(i), [R_slot[i]], [R_w_out])
        i = stage_load(pool_w_d.rearrange("g c d -> c g d"), 512, view=lambda a: a.rearrange("p (g d) -> p g d", g=4))
        cast(poolw_bf[:], slot(i, 512).rearrange("p (g d) -> p g d", g=4), [R_slot[i]], [R_poolw])
        i = stage_load(wst_d.rearrange("h j i -> j h i"), 512, view=lambda a: a.rearrange("p (h i) -> p h i", h=4))
        cast(wst_bf[:], slot(i, 512).rearrange("p (h i) -> p h i", h=4), [R_slot[i]], [R_wst])
        P.emit("dve", lambda e: e.memset(wst_bf[64:128, :, 0:64], 0.0), writes=[R_wst])
        for hp in range(16):
            i = stage_load(wqt_d[hp], 1024)
            ktile, R_kt = kt[hp % 2]
            if hp < 2:
                csem("kt%d" % hp)
            s_kt = S_c["kt%d" % (hp % 2)]
            P.emit("sp", lambda e, ktile=ktile, hp=hp: e.dma_start(out=ktile[:], in_=keyst_d[hp]), writes=[R_kt], dma_sem=s_kt)
            banks = ((pA, R_pA), (pB, R_pB)) if hp % 2 == 0 else ((pC, R_pC), (pD, R_pD))
            for b in range(2):
                pb_t, R_pb = banks[b]
                for q in range(4):
                    kc = b * 4 + q
                    P.emit("pe", lambda e, pb_t=pb_t, q=q, i=i, kc=kc, ktile=ktile: e.matmul(
                        pb_t[:, q * 128:(q + 1) * 128], lhsT=slot(i)[:, kc * 128:(kc + 1) * 128], rhs=ktile[:],
                        start=True, stop=True), reads=[R_slot[i], R_kt], writes=[R_pb], inc_sem=(q == 3))
                cast(wk_v[:, b * 4:(b + 1) * 4, hp * 128:(hp + 1) * 128],
                     pb_t.rearrange("p (q k) -> p q k", q=4), [R_pb], [R_wk])

        def load_x(n):
            xt, R_x = xbuf[n % 2]
            P.emit("sp", lambda e: e.dma_start(out=xt, in_=x_d[n * 128:(n + 1) * 128, :]), writes=[R_x], dma_sem=S_x[n % 2])

        def rms_stats(src, R_src, jk, R_jk, col):
            P.emit("act", lambda e: e.activation(out=jk, in_=src, func=AF.Square, accum_out=small[:, col:col + 1]),
                   reads=[R_src], writes=[R_jk, R_small])
            P.emit("act", lambda e: e.activation(out=small[:, col + 1:col + 2], in_=small[:, col:col + 1], func=AF.Sqrt,
                                                 scale=1.0 / D, bias=EPS), reads=[R_small], writes=[R_small])
            P.emit("dve", lambda e: e.reciprocal(out=small[:, col + 2:col + 3], in_=small[:, col + 1:col + 2]),
                   reads=[R_small], writes=[R_small])
            return small[:, col + 2:col + 3]

        def transposes(src_bf, R_src, dstT, R_dst):
            for kc in range(8):
                P.emit("pe", lambda e, kc=kc: e.transpose(ptr[:, kc, :], src_bf[:, kc * 128:(kc + 1) * 128], ident_bf[:]),
                       reads=[R_src, R_ident], writes=[R_ptr], inc_sem=(kc == 7))
            P.emit("act", lambda e: e.activation(out=dstT, in_=ptr[:], func=AF.Copy), reads=[R_ptr], writes=[R_dst])

        def stage_A(n):
            xt, R_x = xbuf[n % 2]
            first = (n % TILES_PER_SEQ == 0)
            zp_cur, R_zpc = zp_sb[n % 2]
            zp_prev, R_zpp = zp_sb[(n + 1) % 2]

            rstd1 = rms_stats(xt, R_x, xn_bf, R_xn, 0)
            yield
            P.emit("act", lambda e: e.activation(out=xn_bf, in_=xt, func=AF.Copy, scale=rstd1),
                   reads=[R_x, R_small], writes=[R_xn])
            yield
            transposes(xn_bf, R_xn, xnT_v, R_xnT)
            for kc in range(8):
                P.emit("pe", lambda e, kc=kc: e.matmul(pA, lhsT=xnT_v[:, kc, :], rhs=w_in_v[:, kc, 0:512],
                                                       start=(kc == 0), stop=(kc == 7)),
                       reads=[R_xnT, R_w_in], writes=[R_pA], inc_sem=(kc == 7))
            for kc in range(8):
                P.emit("pe", lambda e, kc=kc: e.matmul(pB, lhsT=xnT_v[:, kc, :], rhs=w_in_v[:, kc, 1024:1536],
                                                       start=(kc == 0), stop=(kc == 7)),
                       reads=[R_xnT, R_w_in], writes=[R_pB], inc_sem=(kc == 7))
            for m in range(4):
                for kc in range(8):
                    P.emit("pe", lambda e, kc=kc, m=m: e.matmul(pC[:, m * 128:(m + 1) * 128],
                                                                lhsT=w_in_v[:, kc, 512 + m * 128:512 + (m + 1) * 128],
                                                                rhs=xnT_v[:, kc, :], start=(kc == 0), stop=(kc == 7)),
                           reads=[R_xnT, R_w_in], writes=[R_pC], inc_sem=(kc == 7 and m == 3))
            yield
            P.emit("act", lambda e: e.activation(out=zp_cur, in_=pA, func=AF.Copy), reads=[R_pA], writes=[R_zpc])
            for g in range(4):
                mi = (8 + g) if first else g
                P.emit("pe", lambda e, g=g, mi=mi: e.matmul(
                    pD[:, g * 128:(g + 1) * 128], lhsT=zp_cur[:, g * 128:(g + 1) * 128], rhs=pm_bf[:, mi, :],
                    start=True, stop=first), reads=[R_zpc, R_pm], writes=[R_pD], inc_sem=(first and g == 3))
                if not first:
                    P.emit("pe", lambda e, g=g: e.matmul(
                        pD[:, g * 128:(g + 1) * 128], lhsT=zp_prev[:, g * 128:(g + 1) * 128], rhs=pm_bf[:, 4 + g, :],
                        start=False, stop=True), reads=[R_zpp, R_pm], writes=[R_pD], inc_sem=(g == 3))
            yield
            P.emit("dve", lambda e: e.tensor_copy(out=d_sb, in_=pD), reads=[R_pD], writes=[R_d])
            for g in range(4):
                P.emit("pe", lambda e, g=g: e.matmul(pA[:, g * 128:(g + 1) * 128], lhsT=poolw_bf[:, g, :],
                                                     rhs=d_sb[:, g * 128:(g + 1) * 128], start=True, stop=True),
                       reads=[R_poolw, R_d], writes=[R_pA], inc_sem=(g == 3))
            P.emit("dve", lambda e: e.tensor_tensor(out=ab_v[:, 0:4, :], in0=pA.rearrange("p (g t) -> p g t", g=4),
                                                    in1=bc_ap(pscale[:], [[1, 4], [0, 128]]), op=ALU.mult),
                   reads=[R_pA, R_pscale], writes=[R_ab])
            yield
            P.emit("act", lambda e: e.activation(out=u_sb, in_=pC, func=AF.Gelu_apprx_tanh), reads=[R_pC], writes=[R_u])
            P.emit("act", lambda e: e.activation(out=v_f, in_=pB, func=AF.Gelu_apprx_tanh), reads=[R_pB], writes=[R_v])
            yield
            P.emit("dve", lambda e: e.bn_stats(out=st6[:, 0:6], in_=v_f), reads=[R_v], writes=[R_st6])
            P.emit("dve", lambda e: e.bn_aggr(out=st6[:, 6:8], in_=st6[:, 0:6]), reads=[R_st6], writes=[R_st6])
            P.emit("act", lambda e: e.activation(out=small[:, 8:9], in_=st6[:, 7:8], func=AF.Sqrt, bias=EPS),
                   reads=[R_st6], writes=[R_small])
            yield
            P.emit("dve", lambda e: e.reciprocal(out=small[:, 9:10], in_=small[:, 8:9]), reads=[R_small], writes=[R_small])
            P.emit("dve", lambda e: e.tensor_scalar(out=v_f, in0=v_f, scalar1=st6[:, 6:7], scalar2=small[:, 9:10],
                                                    op0=ALU.subtract, op1=ALU.mult),
                   reads=[R_v, R_st6, R_small], writes=[R_v])
            P.emit("dve", lambda e: e.tensor_tensor(out=vn_bf, in0=v_f, in1=sgug_bc[:], op=ALU.mult),
                   reads=[R_v, R_sgug], writes=[R_vn])
            yield
            for h in range(4):
                P.emit("pe", lambda e, h=h: e.matmul(pB[:, h * 128:(h + 1) * 128], lhsT=vn_bf[:, h * 128:(h + 1) * 128],
                                                     rhs=wst_bf[:, h, :], start=True, stop=True),
                       reads=[R_vn, R_wst], writes=[R_pB], inc_sem=(h == 3))
            yield
            P.emit("dve", lambda e: e.tensor_tensor(out=v_f, in0=pB, in1=b_bc[:], op=ALU.add),
                   reads=[R_pB, R_bbc], writes=[R_v])
            P.emit("dve", lambda e: e.tensor_tensor(out=ab_v[:, 4:8, :], in0=v_f.rearrange("p (h t) -> p h t", h=4),
                                                    in1=u_sb.rearrange("p (h t) -> p h t", h=4), op=ALU.mult),
                   reads=[R_v, R_u], writes=[R_ab])
            yield
            for half, (pO_, R_pO_) in enumerate(((pC, R_pC), (pD, R_pD))):
                for fc in range(8):
                    P.emit("pe", lambda e, fc=fc, half=half, pO_=pO_: e.matmul(
                        pO_, lhsT=ab_v[:, fc, :], rhs=w_out_v[:, fc, half * 512:(half + 1) * 512],
                        start=(fc == 0), stop=(fc == 7)), reads=[R_ab, R_w_out], writes=[R_pO_], inc_sem=(fc == 7))
                P.emit("dve", lambda e, half=half, pO_=pO_: e.tensor_tensor(
                    out=h1[:, half * 512:(half + 1) * 512], in0=pO_, in1=xt[:, half * 512:(half + 1) * 512], op=ALU.add),
                    reads=[R_pO_, R_x], writes=[R_h1])
            yield
            P.emit("sp", lambda e: e.dma_start(out=h1_dram[n * 128:(n + 1) * 128, :], in_=h1), reads=[R_h1], dma_sem=S_h1)
            if n + 2 < NT:
                load_x(n + 2)
            rstd2 = rms_stats(h1, R_h1, junk, R_junk, 3)
            yield
            P.emit("dve", lambda e: e.scalar_tensor_tensor(out=xn_bf, in0=h1, scalar=rstd2, in1=g2_bc[:],
                                                           op0=ALU.mult, op1=ALU.mult),
                   reads=[R_h1, R_small, R_g2], writes=[R_xn])
            transposes(xn_bf, R_xn, xnT_v, R_xnT)
            P.emit("sp", lambda e: e.dma_start(out=h2t_dram[:, :, n * 128:(n + 1) * 128], in_=xnT_v), reads=[R_xnT], dma_sem=S_xnT)
            yield
            sbanks = ((pA, R_pA), (pB, R_pB), (pC, R_pC), (pD, R_pD))
            scores, R_sc = scores_b[n % 2]
            for c, (pb_t, R_pb) in enumerate(sbanks):
                for kc in range(8):
                    P.emit("pe", lambda e, kc=kc, c=c, pb_t=pb_t: e.matmul(
                        pb_t, lhsT=xnT_v[:, kc, :], rhs=wk_v[:, kc, c * 512:(c + 1) * 512],
                        start=(kc == 0), stop=(kc == 7)), reads=[R_xnT, R_wk], writes=[R_pb], inc_sem=(kc == 7))
                P.emit("act", lambda e, c=c, pb_t=pb_t: e.activation(out=scores[:, c * 512:(c + 1) * 512], in_=pb_t, func=AF.Copy),
                       reads=[R_pb], writes=[R_sc])

        gs_ctr = [0]

        def stage_B(n):
            scores, R_sc = scores_b[n % 2]
            for g in range(16):
                sg = scores[:, g * 128:(g + 1) * 128]
                o = g * 16
                P.emit("dve", lambda e, sg=sg, o=o: e.max(out=top_s[:, o:o + 8], in_=sg), reads=[R_sc], writes=[R_tops])
                P.emit("dve", lambda e, sg=sg, o=o: e.max_index(out=top_i[:, o:o + 8], in_max=top_s[:, o:o + 8], in_values=sg),
                       reads=[R_sc, R_tops], writes=[R_topi])
                P.emit("dve", lambda e, sg=sg, o=o: e.match_replace(out=stmp[:, 0:128], in_to_replace=top_s[:, o:o + 8],
                                                                    in_values=sg, imm_value=NEG),
                       reads=[R_sc, R_tops], writes=[R_stmp])
                P.emit("dve", lambda e, o=o: e.max(out=top_s[:, o + 8:o + 16], in_=stmp[:, 0:128]),
                       reads=[R_stmp], writes=[R_tops])
                P.emit("dve", lambda e, o=o: e.max_index(out=top_i[:, o + 8:o + 16], in_max=top_s[:, o + 8:o + 16],
                                                         in_values=stmp[:, 0:128]),
                       reads=[R_stmp, R_tops], writes=[R_topi])
                if g % 2 == 1:
                    yield
            yield
            P.emit("dve", lambda e: e.tensor_copy(out=top_if, in_=top_i), reads=[R_topi], writes=[R_topif])
            cand = ring[:, 0:2048]
            R_cand = [R_slot[0], R_slot[1]]
            P.emit("dve", lambda e: e.tensor_tensor(
                out=cand.rearrange("p (h a b) -> p h a b", h=8, a=16),
                in0=bc_ap(top_s[:, 0:16], [[32, 8], [1, 16], [0, 16]]),
                in1=bc_ap(top_s[:, 16:32], [[32, 8], [0, 16], [1, 16]]), op=ALU.add),
                reads=[R_tops], writes=R_cand)
            for h in range(8):
                ch = cand[:, h * 256:(h + 1) * 256]
                o = h * 16
                P.emit("dve", lambda e, ch=ch, o=o: e.max(out=best_s[:, o:o + 8], in_=ch), reads=R_cand, writes=[R_best])
                P.emit("dve", lambda e, ch=ch, o=o: e.max_index(out=pos_u[:, o:o + 8], in_max=best_s[:, o:o + 8], in_values=ch),
                       reads=R_cand + [R_best], writes=[R_pos])
                P.emit("dve", lambda e, ch=ch, o=o: e.match_replace(out=stmp, in_to_replace=best_s[:, o:o + 8],
                                                                    in_values=ch, imm_value=NEG),
                       reads=R_cand + [R_best], writes=[R_stmp])
                P.emit("dve", lambda e, o=o: e.max(out=best_s[:, o + 8:o + 16], in_=stmp), reads=[R_stmp], writes=[R_best])
                P.emit("dve", lambda e, o=o: e.max_index(out=pos_u[:, o + 8:o + 16], in_max=best_s[:, o + 8:o + 16],
                                                         in_values=stmp),
                       reads=[R_stmp, R_best], writes=[R_pos])
                if h % 2 == 1:
                    yield
            yield
            P.emit("dve", lambda e: e.tensor_single_scalar(out=hi_u, in_=pos_u, scalar=4, op=ALU.logical_shift_right),
                   reads=[R_pos], writes=[R_hiu])
            P.emit("dve", lambda e: e.tensor_single_scalar(out=lo_u, in_=pos_u, scalar=15, op=ALU.bitwise_and),
                   reads=[R_pos], writes=[R_lou])
            P.emit("dve", lambda e: e.tensor_copy(out=hif, in_=hi_u), reads=[R_hiu], writes=[R_hif])
            P.emit("dve", lambda e: e.tensor_copy(out=lof, in_=lo_u), reads=[R_lou], writes=[R_lof])
            wk = ring[:, 2048:4096]
            R_wkk = [R_slot[2], R_slot[3]]
            for (sel, off, dst, R_dst, R_sel) in ((hif, 0, i1, R_i1, R_hif), (lof, 16, i2, R_i2, R_lof)):
                P.emit("dve", lambda e, sel=sel: e.tensor_tensor(
                    out=wk.rearrange("p (k a) -> p k a", a=16),
                    in0=bc_ap(iota16[:], [[0, 128], [1, 16]]),
                    in1=bc_ap(sel, [[1, 128], [0, 16]]), op=ALU.is_equal),
                    reads=[R_iota, R_sel], writes=R_wkk)
                P.emit("dve", lambda e, off=off: e.tensor_tensor(
                    out=wk.rearrange("p (h k a) -> p h k a", h=8, k=16),
                    in0=wk.rearrange("p (h k a) -> p h k a", h=8, k=16),
                    in1=bc_ap(top_if[:, off:off + 16], [[32, 8], [0, 16], [1, 16]]), op=ALU.mult),
                    reads=R_wkk + [R_topif], writes=R_wkk)
                P.emit("dve", lambda e, dst=dst: e.tensor_reduce(
                    out=dst, in_=wk.rearrange("p (k a) -> p k a", a=16), axis=AX.X, op=ALU.add),
                    reads=R_wkk, writes=[R_dst])
            yield
            P.emit("dve", lambda e: e.tensor_tensor(out=ework.rearrange("p (h k) -> p h k", h=8),
                                                    in0=best_s.rearrange("p (h k) -> p h k", h=8),
                                                    in1=bc_ap(best_s, [[16, 8], [0, 16]]), op=ALU.subtract),
                   reads=[R_best], writes=[R_ework])
            P.emit("act", lambda e: e.activation(out=ework, in_=ework, func=AF.Exp), reads=[R_ework], writes=[R_ework])
            yield
            P.emit("dve", lambda e: e.tensor_reduce(out=zsum[:, 0:8], in_=ework.rearrange("p (h k) -> p h k", h=8),
                                                    axis=AX.X, op=ALU.add), reads=[R_ework], writes=[R_zsum])
            P.emit("dve", lambda e: e.reciprocal(out=zsum[:, 8:16], in_=zsum[:, 0:8]), reads=[R_zsum], writes=[R_zsum])
            P.emit("dve", lambda e: e.tensor_tensor(out=gate.rearrange("p (h k) -> p h k", h=8),
                                                    in0=ework.rearrange("p (h k) -> p h k", h=8),
                                                    in1=bc_ap(zsum[:, 8:16], [[1, 8], [0, 16]]), op=ALU.mult),
                   reads=[R_ework, R_zsum], writes=[R_gate])
            yield
            for q, (src, R_src) in enumerate(((i1, R_i1), (i2, R_i2), (gate, R_gate))):
                P.emit("pe", lambda e, q=q, src=src: e.transpose(pT[:, q * 128:(q + 1) * 128], src, ident_f[:]),
                       reads=[R_src, R_identf], writes=[R_pT], inc_sem=(q == 2))
            P.emit("act", lambda e: e.activation(out=aT_sb, in_=pT[:, 0:384], func=AF.Copy), reads=[R_pT], writes=[R_aT])
            yield
            for blk in range(128 // TB):
                t0 = blk * TB
                Pb, R_Pb = Pbuf[blk % 2]
                Qb, R_Qb = Qbuf[0]
                gst, R_gst = Gst[blk % 2]
                s_gst = S_gst[blk % 2]
                P.emit("dve", lambda e, Pb=Pb, t0=t0: e.tensor_tensor(
                    out=Pb.rearrange("p (t i) -> p t i", t=TB),
                    in0=bc_ap(iota128[:], [[0, TB], [1, 128]]),
                    in1=bc_ap(aT_v[:, 0, t0:t0 + TB], [[1, TB], [0, 128]]), op=ALU.is_equal),
                    reads=[R_iota128, R_aT], writes=[R_Pb])
                P.emit("dve", lambda e, Qb=Qb, t0=t0: e.tensor_tensor(
                    out=Qb.rearrange("p (t i) -> p t i", t=TB),
                    in0=bc_ap(iota128[:], [[0, TB], [1, 128]]),
                    in1=bc_ap(aT_v[:, 1, t0:t0 + TB], [[1, TB], [0, 128]]), op=ALU.is_equal),
                    reads=[R_iota128, R_aT], writes=[R_Qb])
                P.emit("pool", lambda e, Pb=Pb, t0=t0: e.tensor_tensor(
                    out=Pb.rearrange("p (t i) -> p t i", t=TB),
                    in0=Pb.rearrange("p (t i) -> p t i", t=TB),
                    in1=bc_ap(aT_v[:, 2, t0:t0 + TB], [[1, TB], [0, 128]]), op=ALU.mult),
                    reads=[R_Pb, R_aT], writes=[R_Pb])
                for t4 in range(TB // 4):
                    bi = gs_ctr[0] % 2
                    gs_ctr[0] += 1
                    bank, R_bank = pGsb[bi], R_pGs[bi]
                    for tk in range(4):
                        t = t4 * 4 + tk
                        P.emit("pe", lambda e, bank=bank, tk=tk, t=t, Pb=Pb, Qb=Qb: e.matmul(
                            bank[:, tk * 128:(tk + 1) * 128], lhsT=Pb[:, t * 128:(t + 1) * 128], rhs=Qb[:, t * 128:(t + 1) * 128],
                            start=True, stop=True), reads=[R_Pb, R_Qb], writes=[R_bank], inc_sem=(tk == 3))
                    P.emit("act", lambda e, bank=bank, gst=gst, t4=t4: e.activation(
                        out=bc_ap(gst[:, t4 * 32:t4 * 32 + 1], [[8, 4], [TB * 8, 16], [1, 8]]),
                        in_=bank.rearrange("p (tk jg jj) -> p tk jg jj", tk=4, jg=16), func=AF.Copy),
                        reads=[R_bank], writes=[R_gst])
                tok = n * 128 + t0
                P.emit("sp", lambda e, gst=gst, tok=tok: e.dma_start(
                    out=g_dram[:, :, tok * 8:(tok + TB) * 8].rearrange("g p x -> p g x"),
                    in_=gst.rearrange("p (g x) -> p g x", g=16)), reads=[R_gst], dma_sem=s_gst)
                yield

        load_x(0)
        if NT > 1:
            load_x(1)
        for _ in stage_A(0):
            pass
        for n in range(NT):
            gb = stage_B(n)
            ga = stage_A(n + 1) if n + 1 < NT else iter(())
            done_a = done_b = False
            while not (done_a and done_b):
                if not done_b:
                    try:
                        next(gb)
                    except StopIteration:
                        done_b = True
                if not done_a:
                    try:
                        next(ga)
                    except StopIteration:
                        done_a = True

        P.barrier()
        bump[0] = 0
        accum, _ = alloc("accum", NTH * D)
        accum_v = accum.rearrange("p (n d) -> p n d", n=NTH)
        R_acc = [Res("acc%d" % i) for i in range(NTH)]
        h2T, R_h2T = alloc("h2T", 8 * THALF, BF16)
        h2T_v = h2T.rearrange("p (k t) -> p k t", k=8)
        wU = [alloc("wU%d" % i, 1024, BF16) for i in range(NW)]
        wV = [alloc("wV%d" % i, 1024, BF16) for i in range(NW)]
        ring2, _ = alloc("ring2", NS2 * 1024)
        R_slot2 = [Res("slot2_%d" % i) for i in range(NS2)]
        gsl = [alloc("gsl%d" % i, TG * 8, BF16) for i in range(2)]
        S_gsl = [P.new_sem("s_gsl%d" % i) for i in range(2)]
        gsb = [alloc("gsb%d" % i, TG) for i in range(2)]
        mt = [alloc("mt%d" % i, TG, BF16) for i in range(3)]
        ystage = [alloc("ystage%d" % i, D) for i in range(2)]
        S_ys = [P.new_sem("s_ys%d" % i) for i in range(2)]
        junk2, R_junk2 = alloc("junk2", D)
        S_acc = [P.new_sem("s_acc%d" % i) for i in range(4)]
        S_h2T = [P.new_sem("s_h2T%d" % i) for i in range(4)]
        R_h2Tq = [Res("h2Tq%d" % i) for i in range(4)]
        pat = [pGs[:, 0:TG], pGs[:, 512:512 + TG], pT[:, 0:TG]]
        R_pat = [Res("pat%d" % i) for i in range(3)]
        pO = [(pAB, Res("pO0")), (pCD, Res("pO1"))]

        def slot2(i):
            return ring2[:, i * 1024:(i + 1) * 1024]

        st2_ctr = [0]
        ys_ctr = [0]

        et_slots = {}

        def stage_etile(j):
            sl = []
            for src in (ut_d[j], vt_d[j]):
                i = st2_ctr[0] % NS2
                st2_ctr[0] += 1
                P.emit("sp", lambda e, i=i, src=src: e.dma_start(out=slot2(i), in_=src), writes=[R_slot2[i]], dma_sem=S_slot[i])
                sl.append(i)
            et_slots[j] = sl

        def cast_etile(j):
            ws = j % NW
            for i, (dst, R_dst) in zip(et_slots.pop(j), (wU[ws], wV[ws])):
                P.emit("act", lambda e, i=i, dst=dst: e.activation(out=dst, in_=slot2(i), func=AF.Copy),
                       reads=[R_slot2[i]], writes=[R_dst])

        def load_gsl(h, jg, tg, idx):
            b = idx % 2
            tok = h * THALF + tg * TG
            g_t, R_g = gsl[b]
            P.emit("act", lambda e: e.dma_start(out=g_t, in_=g_dram[jg, :, tok * 8:(tok + TG) * 8]), writes=[R_g], dma_sem=S_gsl[b])

        for h in range(NHALF):
            tok0 = h * THALF
            na = max(1, NTH // 4)
            for q in range(0, NTH, na):
                P.emit("sp", lambda e, tok0=tok0, q=q: e.dma_start(
                    out=accum_v[:, q:q + na, :],
                    in_=h1_dram[tok0 + q * 128:tok0 + (q + na) * 128, :].rearrange("(n p) d -> p n d", p=128)),
                    writes=R_acc[q:q + na], dma_sem=S_acc[q // na])
            for q in range(4):
                P.emit("sp", lambda e, tok0=tok0, q=q: e.dma_start(out=h2T_v[:, 2 * q:2 * q + 2, :],
                                                                 in_=h2t_dram[:, 2 * q:2 * q + 2, tok0:tok0 + THALF]),
                       writes=[R_h2Tq[q]], dma_sem=S_h2T[q])
            for j in range(8):
                stage_etile(j)
                cast_etile(j)
            its = [(jg, tg, jj) for jg in range(JGMAX) for tg in range(NG) for jj in range(8)]
            NI = len(its)
            load_gsl(h, 0, 0, 0)
            for k in range(NI + 2):
                if k < NI:
                    jg, tg, jj = its[k]
                    gidx = jg * NG + tg
                    j = jg * 8 + jj
                    ws = j % NW
                    if jj == 0:
                        if gidx + 1 < JGMAX * NG:
                            jg2, tg2 = divmod(gidx + 1, NG)
                            load_gsl(h, jg2, tg2, gidx + 1)
                    if tg == NG - 1 and jg + 1 < JGMAX:
                        stage_etile((jg + 1) * 8 + jj)
                    pt_, R_pt = pat[k % 3], R_pat[k % 3]
                    wU_t, R_wU = wU[ws]
                    for c in range(8):
                        P.emit("pe", lambda e, c=c, pt_=pt_, wU_t=wU_t, tg=tg: e.matmul(
                            pt_, lhsT=wU_t[:, c * 128:(c + 1) * 128], rhs=h2T_v[:, c, tg * TG:(tg + 1) * TG],
                            start=(c == 0), stop=(c == 7)), reads=[R_wU, R_h2Tq[c // 2]], writes=[R_pt], inc_sem=(c == 7))
                    g_t, R_g = gsb[k % 2]
                    P.emit("act", lambda e, pt_=pt_, g_t=g_t: e.activation(out=g_t, in_=pt_, func=AF.Gelu_apprx_tanh),
                           reads=[R_pt], writes=[R_g])
                    m_t, R_m = mt[k % 3]
                    gs_t, R_gs = gsl[gidx % 2]
                    P.emit("pool", lambda e, m_t=m_t, g_t=g_t, gs_t=gs_t, jj=jj: e.tensor_tensor(
                        out=m_t, in0=g_t, in1=bc_ap(gs_t[:, jj:jj + 1], [[8, TG]]), op=ALU.mult),
                        reads=[R_g, R_gs], writes=[R_m])
                if k >= 2:
                    jg, tg, jj = its[k - 2]
                    j = jg * 8 + jj
                    ws = j % NW
                    m_t, R_m = mt[(k - 2) % 3]
                    wV_t, R_wV = wV[ws]
                    for ti in range(TG // 128):
                        pO_t, R_pO = pO[ti]
                        for hf in range(2):
                            P.emit("pe", lambda e, pO_t=pO_t, hf=hf, ti=ti, m_t=m_t, wV_t=wV_t, jj=jj: e.matmul(
                                pO_t[:, hf * 512:(hf + 1) * 512], lhsT=m_t[:, ti * 128:(ti + 1) * 128],
                                rhs=wV_t[:, hf * 512:(hf + 1) * 512], start=(jj == 0), stop=(jj == 7)),
                                reads=[R_m, R_wV], writes=[R_pO], inc_sem=(ti == TG // 128 - 1 and hf == 1))
                    if tg == NG - 1 and jg + 1 < JGMAX:
                        cast_etile((jg + 1) * 8 + jj)
                    if jj == 7:
                        for ti in range(TG // 128):
                            pO_t, R_pO = pO[ti]
                            a = tg * (TG // 128) + ti
                            P.emit("dve", lambda e, pO_t=pO_t, a=a: e.tensor_tensor(
                                out=accum_v[:, a, :], in0=pO_t[:], in1=accum_v[:, a, :], op=ALU.add),
                                reads=[R_pO, R_acc[a]], writes=[R_acc[a]])
            for a in range(NTH):
                n = h * NTH + a
                yb = ys_ctr[0] % 2
                ys_ctr[0] += 1
                y_t, R_y = ystage[yb]
                rstd3 = rms_stats(accum_v[:, a, :], R_acc[a], junk2, R_junk2, 6)
                P.emit("dve", lambda e, a=a, y_t=y_t, rstd3=rstd3: e.scalar_tensor_tensor(
                    out=y_t, in0=accum_v[:, a, :], scalar=rstd3, in1=fg_bc[:], op0=ALU.mult, op1=ALU.mult),
                    reads=[R_acc[a], R_small, R_fg], writes=[R_y])
                P.emit("sp", lambda e, n=n, y_t=y_t: e.dma_start(out=y_d[n * 128:(n + 1) * 128, :], in_=y_t),
                       reads=[R_y], dma_sem=S_ys[yb])

        P.final_wait("sp", S_ys)

        with nc.Block() as block:
            @block.sync
            def _(e):
                for f in P.ops["sp"]:
                    f(e)

            @block.tensor
            def _(e):
                for f in P.ops["pe"]:
                    f(e)

            @block.vector
            def _(e):
                for f in P.ops["dve"]:
                    f(e)

            @block.scalar
            def _(e):
                for f in P.ops["act"]:
                    f(e)

            @block.gpsimd
            def _(e):
                for f in P.ops["pool"]:
                    f(e)
    return nc


def _pool_mats():
    m = np.zeros((12, 128, 128), np.float32)
    tp = np.arange(128)[:, None]
    t = np.arange(128)[None, :]
    for g, w in enumerate(WINDOWS):
        band = ((tp <= t) & (tp > t - w)).astype(np.float32)
        m[g] = band / w - (tp == t)
        m[4 + g] = ((tp - 128) > (t - w)).astype(np.float32) / w
        cnt = np.minimum(t + 1, w).astype(np.float32)
        m[8 + g] = band / cnt - (tp == t)
    return m


def host_layout(inp):
    f = lambda a: np.ascontiguousarray(np.asarray(a, dtype=np.float32))
    wq = f(inp["peer_wq"])[0]
    keys = f(inp["peer_keys"])[0]
    pu = f(inp["peer_u"])[0]
    pv = f(inp["peer_v"])[0]
    return {
        "w_in": f(inp["w_in"])[0],
        "w_out": f(inp["w_out"])[0],
        "pool_w": f(inp["pool_w"])[0],
        "sgu_wt": f(np.transpose(f(inp["sgu_w"])[0], (0, 2, 1))),
        "sgu_b": f(inp["sgu_b"])[0].reshape(512),
        "sgu_g": f(inp["sgu_norm_g"])[0].reshape(512),
        "g1col": f(f(inp["norm1_g"])[0].reshape(8, 128).T),
        "pscale": f(f(inp["pool_scale"])[0].reshape(4, 128).T),
        "g2": f(inp["norm2_g"])[0].reshape(D),
        "fg": f(inp["final_g"]).reshape(D),
        "wq_t": f(wq.reshape(D, 16, 128).transpose(1, 2, 0)),
        "keys_t": f(keys.transpose(1, 0, 3, 2).reshape(16, 128, 128)),
        "peer_ut": f(pu.reshape(128, 128, 8, 128).transpose(1, 3, 2, 0)).reshape(128, 128, D),
        "peer_vt": f(pv.reshape(128, 128, D).transpose(1, 0, 2)),
        "ident": np.eye(128, dtype=np.float32),
        "pmats": _pool_mats(),
        "iota16": f(np.broadcast_to(np.arange(16, dtype=np.float32), (128, 16))),
        "iota128": f(np.broadcast_to(np.arange(128, dtype=np.float32), (128, 128))),
    }


_NC_CACHE = {}


def kernel(x, norm1_g, w_in, pool_w, pool_scale, sgu_norm_g, sgu_w, sgu_b, w_out, norm2_g,
           peer_wq, peer_keys, peer_u, peer_v, final_g):
    x = np.ascontiguousarray(np.asarray(x, dtype=np.float32))
    B = x.shape[0]
    xs = x.reshape(NCORES, (B // NCORES) * SEQ, D)
    shared = host_layout(dict(norm1_g=norm1_g, w_in=w_in, pool_w=pool_w, pool_scale=pool_scale, sgu_norm_g=sgu_norm_g,
                              sgu_w=sgu_w, sgu_b=sgu_b, w_out=w_out, norm2_g=norm2_g, peer_wq=peer_wq,
                              peer_keys=peer_keys, peer_u=peer_u, peer_v=peer_v, final_g=final_g))
    if "nc" not in _NC_CACHE:
        _NC_CACHE["nc"] = build_program()
    nc = _NC_CACHE["nc"]
    in_maps = [dict(shared, x=np.ascontiguousarray(xs[c])) for c in range(NCORES)]
    res = run_bass_kernel_spmd(nc, in_maps, core_ids=list(range(NCORES)))
    out = np.stack([res.results[c]["y"] for c in range(NCORES)], axis=0)
    return out.reshape(B, SEQ, D).astype(np.float32)
```

```python
from contextlib import ExitStack
import numpy as np
import concourse.bass as bass
import concourse.mybir as mybir
from concourse.bass_utils import run_bass_kernel_spmd

F32 = mybir.dt.float32
BF16 = mybir.dt.bfloat16
U32 = mybir.dt.uint32
I32 = mybir.dt.int32
ALU = mybir.AluOpType
AF = mybir.ActivationFunctionType
AX = mybir.AxisListType

D = 1024
SEQ = 2048
NCORES = 8
TOK_PER_CORE = 2 * SEQ
NT = TOK_PER_CORE // 128
TILES_PER_SEQ = SEQ // 128
D_IN = 1536
NEXP = 16384
EPS = 1e-6
WINDOWS = (2, 4, 8, 16)
NS = 4
NS2 = 8
TB = 32
TG = 256
NW = 8
NEG = -1.0e30
ARENA_COLS = 47400
SEM_LIMIT = 12000
JGMAX = 16
EXTRA_SEMS = 0


class Sem:
    def __init__(self, h):
        self.h = h
        self.n = 0


class Res:
    def __init__(self, name):
        self.name = name
        self.writers = {}
        self.readers = {}


class Prog:
    def __init__(self, nc, stack):
        self.nc = nc
        self.stack = stack
        self.all_sems = []
        self.ops = {k: [] for k in ("pe", "dve", "act", "pool", "sp")}
        self.esem = {k: self.new_sem("e_" + k) for k in self.ops}
        self.pe_sems = {self.esem["pe"]}
        self.epoch = {k: 0 for k in self.ops}
        self.pending = {k: False for k in self.ops}
        self.waited = {k: {} for k in self.ops}

    def new_sem(self, name):
        s = Sem(self.stack.enter_context(self.nc.semaphore(name)))
        self.all_sems.append(s)
        return s

    def emit(self, eng, fn, reads=(), writes=(), dma_sem=None, inc_sem=True):
        if dma_sem is None and self.esem[eng].n >= SEM_LIMIT and not self.pending[eng]:
            self.epoch[eng] += 1
            self.esem[eng] = self.new_sem("e_%s_%d" % (eng, self.epoch[eng]))
            if eng == "pe":
                self.pe_sems.add(self.esem[eng])
        mysem = dma_sem if dma_sem is not None else self.esem[eng]
        inc = 16 if dma_sem is not None else 1
        deps = {}
        for r in reads:
            for s, v in r.writers.items():
                deps[s] = max(deps.get(s, 0), v)
        for w in writes:
            for s, v in w.writers.items():
                deps[s] = max(deps.get(s, 0), v)
            for s, v in w.readers.items():
                deps[s] = max(deps.get(s, 0), v)
        waits = []
        for s, v in deps.items():
            if eng == "pe" and s in self.pe_sems and dma_sem is None:
                continue
            if dma_sem is not None and s is dma_sem:
                continue
            if self.waited[eng].get(s, 0) >= v:
                continue
            self.waited[eng][s] = v
            waits.append((s.h, v))
        if inc_sem:
            mysem.n += inc
            val = mysem.n
            if dma_sem is None:
                self.pending[eng] = False
        else:
            assert dma_sem is None
            val = mysem.n + inc
            self.pending[eng] = True
        for w in writes:
            w.writers = {mysem: val}
            w.readers = {}
        for r in reads:
            if r not in writes:
                r.readers[mysem] = val
        h = mysem.h

        def run(e, waits=waits, fn=fn, h=h, inc=inc, inc_sem=inc_sem):
            for (sh, v) in waits:
                e.wait_ge(sh, v)
            if inc_sem:
                fn(e).then_inc(h, inc)
            else:
                fn(e)

        self.ops[eng].append(run)

    def barrier(self):
        snap = [(s, s.n) for s in self.all_sems if s.n > 0]
        for eng in self.ops:
            lst = [(s.h, v) for (s, v) in snap if self.waited[eng].get(s, 0) < v]
            for (s, v) in snap:
                self.waited[eng][s] = v

            def run(e, lst=lst):
                for (sh, v) in lst:
                    e.wait_ge(sh, v)

            self.ops[eng].append(run)

    def final_wait(self, eng, sems):
        lst = [(s.h, s.n) for s in sems if s.n > 0]

        def run(e, lst=lst):
            for (sh, v) in lst:
                e.wait_ge(sh, v)

        self.ops[eng].append(run)


def bc_ap(ap, dims):
    return bass.AP(ap.tensor, ap.offset, [list(ap.ap[0])] + [list(d) for d in dims])


def build_program(NT=NT, NHALF=2, debug=False):
    TOK = NT * 128
    NTH = NT // NHALF
    THALF = NTH * 128
    NG = THALF // TG
    nc = bass.Bass("TRN2", target_bir_lowering=False)
    dt_in = lambda name, shape, dt=F32: nc.dram_tensor(name, list(shape), dt, kind="ExternalInput").ap()
    x_d = dt_in("x", [TOK, D])
    w_in_d = dt_in("w_in", [D, D_IN])
    w_out_d = dt_in("w_out", [D, D])
    pool_w_d = dt_in("pool_w", [4, 128, 128])
    wst_d = dt_in("sgu_wt", [4, 128, 128])
    sgub_d = dt_in("sgu_b", [512])
    sgug_d = dt_in("sgu_g", [512])
    g1col_d = dt_in("g1col", [128, 8])
    pscale_d = dt_in("pscale", [128, 4])
    g2_d = dt_in("g2", [D])
    fg_d = dt_in("fg", [D])
    wqt_d = dt_in("wq_t", [16, 128, D])
    keyst_d = dt_in("keys_t", [16, 128, 128])
    ut_d = dt_in("peer_ut", [128, 128, D])
    vt_d = dt_in("peer_vt", [128, 128, D])
    ident_d = dt_in("ident", [128, 128])
    pm_d = dt_in("pmats", [12, 128, 128])
    iota_d = dt_in("iota16", [128, 16])
    iota128_d = dt_in("iota128", [128, 128])
    y_d = nc.dram_tensor("y", [TOK, D], F32, kind="ExternalOutput").ap()
    skind = "ExternalOutput" if debug else "Internal"
    h1_dram = nc.dram_tensor("h1_scr", [TOK, D], F32, kind=skind).ap()
    h2t_dram = nc.dram_tensor("h2t_scr", [128, 8, TOK], BF16, kind=skind).ap()
    g_dram = nc.dram_tensor("g_scr", [16, 128, TOK * 8], BF16, kind=skind).ap()

    with ExitStack() as stack:
        P = Prog(nc, stack)
        for _i in range(EXTRA_SEMS):
            P.new_sem("dummy%d" % _i)

        def sb(name, shape, dt=F32):
            t = stack.enter_context(nc.sbuf_tensor("sb_" + name, list(shape), dt))
            return t, Res(name)

        def ps(name, shape, dt=F32):
            t = stack.enter_context(nc.psum_tensor("ps_" + name, list(shape), dt))
            return t, Res(name)

        poolw_bf, R_poolw = sb("poolw_bf", [128, 4, 128], BF16)
        wst_bf, R_wst = sb("wst_bf", [128, 4, 128], BF16)
        ident_f, R_identf = sb("ident_f", [128, 128], F32)
        ident_bf, R_ident = sb("ident_bf", [128, 128], BF16)
        pm_bf, R_pm = sb("pm_bf", [128, 12, 128], BF16)
        g1col, R_g1col = sb("g1col", [128, 8])
        pscale, R_pscale = sb("pscale", [128, 4])
        iota16, R_iota = sb("iota16", [128, 16])
        iota128, R_iota128 = sb("iota128", [128, 128])
        g2_bc, R_g2 = sb("g2_bc", [128, D])
        fg_bc, R_fg = sb("fg_bc", [128, D])
        sgug_bc, R_sgug = sb("sgug_bc", [128, 512])
        b_bc, R_bbc = sb("b_bc", [128, 512])
        kt = [sb("kt%d" % i, [128, 128]) for i in range(2)]
        small, R_small = sb("small", [128, 32])
        st6, R_st6 = sb("st6", [128, 8])
        zsum, R_zsum = sb("zsum", [128, 16])

        arena = stack.enter_context(nc.sbuf_tensor("sb_arena", [128, ARENA_COLS], F32))
        bump = [0]

        def alloc(name, cols, dt=F32):
            f32cols = cols if dt in (F32, U32, I32) else (cols + 1) // 2
            o = bump[0]
            bump[0] += f32cols
            assert bump[0] <= ARENA_COLS, (name, bump[0])
            a = arena[:, o:o + f32cols]
            if dt != F32:
                a = a.bitcast(dt)
            return a, Res(name)

        w_in_bf, R_w_in = alloc("w_in_bf", 8 * D_IN, BF16)
        w_out_bf, R_w_out = alloc("w_out_bf", 8 * D, BF16)
        wk_bf, R_wk = alloc("wk_bf", 8 * 2048, BF16)
        w_in_v = w_in_bf.rearrange("p (k c) -> p k c", k=8)
        w_out_v = w_out_bf.rearrange("p (k c) -> p k c", k=8)
        wk_v = wk_bf.rearrange("p (k c) -> p k c", k=8)
        ring, _ = alloc("ring", NS * 1024)
        R_slot = [Res("slot%d" % i) for i in range(NS)]
        S_slot = [P.new_sem("s_slot%d" % i) for i in range(max(NS, NS2))]
        xbuf = [alloc("xbuf%d" % i, D) for i in range(2)]
        S_x = [P.new_sem("s_x%d" % i) for i in range(2)]
        xn_bf, R_xn = alloc("xn_bf", D, BF16)
        xnT, R_xnT = alloc("xnT", 8 * 128, BF16)
        xnT_v = xnT.rearrange("p (k t) -> p k t", k=8)
        zp_sb = [alloc("zp_sb%d" % i, 512, BF16) for i in range(2)]
        d_sb, R_d = alloc("d_sb", 512, BF16)
        ab_sb, R_ab = alloc("ab_sb", 8 * 128, BF16)
        ab_v = ab_sb.rearrange("p (k t) -> p k t", k=8)
        u_sb, R_u = alloc("u_sb", 512)
        v_f, R_v = alloc("v_f", 512)
        vn_bf, R_vn = alloc("vn_bf", 512, BF16)
        h1, R_h1 = alloc("h1", D)
        junk, R_junk = alloc("junk", D)
        scores_b = [alloc("scores%d" % i, 2048) for i in range(2)]
        top_s, R_tops = alloc("top_s", 256)
        top_i, R_topi = alloc("top_i", 256, U32)
        top_if, R_topif = alloc("top_if", 256)
        stmp, R_stmp = alloc("stmp", 256)
        best_s, R_best = alloc("best_s", 128)
        pos_u, R_pos = alloc("pos_u", 128, U32)
        hi_u, R_hiu = alloc("hi_u", 128, U32)
        lo_u, R_lou = alloc("lo_u", 128, U32)
        hif, R_hif = alloc("hif", 128)
        lof, R_lof = alloc("lof", 128)
        i1, R_i1 = alloc("i1", 128)
        i2, R_i2 = alloc("i2", 128)
        ework, R_ework = alloc("ework", 128)
        gate, R_gate = alloc("gate", 128)
        aT_sb, R_aT = alloc("aT_sb", 3 * 128)
        aT_v = aT_sb.rearrange("p (q t) -> p q t", q=3)
        Pbuf = [alloc("Pb%d" % i, TB * 128, BF16) for i in range(2)]
        Qbuf = [alloc("Qb%d" % i, TB * 128, BF16) for i in range(1)]
        Gst = [alloc("Gst%d" % i, 16 * TB * 8, BF16) for i in range(2)]
        S_gst = [P.new_sem("s_gst%d" % i) for i in range(2)]
        S_h1 = P.new_sem("s_h1")
        S_xnT = P.new_sem("s_xnT")
        S_c = {}

        def csem(name):
            S_c[name] = P.new_sem("s_" + name)
            return S_c[name]

        ptr, R_ptr = ps("ptr", [128, 8, 128], BF16)
        pAB, _ = ps("pAB", [128, 1024])
        pCD, _ = ps("pCD", [128, 1024])
        pGs, _ = ps("pGs", [128, 1024])
        pT, R_pT = ps("pT", [128, 512])
        pA, pB, pC, pD = pAB[:, 0:512], pAB[:, 512:1024], pCD[:, 0:512], pCD[:, 512:1024]
        R_pA, R_pB, R_pC, R_pD = [Res("pb%d" % i) for i in range(4)]
        pGsb = [pGs[:, 0:512], pGs[:, 512:1024]]
        R_pGs = [Res("pGs%d" % i) for i in range(2)]

        def slot(i, n=1024):
            return ring[:, i * 1024: i * 1024 + n]

        def load_const(dst, R, src, name):
            s = csem(name)
            P.emit("sp", lambda e: e.dma_start(out=dst, in_=src), writes=[R], dma_sem=s)

        load_const(ident_f[:], R_identf, ident_d, "ident")
        load_const(g1col[:], R_g1col, g1col_d, "g1col")
        load_const(pscale[:], R_pscale, pscale_d, "pscale")
        load_const(iota16[:], R_iota, iota_d, "iota")
        load_const(iota128[:], R_iota128, iota128_d, "iota128")
        load_const(g2_bc[:], R_g2, g2_d.partition_broadcast(128), "g2")
        load_const(fg_bc[:], R_fg, fg_d.partition_broadcast(128), "fg")
        load_const(sgug_bc[:], R_sgug, sgug_d.partition_broadcast(128), "sgug")
        load_const(b_bc[:], R_bbc, sgub_d.partition_broadcast(128), "bbc")
        P.emit("dve", lambda e: e.tensor_copy(out=ident_bf[:], in_=ident_f[:]), reads=[R_identf], writes=[R_ident])

        stage_ctr = [0]

        def stage_load(src_ap, ncols, view=None):
            i = stage_ctr[0] % NS
            stage_ctr[0] += 1
            dst = slot(i, ncols)
            if view is not None:
                dst = view(dst)
            P.emit("sp", lambda e: e.dma_start(out=dst, in_=src_ap), writes=[R_slot[i]], dma_sem=S_slot[i])
            return i

        cast_ctr = [0]

        def cast(dst, src, reads, writes, scale_ap=None):
            k = cast_ctr[0]
            cast_ctr[0] += 1
            if k % 2 == 0:
                if scale_ap is None:
                    P.emit("dve", lambda e: e.tensor_copy(out=dst, in_=src), reads=reads, writes=writes)
                else:
                    P.emit("dve", lambda e: e.tensor_scalar(out=dst, in0=src, scalar1=scale_ap, scalar2=None, op0=ALU.mult),
                           reads=reads, writes=writes)
            else:
                if scale_ap is None:
                    P.emit("act", lambda e: e.activation(out=dst, in_=src, func=AF.Copy), reads=reads, writes=writes)
                else:
                    P.emit("act", lambda e: e.activation(out=dst, in_=src, func=AF.Copy, scale=scale_ap), reads=reads, writes=writes)

        for half in range(2):
            i = stage_load(pm_d[half * 6:(half + 1) * 6].rearrange("m a b -> a m b"), 768,
                           view=lambda a: a.rearrange("p (m b) -> p m b", m=6))
            cast(pm_bf[:, half * 6:(half + 1) * 6, :], slot(i, 768).rearrange("p (m b) -> p m b", m=6),
                 [R_slot[i]], [R_pm])
        for kc in range(8):
            for (c0, c1) in ((0, 1024), (1024, 1536)):
                i = stage_load(w_in_d[kc * 128:(kc + 1) * 128, c0:c1], c1 - c0)
                cast(w_in_v[:, kc, c0:c1], slot(i, c1 - c0), [R_slot[i], R_g1col], [R_w_in], scale_ap=g1col[:, kc:kc + 1])
        for kc in range(8):
            i = stage_load(w_out_d[kc * 128:(kc + 1) * 128, :], 1024)
            cast(w_out_v[:, kc, :], slot(i), [R_slot[i]], [R_w_out])
        i = stage_load(pool_w_d.rearrange("g c d -> c g d"), 512, view=lambda a: a.rearrange("p (g d) -> p g d", g=4))
        cast(poolw_bf[:], slot(i, 512).rearrange("p (g d) -> p g d", g=4), [R_slot[i]], [R_poolw])
        i = stage_load(wst_d.rearrange("h j i -> j h i"), 512, view=lambda a: a.rearrange("p (h i) -> p h i", h=4))
        cast(wst_bf[:], slot(i, 512).rearrange("p (h i) -> p h i", h=4), [R_slot[i]], [R_wst])
        P.emit("dve", lambda e: e.memset(wst_bf[64:128, :, 0:64], 0.0), writes=[R_wst])
        for hp in range(16):
            i = stage_load(wqt_d[hp], 1024)
            ktile, R_kt = kt[hp % 2]
            if hp < 2:
                csem("kt%d" % hp)
            s_kt = S_c["kt%d" % (hp % 2)]
            P.emit("sp", lambda e, ktile=ktile, hp=hp: e.dma_start(out=ktile[:], in_=keyst_d[hp]), writes=[R_kt], dma_sem=s_kt)
            banks = ((pA, R_pA), (pB, R_pB)) if hp % 2 == 0 else ((pC, R_pC), (pD, R_pD))
            for b in range(2):
                pb_t, R_pb = banks[b]
                for q in range(4):
                    kc = b * 4 + q
                    P.emit("pe", lambda e, pb_t=pb_t, q=q, i=i, kc=kc, ktile=ktile: e.matmul(
                        pb_t[:, q * 128:(q + 1) * 128], lhsT=slot(i)[:, kc * 128:(kc + 1) * 128], rhs=ktile[:],
                        start=True, stop=True), reads=[R_slot[i], R_kt], writes=[R_pb], inc_sem=(q == 3))
                cast(wk_v[:, b * 4:(b + 1) * 4, hp * 128:(hp + 1) * 128],
                     pb_t.rearrange("p (q k) -> p q k", q=4), [R_pb], [R_wk])

        def load_x(n):
            xt, R_x = xbuf[n % 2]
            P.emit("sp", lambda e: e.dma_start(out=xt, in_=x_d[n * 128:(n + 1) * 128, :]), writes=[R_x], dma_sem=S_x[n % 2])

        def rms_stats(src, R_src, jk, R_jk, col):
            P.emit("act", lambda e: e.activation(out=jk, in_=src, func=AF.Square, accum_out=small[:, col:col + 1]),
                   reads=[R_src], writes=[R_jk, R_small])
            P.emit("act", lambda e: e.activation(out=small[:, col + 1:col + 2], in_=small[:, col:col + 1], func=AF.Sqrt,
                                                 scale=1.0 / D, bias=EPS), reads=[R_small], writes=[R_small])
            P.emit("dve", lambda e: e.reciprocal(out=small[:, col + 2:col + 3], in_=small[:, col + 1:col + 2]),
                   reads=[R_small], writes=[R_small])
            return small[:, col + 2:col + 3]

        def transposes(src_bf, R_src, dstT, R_dst):
            for kc in range(8):
                P.emit("pe", lambda e, kc=kc: e.transpose(ptr[:, kc, :], src_bf[:, kc * 128:(kc + 1) * 128], ident_bf[:]),
                       reads=[R_src, R_ident], writes=[R_ptr], inc_sem=(kc == 7))
            P.emit("act", lambda e: e.activation(out=dstT, in_=ptr[:], func=AF.Copy), reads=[R_ptr], writes=[R_dst])

        def stage_A(n):
            xt, R_x = xbuf[n % 2]
            first = (n % TILES_PER_SEQ == 0)
            zp_cur, R_zpc = zp_sb[n % 2]
            zp_prev, R_zpp = zp_sb[(n + 1) % 2]

            rstd1 = rms_stats(xt, R_x, xn_bf, R_xn, 0)
            yield
            P.emit("act", lambda e: e.activation(out=xn_bf, in_=xt, func=AF.Copy, scale=rstd1),
                   reads=[R_x, R_small], writes=[R_xn])
            yield
            transposes(xn_bf, R_xn, xnT_v, R_xnT)
            for kc in range(8):
                P.emit("pe", lambda e, kc=kc: e.matmul(pA, lhsT=xnT_v[:, kc, :], rhs=w_in_v[:, kc, 0:512],
                                                       start=(kc == 0), stop=(kc == 7)),
                       reads=[R_xnT, R_w_in], writes=[R_pA], inc_sem=(kc == 7))
            for kc in range(8):
                P.emit("pe", lambda e, kc=kc: e.matmul(pB, lhsT=xnT_v[:, kc, :], rhs=w_in_v[:, kc, 1024:1536],
                                                       start=(kc == 0), stop=(kc == 7)),
                       reads=[R_xnT, R_w_in], writes=[R_pB], inc_sem=(kc == 7))
            for m in range(4):
                for kc in range(8):
                    P.emit("pe", lambda e, kc=kc, m=m: e.matmul(pC[:, m * 128:(m + 1) * 128],
                                                                lhsT=w_in_v[:, kc, 512 + m * 128:512 + (m + 1) * 128],
                                                                rhs=xnT_v[:, kc, :], start=(kc == 0), stop=(kc == 7)),
                           reads=[R_xnT, R_w_in], writes=[R_pC], inc_sem=(kc == 7 and m == 3))
            yield
            P.emit("act", lambda e: e.activation(out=zp_cur, in_=pA, func=AF.Copy), reads=[R_pA], writes=[R_zpc])
            for g in range(4):
                mi = (8 + g) if first else g
                P.emit("pe", lambda e, g=g, mi=mi: e.matmul(
                    pD[:, g * 128:(g + 1) * 128], lhsT=zp_cur[:, g * 128:(g + 1) * 128], rhs=pm_bf[:, mi, :],
                    start=True, stop=first), reads=[R_zpc, R_pm], writes=[R_pD], inc_sem=(first and g == 3))
                if not first:
                    P.emit("pe", lambda e, g=g: e.matmul(
                        pD[:, g * 128:(g + 1) * 128], lhsT=zp_prev[:, g * 128:(g + 1) * 128], rhs=pm_bf[:, 4 + g, :],
                        start=False, stop=True), reads=[R_zpp, R_pm], writes=[R_pD], inc_sem=(g == 3))
            yield
            P.emit("dve", lambda e: e.tensor_copy(out=d_sb, in_=pD), reads=[R_pD], writes=[R_d])
            for g in range(4):
                P.emit("pe", lambda e, g=g: e.matmul(pA[:, g * 128:(g + 1) * 128], lhsT=poolw_bf[:, g, :],
                                                     rhs=d_sb[:, g * 128:(g + 1) * 128], start=True, stop=True),
                       reads=[R_poolw, R_d], writes=[R_pA], inc_sem=(g == 3))
            P.emit("dve", lambda e: e.tensor_tensor(out=ab_v[:, 0:4, :], in0=pA.rearrange("p (g t) -> p g t", g=4),
                                                    in1=bc_ap(pscale[:], [[1, 4], [0, 128]]), op=ALU.mult),
                   reads=[R_pA, R_pscale], writes=[R_ab])
            yield
            P.emit("act", lambda e: e.activation(out=u_sb, in_=pC, func=AF.Gelu_apprx_tanh), reads=[R_pC], writes=[R_u])
            P.emit("act", lambda e: e.activation(out=v_f, in_=pB, func=AF.Gelu_apprx_tanh), reads=[R_pB], writes=[R_v])
            yield
            P.emit("dve", lambda e: e.bn_stats(out=st6[:, 0:6], in_=v_f), reads=[R_v], writes=[R_st6])
            P.emit("dve", lambda e: e.bn_aggr(out=st6[:, 6:8], in_=st6[:, 0:6]), reads=[R_st6], writes=[R_st6])
            P.emit("act", lambda e: e.activation(out=small[:, 8:9], in_=st6[:, 7:8], func=AF.Sqrt, bias=EPS),
                   reads=[R_st6], writes=[R_small])
            yield
            P.emit("dve", lambda e: e.reciprocal(out=small[:, 9:10], in_=small[:, 8:9]), reads=[R_small], writes=[R_small])
            P.emit("dve", lambda e: e.tensor_scalar(out=v_f, in0=v_f, scalar1=st6[:, 6:7], scalar2=small[:, 9:10],
                                                    op0=ALU.subtract, op1=ALU.mult),
                   reads=[R_v, R_st6, R_small], writes=[R_v])
            P.emit("dve", lambda e: e.tensor_tensor(out=vn_bf, in0=v_f, in1=sgug_bc[:], op=ALU.mult),
                   reads=[R_v, R_sgug], writes=[R_vn])
            yield
            for h in range(4):
                P.emit("pe", lambda e, h=h: e.matmul(pB[:, h * 128:(h + 1) * 128], lhsT=vn_bf[:, h * 128:(h + 1) * 128],
                                                     rhs=wst_bf[:, h, :], start=True, stop=True),
                       reads=[R_vn, R_wst], writes=[R_pB], inc_sem=(h == 3))
            yield
            P.emit("dve", lambda e: e.tensor_tensor(out=v_f, in0=pB, in1=b_bc[:], op=ALU.add),
                   reads=[R_pB, R_bbc], writes=[R_v])
            P.emit("dve", lambda e: e.tensor_tensor(out=ab_v[:, 4:8, :], in0=v_f.rearrange("p (h t) -> p h t", h=4),
                                                    in1=u_sb.rearrange("p (h t) -> p h t", h=4), op=ALU.mult),
                   reads=[R_v, R_u], writes=[R_ab])
            yield
            for half, (pO_, R_pO_) in enumerate(((pC, R_pC), (pD, R_pD))):
                for fc in range(8):
                    P.emit("pe", lambda e, fc=fc, half=half, pO_=pO_: e.matmul(
                        pO_, lhsT=ab_v[:, fc, :], rhs=w_out_v[:, fc, half * 512:(half + 1) * 512],
                        start=(fc == 0), stop=(fc == 7)), reads=[R_ab, R_w_out], writes=[R_pO_], inc_sem=(fc == 7))
                P.emit("dve", lambda e, half=half, pO_=pO_: e.tensor_tensor(
                    out=h1[:, half * 512:(half + 1) * 512], in0=pO_, in1=xt[:, half * 512:(half + 1) * 512], op=ALU.add),
                    reads=[R_pO_, R_x], writes=[R_h1])
            yield
            P.emit("sp", lambda e: e.dma_start(out=h1_dram[n * 128:(n + 1) * 128, :], in_=h1), reads=[R_h1], dma_sem=S_h1)
            if n + 2 < NT:
                load_x(n + 2)
            rstd2 = rms_stats(h1, R_h1, junk, R_junk, 3)
            yield
            P.emit("dve", lambda e: e.scalar_tensor_tensor(out=xn_bf, in0=h1, scalar=rstd2, in1=g2_bc[:],
                                                           op0=ALU.mult, op1=ALU.mult),
                   reads=[R_h1, R_small, R_g2], writes=[R_xn])
            transposes(xn_bf, R_xn, xnT_v, R_xnT)
            P.emit("sp", lambda e: e.dma_start(out=h2t_dram[:, :, n * 128:(n + 1) * 128], in_=xnT_v), reads=[R_xnT], dma_sem=S_xnT)
            yield
            sbanks = ((pA, R_pA), (pB, R_pB), (pC, R_pC), (pD, R_pD))
            scores, R_sc = scores_b[n % 2]
            for c, (pb_t, R_pb) in enumerate(sbanks):
                for kc in range(8):
                    P.emit("pe", lambda e, kc=kc, c=c, pb_t=pb_t: e.matmul(
                        pb_t, lhsT=xnT_v[:, kc, :], rhs=wk_v[:, kc, c * 512:(c + 1) * 512],
                        start=(kc == 0), stop=(kc == 7)), reads=[R_xnT, R_wk], writes=[R_pb], inc_sem=(kc == 7))
                P.emit("act", lambda e, c=c, pb_t=pb_t: e.activation(out=scores[:, c * 512:(c + 1) * 512], in_=pb_t, func=AF.Copy),
                       reads=[R_pb], writes=[R_sc])

        gs_ctr = [0]

        def stage_B(n):
            scores, R_sc = scores_b[n % 2]
            for g in range(16):
                sg = scores[:, g * 128:(g + 1) * 128]
                o = g * 16
                P.emit("dve", lambda e, sg=sg, o=o: e.max(out=top_s[:, o:o + 8], in_=sg), reads=[R_sc], writes=[R_tops])
                P.emit("dve", lambda e, sg=sg, o=o: e.max_index(out=top_i[:, o:o + 8], in_max=top_s[:, o:o + 8], in_values=sg),
                       reads=[R_sc, R_tops], writes=[R_topi])
                P.emit("dve", lambda e, sg=sg, o=o: e.match_replace(out=stmp[:, 0:128], in_to_replace=top_s[:, o:o + 8],
                                                                    in_values=sg, imm_value=NEG),
                       reads=[R_sc, R_tops], writes=[R_stmp])
                P.emit("dve", lambda e, o=o: e.max(out=top_s[:, o + 8:o + 16], in_=stmp[:, 0:128]),
                       reads=[R_stmp], writes=[R_tops])
                P.emit("dve", lambda e, o=o: e.max_index(out=top_i[:, o + 8:o + 16], in_max=top_s[:, o + 8:o + 16],
                                                         in_values=stmp[:, 0:128]),
                       reads=[R_stmp, R_tops], writes=[R_topi])
                if g % 2 == 1:
                    yield
            yield
            P.emit("dve", lambda e: e.tensor_copy(out=top_if, in_=top_i), reads=[R_topi], writes=[R_topif])
            cand = ring[:, 0:2048]
            R_cand = [R_slot[0], R_slot[1]]
            P.emit("dve", lambda e: e.tensor_tensor(
                out=cand.rearrange("p (h a b) -> p h a b", h=8, a=16),
                in0=bc_ap(top_s[:, 0:16], [[32, 8], [1, 16], [0, 16]]),
                in1=bc_ap(top_s[:, 16:32], [[32, 8], [0, 16], [1, 16]]), op=ALU.add),
                reads=[R_tops], writes=R_cand)
            for h in range(8):
                ch = cand[:, h * 256:(h + 1) * 256]
                o = h * 16
                P.emit("dve", lambda e, ch=ch, o=o: e.max(out=best_s[:, o:o + 8], in_=ch), reads=R_cand, writes=[R_best])
                P.emit("dve", lambda e, ch=ch, o=o: e.max_index(out=pos_u[:, o:o + 8], in_max=best_s[:, o:o + 8], in_values=ch),
                       reads=R_cand + [R_best], writes=[R_pos])
                P.emit("dve", lambda e, ch=ch, o=o: e.match_replace(out=stmp, in_to_replace=best_s[:, o:o + 8],
                                                                    in_values=ch, imm_value=NEG),
                       reads=R_cand + [R_best], writes=[R_stmp])
                P.emit("dve", lambda e, o=o: e.max(out=best_s[:, o + 8:o + 16], in_=stmp), reads=[R_stmp], writes=[R_best])
                P.emit("dve", lambda e, o=o: e.max_index(out=pos_u[:, o + 8:o + 16], in_max=best_s[:, o + 8:o + 16],
                                                         in_values=stmp),
                       reads=[R_stmp, R_best], writes=[R_pos])
                if h % 2 == 1:
                    yield
            yield
            P.emit("dve", lambda e: e.tensor_single_scalar(out=hi_u, in_=pos_u, scalar=4, op=ALU.logical_shift_right),
                   reads=[R_pos], writes=[R_hiu])
            P.emit("dve", lambda e: e.tensor_single_scalar(out=lo_u, in_=pos_u, scalar=15, op=ALU.bitwise_and),
                   reads=[R_pos], writes=[R_lou])
            P.emit("dve", lambda e: e.tensor_copy(out=hif, in_=hi_u), reads=[R_hiu], writes=[R_hif])
            P.emit("dve", lambda e: e.tensor_copy(out=lof, in_=lo_u), reads=[R_lou], writes=[R_lof])
            wk = ring[:, 2048:4096]
            R_wkk = [R_slot[2], R_slot[3]]
            for (sel, off, dst, R_dst, R_sel) in ((hif, 0, i1, R_i1, R_hif), (lof, 16, i2, R_i2, R_lof)):
                P.emit("dve", lambda e, sel=sel: e.tensor_tensor(
                    out=wk.rearrange("p (k a) -> p k a", a=16),
                    in0=bc_ap(iota16[:], [[0, 128], [1, 16]]),
                    in1=bc_ap(sel, [[1, 128], [0, 16]]), op=ALU.is_equal),
                    reads=[R_iota, R_sel], writes=R_wkk)
                P.emit("dve", lambda e, off=off: e.tensor_tensor(
                    out=wk.rearrange("p (h k a) -> p h k a", h=8, k=16),
                    in0=wk.rearrange("p (h k a) -> p h k a", h=8, k=16),
                    in1=bc_ap(top_if[:, off:off + 16], [[32, 8], [0, 16], [1, 16]]), op=ALU.mult),
                    reads=R_wkk + [R_topif], writes=R_wkk)
                P.emit("dve", lambda e, dst=dst: e.tensor_reduce(
                    out=dst, in_=wk.rearrange("p (k a) -> p k a", a=16), axis=AX.X, op=ALU.add),
                    reads=R_wkk, writes=[R_dst])
            yield
            P.emit("dve", lambda e: e.tensor_tensor(out=ework.rearrange("p (h k) -> p h k", h=8),
                                                    in0=best_s.rearrange("p (h k) -> p h k", h=8),
                                                    in1=bc_ap(best_s, [[16, 8], [0, 16]]), op=ALU.subtract),
                   reads=[R_best], writes=[R_ework])
            P.emit("act", lambda e: e.activation(out=ework, in_=ework, func=AF.Exp), reads=[R_ework], writes=[R_ework])
            yield
            P.emit("dve", lambda e: e.tensor_reduce(out=zsum[:, 0:8], in_=ework.rearrange("p (h k) -> p h k", h=8),
                                                    axis=AX.X, op=ALU.add), reads=[R_ework], writes=[R_zsum])
            P.emit("dve", lambda e: e.reciprocal(out=zsum[:, 8:16], in_=zsum[:, 0:8]), reads=[R_zsum], writes=[R_zsum])
            P.emit("dve", lambda e: e.tensor_tensor(out=gate.rearrange("p (h k) -> p h k", h=8),
                                                    in0=ework.rearrange("p (h k) -> p h k", h=8),
                                                    in1=bc_ap(zsum[:, 8:16], [[1, 8], [0, 16]]), op=ALU.mult),
                   reads=[R_ework, R_zsum], writes=[R_gate])
            yield
            for q, (src, R_src) in enumerate(((i1, R_i1), (i2, R_i2), (gate, R_gate))):
                P.emit("pe", lambda e, q=q, src=src: e.transpose(pT[:, q * 128:(q + 1) * 128], src, ident_f[:]),
                       reads=[R_src, R_identf], writes=[R_pT], inc_sem=(q == 2))
            P.emit("act", lambda e: e.activation(out=aT_sb, in_=pT[:, 0:384], func=AF.Copy), reads=[R_pT], writes=[R_aT])
            yield
            for blk in range(128 // TB):
                t0 = blk * TB
                Pb, R_Pb = Pbuf[blk % 2]
                Qb, R_Qb = Qbuf[0]
                gst, R_gst = Gst[blk % 2]
                s_gst = S_gst[blk % 2]
                P.emit("dve", lambda e, Pb=Pb, t0=t0: e.tensor_tensor(
                    out=Pb.rearrange("p (t i) -> p t i", t=TB),
                    in0=bc_ap(iota128[:], [[0, TB], [1, 128]]),
                    in1=bc_ap(aT_v[:, 0, t0:t0 + TB], [[1, TB], [0, 128]]), op=ALU.is_equal),
                    reads=[R_iota128, R_aT], writes=[R_Pb])
                P.emit("dve", lambda e, Qb=Qb, t0=t0: e.tensor_tensor(
                    out=Qb.rearrange("p (t i) -> p t i", t=TB),
                    in0=bc_ap(iota128[:], [[0, TB], [1, 128]]),
                    in1=bc_ap(aT_v[:, 1, t0:t0 + TB], [[1, TB], [0, 128]]), op=ALU.is_equal),
                    reads=[R_iota128, R_aT], writes=[R_Qb])
                P.emit("pool", lambda e, Pb=Pb, t0=t0: e.tensor_tensor(
                    out=Pb.rearrange("p (t i) -> p t i", t=TB),
                    in0=Pb.rearrange("p (t i) -> p t i", t=TB),
                    in1=bc_ap(aT_v[:, 2, t0:t0 + TB], [[1, TB], [0, 128]]), op=ALU.mult),
                    reads=[R_Pb, R_aT], writes=[R_Pb])
                for t4 in range(TB // 4):
                    bi = gs_ctr[0] % 2
                    gs_ctr[0] += 1
                    bank, R_bank = pGsb[bi], R_pGs[bi]
                    for tk in range(4):
                        t = t4 * 4 + tk
                        P.emit("pe", lambda e, bank=bank, tk=tk, t=t, Pb=Pb, Qb=Qb: e.matmul(
                            bank[:, tk * 128:(tk + 1) * 128], lhsT=Pb[:, t * 128:(t + 1) * 128], rhs=Qb[:, t * 128:(t + 1) * 128],
                            start=True, stop=True), reads=[R_Pb, R_Qb], writes=[R_bank], inc_sem=(tk == 3))
                    P.emit("act", lambda e, bank=bank, gst=gst, t4=t4: e.activation(
                        out=bc_ap(gst[:, t4 * 32:t4 * 32 + 1], [[8, 4], [TB * 8, 16], [1, 8]]),
                        in_=bank.rearrange("p (tk jg jj) -> p tk jg jj", tk=4, jg=16), func=AF.Copy),
                        reads=[R_bank], writes=[R_gst])
                tok = n * 128 + t0
                P.emit("sp", lambda e, gst=gst, tok=tok: e.dma_start(
                    out=g_dram[:, :, tok * 8:(tok + TB) * 8].rearrange("g p x -> p g x"),
                    in_=gst.rearrange("p (g x) -> p g x", g=16)), reads=[R_gst], dma_sem=s_gst)
                yield

        load_x(0)
        if NT > 1:
            load_x(1)
        for _ in stage_A(0):
            pass
        for n in range(NT):
            gb = stage_B(n)
            ga = stage_A(n + 1) if n + 1 < NT else iter(())
            done_a = done_b = False
            while not (done_a and done_b):
                if not done_b:
                    try:
                        next(gb)
                    except StopIteration:
                        done_b = True
                if not done_a:
                    try:
                        next(ga)
                    except StopIteration:
                        done_a = True

        P.barrier()
        bump[0] = 0
        accum, _ = alloc("accum", NTH * D)
        accum_v = accum.rearrange("p (n d) -> p n d", n=NTH)
        R_acc = [Res("acc%d" % i) for i in range(NTH)]
        h2T, R_h2T = alloc("h2T", 8 * THALF, BF16)
        h2T_v = h2T.rearrange("p (k t) -> p k t", k=8)
        wU = [alloc("wU%d" % i, 1024, BF16) for i in range(NW)]
        wV = [alloc("wV%d" % i, 1024, BF16) for i in range(NW)]
        ring2, _ = alloc("ring2", NS2 * 1024)
        R_slot2 = [Res("slot2_%d" % i) for i in range(NS2)]
        gsl = [alloc("gsl%d" % i, TG * 8, BF16) for i in range(2)]
        S_gsl = [P.new_sem("s_gsl%d" % i) for i in range(2)]
        gsb = [alloc("gsb%d" % i, TG) for i in range(2)]
        mt = [alloc("mt%d" % i, TG, BF16) for i in range(3)]
        ystage = [alloc("ystage%d" % i, D) for i in range(2)]
        S_ys = [P.new_sem("s_ys%d" % i) for i in range(2)]
        junk2, R_junk2 = alloc("junk2", D)
        S_acc = [P.new_sem("s_acc%d" % i) for i in range(4)]
        S_h2T = [P.new_sem("s_h2T%d" % i) for i in range(4)]
        R_h2Tq = [Res("h2Tq%d" % i) for i in range(4)]
        pat = [pGs[:, 0:TG], pGs[:, 512:512 + TG], pT[:, 0:TG]]
        R_pat = [Res("pat%d" % i) for i in range(3)]
        pO = [(pAB, Res("pO0")), (pCD, Res("pO1"))]

        def slot2(i):
            return ring2[:, i * 1024:(i + 1) * 1024]

        st2_ctr = [0]
        ys_ctr = [0]

        et_slots = {}

        def stage_etile(j):
            sl = []
            for src in (ut_d[j], vt_d[j]):
                i = st2_ctr[0] % NS2
                st2_ctr[0] += 1
                P.emit("sp", lambda e, i=i, src=src: e.dma_start(out=slot2(i), in_=src), writes=[R_slot2[i]], dma_sem=S_slot[i])
                sl.append(i)
            et_slots[j] = sl

        def cast_etile(j):
            ws = j % NW
            for i, (dst, R_dst) in zip(et_slots.pop(j), (wU[ws], wV[ws])):
                P.emit("act", lambda e, i=i, dst=dst: e.activation(out=dst, in_=slot2(i), func=AF.Copy),
                       reads=[R_slot2[i]], writes=[R_dst])

        def load_gsl(h, jg, tg, idx):
            b = idx % 2
            tok = h * THALF + tg * TG
            g_t, R_g = gsl[b]
            P.emit("act", lambda e: e.dma_start(out=g_t, in_=g_dram[jg, :, tok * 8:(tok + TG) * 8]), writes=[R_g], dma_sem=S_gsl[b])

        for h in range(NHALF):
            tok0 = h * THALF
            na = max(1, NTH // 4)
            for q in range(0, NTH, na):
                P.emit("sp", lambda e, tok0=tok0, q=q: e.dma_start(
                    out=accum_v[:, q:q + na, :],
                    in_=h1_dram[tok0 + q * 128:tok0 + (q + na) * 128, :].rearrange("(n p) d -> p n d", p=128)),
                    writes=R_acc[q:q + na], dma_sem=S_acc[q // na])
            for q in range(4):
                P.emit("sp", lambda e, tok0=tok0, q=q: e.dma_start(out=h2T_v[:, 2 * q:2 * q + 2, :],
                                                                 in_=h2t_dram[:, 2 * q:2 * q + 2, tok0:tok0 + THALF]),
                       writes=[R_h2Tq[q]], dma_sem=S_h2T[q])
            for j in range(8):
                stage_etile(j)
                cast_etile(j)
            its = [(jg, tg, jj) for jg in range(JGMAX) for tg in range(NG) for jj in range(8)]
            NI = len(its)
            load_gsl(h, 0, 0, 0)
            if NI > 0 and its[0][1] == NG - 1 and its[0][0] + 1 < JGMAX:
                stage_etile((its[0][0] + 1) * 8 + its[0][2])
            for k in range(NI + 2):
                if k < NI:
                    jg, tg, jj = its[k]
                    gidx = jg * NG + tg
                    j = jg * 8 + jj
                    ws = j % NW
                    if jj == 0:
                        if gidx + 1 < JGMAX * NG:
                            jg2, tg2 = divmod(gidx + 1, NG)
                            load_gsl(h, jg2, tg2, gidx + 1)
                    if k + 1 < NI:
                        jg_n, tg_n, jj_n = its[k + 1]
                        if tg_n == NG - 1 and jg_n + 1 < JGMAX:
                            stage_etile((jg_n + 1) * 8 + jj_n)
                    pt_, R_pt = pat[k % 3], R_pat[k % 3]
                    wU_t, R_wU = wU[ws]
                    for c in range(8):
                        P.emit("pe", lambda e, c=c, pt_=pt_, wU_t=wU_t, tg=tg: e.matmul(
                            pt_, lhsT=wU_t[:, c * 128:(c + 1) * 128], rhs=h2T_v[:, c, tg * TG:(tg + 1) * TG],
                            start=(c == 0), stop=(c == 7)), reads=[R_wU, R_h2Tq[c // 2]], writes=[R_pt], inc_sem=(c == 7))
                    g_t, R_g = gsb[k % 2]
                    P.emit("act", lambda e, pt_=pt_, g_t=g_t: e.activation(out=g_t, in_=pt_, func=AF.Gelu_apprx_tanh),
                           reads=[R_pt], writes=[R_g])
                    m_t, R_m = mt[k % 3]
                    gs_t, R_gs = gsl[gidx % 2]
                    P.emit("pool", lambda e, m_t=m_t, g_t=g_t, gs_t=gs_t, jj=jj: e.tensor_tensor(
                        out=m_t, in0=g_t, in1=bc_ap(gs_t[:, jj:jj + 1], [[8, TG]]), op=ALU.mult),
                        reads=[R_g, R_gs], writes=[R_m])
                if k >= 2:
                    jg, tg, jj = its[k - 2]
                    j = jg * 8 + jj
                    ws = j % NW
                    m_t, R_m = mt[(k - 2) % 3]
                    wV_t, R_wV = wV[ws]
                    for ti in range(TG // 128):
                        pO_t, R_pO = pO[ti]
                        for hf in range(2):
                            P.emit("pe", lambda e, pO_t=pO_t, hf=hf, ti=ti, m_t=m_t, wV_t=wV_t, jj=jj: e.matmul(
                                pO_t[:, hf * 512:(hf + 1) * 512], lhsT=m_t[:, ti * 128:(ti + 1) * 128],
                                rhs=wV_t[:, hf * 512:(hf + 1) * 512], start=(jj == 0), stop=(jj == 7)),
                                reads=[R_m, R_wV], writes=[R_pO], inc_sem=(ti == TG // 128 - 1 and hf == 1))
                    if tg == NG - 1 and jg + 1 < JGMAX:
                        cast_etile((jg + 1) * 8 + jj)
                    if jj == 7:
                        for ti in range(TG // 128):
                            pO_t, R_pO = pO[ti]
                            a = tg * (TG // 128) + ti
                            P.emit("dve", lambda e, pO_t=pO_t, a=a: e.tensor_tensor(
                                out=accum_v[:, a, :], in0=pO_t[:], in1=accum_v[:, a, :], op=ALU.add),
                                reads=[R_pO, R_acc[a]], writes=[R_acc[a]])
            for a in range(NTH):
                n = h * NTH + a
                yb = ys_ctr[0] % 2
                ys_ctr[0] += 1
                y_t, R_y = ystage[yb]
                rstd3 = rms_stats(accum_v[:, a, :], R_acc[a], junk2, R_junk2, 6)
                P.emit("dve", lambda e, a=a, y_t=y_t, rstd3=rstd3: e.scalar_tensor_tensor(
                    out=y_t, in0=accum_v[:, a, :], scalar=rstd3, in1=fg_bc[:], op0=ALU.mult, op1=ALU.mult),
                    reads=[R_acc[a], R_small, R_fg], writes=[R_y])
                P.emit("sp", lambda e, n=n, y_t=y_t: e.dma_start(out=y_d[n * 128:(n + 1) * 128, :], in_=y_t),
                       reads=[R_y], dma_sem=S_ys[yb])

        P.final_wait("sp", S_ys)

        with nc.Block() as block:
            @block.sync
            def _(e):
                for f in P.ops["sp"]:
                    f(e)

            @block.tensor
            def _(e):
                for f in P.ops["pe"]:
                    f(e)

            @block.vector
            def _(e):
                for f in P.ops["dve"]:
                    f(e)

            @block.scalar
            def _(e):
                for f in P.ops["act"]:
                    f(e)

            @block.gpsimd
            def _(e):
                for f in P.ops["pool"]:
                    f(e)
    return nc


def _pool_mats():
    m = np.zeros((12, 128, 128), np.float32)
    tp = np.arange(128)[:, None]
    t = np.arange(128)[None, :]
    for g, w in enumerate(WINDOWS):
        band = ((tp <= t) & (tp > t - w)).astype(np.float32)
        m[g] = band / w - (tp == t)
        m[4 + g] = ((tp - 128) > (t - w)).astype(np.float32) / w
        cnt = np.minimum(t + 1, w).astype(np.float32)
        m[8 + g] = band / cnt - (tp == t)
    return m


def host_layout(inp):
    f = lambda a: np.ascontiguousarray(np.asarray(a, dtype=np.float32))
    wq = f(inp["peer_wq"])[0]
    keys = f(inp["peer_keys"])[0]
    pu = f(inp["peer_u"])[0]
    pv = f(inp["peer_v"])[0]
    return {
        "w_in": f(inp["w_in"])[0],
        "w_out": f(inp["w_out"])[0],
        "pool_w": f(inp["pool_w"])[0],
        "sgu_wt": f(np.transpose(f(inp["sgu_w"])[0], (0, 2, 1))),
        "sgu_b": f(inp["sgu_b"])[0].reshape(512),
        "sgu_g": f(inp["sgu_norm_g"])[0].reshape(512),
        "g1col": f(f(inp["norm1_g"])[0].reshape(8, 128).T),
        "pscale": f(f(inp["pool_scale"])[0].reshape(4, 128).T),
        "g2": f(inp["norm2_g"])[0].reshape(D),
        "fg": f(inp["final_g"]).reshape(D),
        "wq_t": f(wq.reshape(D, 16, 128).transpose(1, 2, 0)),
        "keys_t": f(keys.transpose(1, 0, 3, 2).reshape(16, 128, 128)),
        "peer_ut": f(pu.reshape(128, 128, 8, 128).transpose(1, 3, 2, 0)).reshape(128, 128, D),
        "peer_vt": f(pv.reshape(128, 128, D).transpose(1, 0, 2)),
        "ident": np.eye(128, dtype=np.float32),
        "pmats": _pool_mats(),
        "iota16": f(np.broadcast_to(np.arange(16, dtype=np.float32), (128, 16))),
        "iota128": f(np.broadcast_to(np.arange(128, dtype=np.float32), (128, 128))),
    }


_NC_CACHE = {}


def kernel(x, norm1_g, w_in, pool_w, pool_scale, sgu_norm_g, sgu_w, sgu_b, w_out, norm2_g,
           peer_wq, peer_keys, peer_u, peer_v, final_g):
    x = np.ascontiguousarray(np.asarray(x, dtype=np.float32))
    B = x.shape[0]
    xs = x.reshape(NCORES, (B // NCORES) * SEQ, D)
    shared = host_layout(dict(norm1_g=norm1_g, w_in=w_in, pool_w=pool_w, pool_scale=pool_scale, sgu_norm_g=sgu_norm_g,
                              sgu_w=sgu_w, sgu_b=sgu_b, w_out=w_out, norm2_g=norm2_g, peer_wq=peer_wq,
                              peer_keys=peer_keys, peer_u=peer_u, peer_v=peer_v, final_g=final_g))
    if "nc" not in _NC_CACHE:
        _NC_CACHE["nc"] = build_program()
    nc = _NC_CACHE["nc"]
    in_maps = [dict(shared, x=np.ascontiguousarray(xs[c])) for c in range(NCORES)]
    res = run_bass_kernel_spmd(nc, in_maps, core_ids=list(range(NCORES)))
    out = np.stack([res.results[c]["y"] for c in range(NCORES)], axis=0)
    return out.reshape(B, SEQ, D).astype(np.float32)
```
